# Optimizing a Trainium2 kernel written in Bass

```python
import math
import jax, jax.numpy as jnp
from jax import lax
import numpy as np


D_MODEL = 2048
BATCH = 4
SEQ = 4096
DEPTH = 4

GRID_W = 64
CTX_LEN = 256
N_MIXERS = 2
N_LAYERS_A = (DEPTH + N_MIXERS - 1) // N_MIXERS
N_LAYERS_B = DEPTH // N_MIXERS

MLSTM_HEADS = 8
MLSTM_DK = D_MODEL // (2 * MLSTM_HEADS)
MLSTM_DV = D_MODEL // MLSTM_HEADS
MLSTM_CHUNK = 64
GATE_CAP = 15.0
MLSTM_SPLITS = (MLSTM_HEADS * MLSTM_DK, 2 * MLSTM_HEADS * MLSTM_DK, 2 * MLSTM_HEADS * MLSTM_DK + MLSTM_HEADS * MLSTM_DV, 2 * MLSTM_HEADS * MLSTM_DK + 2 * MLSTM_HEADS * MLSTM_DV)
MLSTM_IN = MLSTM_SPLITS[-1] + 4 * MLSTM_HEADS

DIFF_HEADS = 8
DIFF_DH = D_MODEL // (2 * DIFF_HEADS)
Q_BLOCK = 128
ROPE_BASE = 10000.0
LAMBDA_STD = 0.1

FFN_HIDDEN = -(-(8 * D_MODEL) // (3 * 256)) * 256
EPS = 1e-6

kernel_name = 'hybrid_mlstm_diffattn_block'


def rms_norm(x, g):
    xf = x.astype(jnp.float32)
    y = xf * lax.rsqrt(jnp.mean(xf * xf, axis=-1, keepdims=True) + EPS)
    return (y * g.astype(jnp.float32)).astype(x.dtype)


def modulate(h, shift, scale):
    return h * (1.0 + scale) + shift


def softcap(x, cap):
    return cap * jnp.tanh(x / cap)


def rope_2d(x, cos, sin):
    Bn, S, Hn, dh = x.shape
    xr = x.astype(jnp.float32).reshape(Bn, S, Hn, 2, 2, dh // 4)
    x1 = xr[..., 0, :]
    x2 = xr[..., 1, :]
    c = cos[None, :, None]
    s = sin[None, :, None]
    out = jnp.stack([x1 * c - x2 * s, x2 * c + x1 * s], axis=-2)
    return out.reshape(Bn, S, Hn, dh).astype(x.dtype)


def swiglu(h, w_gu, w_down):
    g, u = jnp.split(h @ w_gu, 2, axis=-1)
    return (jax.nn.silu(g) * u) @ w_down


def mlstm_scan(q, k, v, ig, lf, state):
    Bn, Hn, S, _ = q.shape
    dv = v.shape[-1]
    L = MLSTM_CHUNK
    nc = S // L

    def chunks(a):
        return jnp.moveaxis(a.reshape(Bn, Hn, nc, L, *a.shape[3:]), 2, 0)

    xs = (chunks(q), chunks(k), chunks(v), chunks(ig), chunks(lf))
    mask = jnp.tril(jnp.ones((L, L), dtype=bool))

    def step(carry, inp):
        C, n, m = carry
        qc, kc, vc, ic, fc = inp
        b = jnp.cumsum(fc, axis=-1)
        dmat = b[..., :, None] - b[..., None, :] + ic[..., None, :]
        dmat = jnp.where(mask, dmat, -jnp.inf)
        inter = b + m[..., None]
        m_t = jnp.maximum(inter, jnp.max(dmat, axis=-1))
        w_intra = jnp.exp(dmat - m_t[..., None])
        w_inter = jnp.exp(inter - m_t)
        s = jnp.einsum('bhtd,bhsd->bhts', qc, kc) * w_intra
        num = w_inter[..., None] * jnp.einsum('bhtd,bhde->bhte', qc, C) + jnp.einsum('bhts,bhse->bhte', s, vc)
        den = w_inter * jnp.einsum('bhtd,bhd->bht', qc, n) + jnp.sum(s, axis=-1)
        h = num / jnp.maximum(jnp.abs(den), jnp.exp(-m_t))[..., None]
        bL = b[..., -1]
        a = bL[..., None] - b + ic
        m_new = jnp.maximum(bL + m, jnp.max(a, axis=-1))
        wk = jnp.exp(a - m_new[..., None])
        dec = jnp.exp(bL + m - m_new)
        kw = kc * wk[..., None]
        C_new = dec[..., None, None] * C + jnp.einsum('bhsd,bhse->bhde', kw, vc)
        n_new = dec[..., None] * n + jnp.sum(kw, axis=2)
        return (C_new, n_new, m_new), h

    state, h = lax.scan(step, state, xs)
    h = jnp.moveaxis(h, 0, 2).reshape(Bn, Hn, S, dv)
    return state, h


def mlstm_mixer(hx, hc, w_in, gate_b, head_g, w_out, update_ctx):
    H, DK, DV = MLSTM_HEADS, MLSTM_DK, MLSTM_DV

    def project(h):
        Bn, S, _ = h.shape
        q, k, v, o, g = jnp.split(h @ w_in, MLSTM_SPLITS, axis=-1)

        def heads(a, d):
            return a.reshape(Bn, S, H, d).transpose(0, 2, 1, 3).astype(jnp.float32)

        q = heads(q, DK) * (DK ** -0.5)
        k = heads(k, DK)
        v = heads(v, DV)
        g = softcap(g.astype(jnp.float32) + gate_b.astype(jnp.float32), GATE_CAP)
        g = g.reshape(Bn, S, 4, H).transpose(2, 0, 3, 1)
        gates = (g[0], jax.nn.log_sigmoid(g[1]), g[2], jax.nn.log_sigmoid(g[3]))
        return q, k, v, o, gates

    qx, kx, vx, ox_gate, gx = project(hx)
    qc, kc, vc, oc_gate, gc = project(hc)
    Bn = hx.shape[0]
    state0 = (jnp.zeros((Bn, H, DK, DV), jnp.float32), jnp.zeros((Bn, H, DK), jnp.float32), jnp.zeros((Bn, H), jnp.float32))

    def flip(a):
        return jnp.flip(a, axis=2)

    st_f, hc_f = mlstm_scan(qc, kc, vc, gc[0], gc[1], state0)
    _, hx_f = mlstm_scan(qx, kx, vx, gx[0], gx[1], st_f)
    st_b, hc_b = mlstm_scan(flip(qc), flip(kc), flip(vc), flip(gc[2]), flip(gc[3]), state0)
    _, hx_b = mlstm_scan(flip(qx), flip(kx), flip(vx), flip(gx[2]), flip(gx[3]), st_b)

    def finish(hf, hb, o):
        Bs, _, S, _ = hf.shape
        h = (hf + hb).transpose(0, 2, 1, 3).astype(o.dtype)
        h = rms_norm(h, head_g.reshape(H, DV)).reshape(Bs, S, H * DV)
        return (h * jax.nn.sigmoid(o)) @ w_out

    ox = finish(hx_f, flip(hx_b), ox_gate)
    oc = finish(hc_f, flip(hc_b), oc_gate) if update_ctx else None
    return ox, oc


def diff_attn_mixer(hx, hc, w_in, w_out, q_g, k_g, lq1, lk1, lq2, lk2, subln_g, lam_init, cos, sin, update_ctx):
    H, DH = DIFF_HEADS, DIFF_DH
    scale = DH ** -0.5

    def project(h, rope):
        Bn, S, _ = h.shape
        q, k, v = jnp.split(h @ w_in, 3, axis=-1)
        q = rms_norm(q.reshape(Bn, S, 2 * H, DH), q_g)
        k = rms_norm(k.reshape(Bn, S, 2 * H, DH), k_g)
        if rope:
            q = rope_2d(q, cos, sin)
            k = rope_2d(k, cos, sin)
        v = v.reshape(Bn, S, H, 2 * DH)
        return q.transpose(0, 2, 1, 3), k.transpose(0, 2, 1, 3), v.transpose(0, 2, 1, 3)

    qx, kx, vx = project(hx, True)
    qc, kc, vc = project(hc, False)
    lam = (jnp.exp(jnp.sum(lq1.astype(jnp.float32) * lk1.astype(jnp.float32)))
           - jnp.exp(jnp.sum(lq2.astype(jnp.float32) * lk2.astype(jnp.float32))) + lam_init)

    def attend(q, k, v):
        Bn, _, Q, _ = q.shape
        s = jnp.einsum('bhqd,bhkd->bhqk', q, k).astype(jnp.float32) * scale
        p = jax.nn.softmax(s, axis=-1).reshape(Bn, H, 2, Q, k.shape[2])
        a = p[:, :, 0] - lam * p[:, :, 1]
        return jnp.einsum('bhqk,bhke->bhqe', a.astype(v.dtype), v)

    def finish(o):
        Bn, S = o.shape[0], o.shape[1]
        o = rms_norm(o, subln_g) * (1.0 - lam_init)
        return o.reshape(Bn, S, H * 2 * DH) @ w_out

    k_all = jnp.concatenate([kx, kc], axis=2)
    v_all = jnp.concatenate([vx, vc], axis=2)
    Bn, _, S, _ = qx.shape
    nqb = S // Q_BLOCK
    qb = qx.reshape(Bn, 2 * H, nqb, Q_BLOCK, DH).transpose(2, 0, 1, 3, 4)
    ob = lax.map(lambda q: attend(q, k_all, v_all), qb)
    ox = ob.transpose(1, 0, 3, 2, 4).reshape(Bn, S, H, 2 * DH)
    ox = finish(ox)
    oc = finish(attend(qc, kc, vc).transpose(0, 2, 1, 3)) if update_ctx else None
    return ox, oc


def setup_inputs(seed: int = 0) -> dict:
    key = jax.random.key(seed)
    ks = jax.random.split(key, 24)
    f32 = jnp.float32

    def nrm(k, shape, s):
        return jax.random.normal(k, shape, f32) * s

    H = MLSTM_HEADS
    gate_base = jnp.concatenate([jnp.zeros((H,), f32), jnp.linspace(3.0, 6.0, H, dtype=f32),
                                 jnp.zeros((H,), f32), jnp.linspace(3.0, 6.0, H, dtype=f32)])
    return {
        'x': nrm(ks[0], (BATCH, SEQ, D_MODEL), 1.0),
        'c': nrm(ks[1], (BATCH, D_MODEL), 1.0),
        'ctx': nrm(ks[2], (BATCH, CTX_LEN, D_MODEL), 1.0),
        'c_ctx': nrm(ks[3], (D_MODEL,), 1.0),
        'ada_w': nrm(ks[4], (DEPTH, D_MODEL, 6 * D_MODEL), 0.5 * D_MODEL ** -0.5),
        'ada_b': nrm(ks[5], (DEPTH, 6 * D_MODEL), 0.02),
        'norm_g': 1.0 + nrm(ks[6], (DEPTH, 2, D_MODEL), 0.02),
        'mlstm_w_in': nrm(ks[7], (N_LAYERS_A, D_MODEL, MLSTM_IN), D_MODEL ** -0.5),
        'mlstm_gate_b': gate_base + nrm(ks[8], (N_LAYERS_A, 4 * H), 0.1),
        'mlstm_head_g': 1.0 + nrm(ks[9], (N_LAYERS_A, H * MLSTM_DV), 0.02),
        'mlstm_w_out': nrm(ks[10], (N_LAYERS_A, H * MLSTM_DV, D_MODEL), (H * MLSTM_DV) ** -0.5),
        'diff_w_in': nrm(ks[11], (N_LAYERS_B, D_MODEL, 3 * D_MODEL), D_MODEL ** -0.5),
        'diff_w_out': nrm(ks[12], (N_LAYERS_B, D_MODEL, D_MODEL), D_MODEL ** -0.5),
        'diff_q_g': 1.0 + nrm(ks[13], (N_LAYERS_B, DIFF_DH), 0.02),
        'diff_k_g': 1.0 + nrm(ks[14], (N_LAYERS_B, DIFF_DH), 0.02),
        'diff_lq1': nrm(ks[15], (N_LAYERS_B, DIFF_DH), LAMBDA_STD),
        'diff_lk1': nrm(ks[16], (N_LAYERS_B, DIFF_DH), LAMBDA_STD),
        'diff_lq2': nrm(ks[17], (N_LAYERS_B, DIFF_DH), LAMBDA_STD),
        'diff_lk2': nrm(ks[18], (N_LAYERS_B, DIFF_DH), LAMBDA_STD),
        'diff_subln_g': 1.0 + nrm(ks[19], (N_LAYERS_B, 2 * DIFF_DH), 0.02),
        'ffn_w_gu': nrm(ks[20], (DEPTH, D_MODEL, 2 * FFN_HIDDEN), D_MODEL ** -0.5),
        'ffn_w_down': nrm(ks[21], (DEPTH, FFN_HIDDEN, D_MODEL), FFN_HIDDEN ** -0.5),
    }


def reference(x, c, ctx, c_ctx, ada_w, ada_b, norm_g, mlstm_w_in, mlstm_gate_b, mlstm_head_g, mlstm_w_out,
              diff_w_in, diff_w_out, diff_q_g, diff_k_g, diff_lq1, diff_lk1, diff_lq2, diff_lk2, diff_subln_g,
              ffn_w_gu, ffn_w_down):
    S = x.shape[1]
    ROWS = S // GRID_W
    row = jnp.repeat(jnp.arange(ROWS), GRID_W)
    col = jnp.tile(jnp.arange(GRID_W), ROWS)
    n_freq = DIFF_DH // 4
    freqs = ROPE_BASE ** (-jnp.arange(n_freq, dtype=jnp.float32) / n_freq)
    ang = jnp.stack([row, col], axis=-1).astype(jnp.float32)[:, :, None] * freqs
    cos, sin = jnp.cos(ang), jnp.sin(ang)

    for i in range(DEPTH):
        update_ctx = i < DEPTH - 1
        mx = [m[:, None, :] for m in jnp.split(jax.nn.silu(c) @ ada_w[i] + ada_b[i], 6, axis=-1)]
        mc = jnp.split(jax.nn.silu(c_ctx) @ ada_w[i] + ada_b[i], 6, axis=-1)
        hx = modulate(rms_norm(x, norm_g[i, 0]), mx[0], mx[1])
        hc = modulate(rms_norm(ctx, norm_g[i, 0]), mc[0], mc[1])
        j = i // N_MIXERS
        if i % N_MIXERS == 0:
            ox, oc = mlstm_mixer(hx, hc, mlstm_w_in[j], mlstm_gate_b[j], mlstm_head_g[j], mlstm_w_out[j], update_ctx)
        else:
            lam_init = 0.8 - 0.6 * math.exp(-0.3 * i)
            ox, oc = diff_attn_mixer(hx, hc, diff_w_in[j], diff_w_out[j], diff_q_g[j], diff_k_g[j],
                                     diff_lq1[j], diff_lk1[j], diff_lq2[j], diff_lk2[j], diff_subln_g[j],
                                     lam_init, cos, sin, update_ctx)
        x = x + mx[2] * ox
        hx2 = modulate(rms_norm(x, norm_g[i, 1]), mx[3], mx[4])
        x = x + mx[5] * swiglu(hx2, ffn_w_gu[i], ffn_w_down[i])
        if update_ctx:
            ctx = ctx + mc[2] * oc
            hc2 = modulate(rms_norm(ctx, norm_g[i, 1]), mc[3], mc[4])
            ctx = ctx + mc[5] * swiglu(hc2, ffn_w_gu[i], ffn_w_down[i])
    return x
```

```python
import math
from contextlib import ExitStack

import numpy as np
import ml_dtypes

import concourse.bass as bass
import concourse.mybir as mybir
from concourse.bass_utils import run_bass_kernel_spmd

F32 = mybir.dt.float32
BF16 = mybir.dt.bfloat16
AF = mybir.ActivationFunctionType
ALU = mybir.AluOpType
AX = mybir.AxisListType

D = 2048
FF = 5632
EPS = 1e-6
GATE_CAP = 15.0
GRID_W = 64
ROPE_BASE = 10000.0
NB = 4


class Sched:
    NDMA = 28

    def __init__(self, nc, stack):
        self.nc = nc
        self.eng = {"pe": nc.tensor, "act": nc.scalar, "dve": nc.vector, "pool": nc.gpsimd, "sp": nc.sync}
        self.sem = {}
        self.cnt = {}
        for e in ("pe", "act", "dve", "pool"):
            self.sem[e] = stack.enter_context(nc.semaphore("prog_" + e))
            self.cnt[e] = 0
        self.dsem = [stack.enter_context(nc.semaphore("dma%d" % i)) for i in range(self.NDMA)]
        self.dval = [0] * self.NDMA
        self.dnext = 0
        self.NPOOL = 6
        self.pnext = 0
        self.ccsem = stack.enter_context(nc.semaphore("cc"))
        self.ccval = 0
        self.known = {e: {} for e in self.eng}
        self.semobj = {}
        for e in self.sem:
            self.semobj[("p", e)] = self.sem[e]
        for i in range(self.NDMA):
            self.semobj[("d", i)] = self.dsem[i]
        self.semobj[("c", 0)] = self.ccsem
        self.lastw = {}
        self.readers = {}
        self.ninstr = 0

    def _wait(self, e, semkey, val):
        if semkey == ("p", "pe") and e == "pe":
            return
        k = self.known[e]
        if k.get(semkey, 0) >= val:
            return
        self.eng[e].wait_ge(self.semobj[semkey], val)
        k[semkey] = val
        self.ninstr += 1

    def _deps(self, e, reads, writes):
        for r in reads:
            w = self.lastw.get(r)
            if w is not None:
                self._wait(e, w[0], w[1])
        for wkey in writes:
            w = self.lastw.get(wkey)
            if w is not None:
                self._wait(e, w[0], w[1])
            rd = self.readers.get(wkey)
            if rd:
                for sk, v in rd.items():
                    self._wait(e, sk, v)

    def _commit(self, tok, reads, writes):
        for r in reads:
            d = self.readers.setdefault(r, {})
            if d.get(tok[0], 0) < tok[1]:
                d[tok[0]] = tok[1]
        for w in writes:
            self.lastw[w] = tok
            self.readers[w] = {}

    def op(self, e, fn, reads=(), writes=()):
        self._deps(e, reads, writes)
        ins = fn(self.eng[e])
        self.cnt[e] += 1
        ins.then_inc(self.sem[e], 1)
        self.ninstr += 1
        self._commit((("p", e), self.cnt[e]), reads, writes)
        return ins

    def dma(self, e, out, in_, reads=(), writes=(), **kw):
        self._deps(e, reads, writes)
        if e == "pool":
            i = self.NDMA - self.NPOOL + self.pnext
            self.pnext = (self.pnext + 1) % self.NPOOL
        else:
            i = self.dnext
            self.dnext = (self.dnext + 1) % (self.NDMA - self.NPOOL)
        if self.dval[i] > 0:
            self._wait(e, ("d", i), self.dval[i])
        ins = self.eng[e].dma_start(out=out, in_=in_, **kw)
        self.dval[i] += 16
        ins.then_inc(self.dsem[i], 16)
        self.ninstr += 1
        self._commit((("d", i), self.dval[i]), reads, writes)
        return ins

    def collective(self, kind, op, groups, in_ap, out_ap, reads=(), writes=()):
        e = "pool"
        self._deps(e, reads, writes)
        ins = self.eng[e].collective_compute(kind, op, replica_groups=groups, ins=[in_ap], outs=[out_ap])
        self.ccval += 1
        ins.then_inc(self.ccsem, 1)
        self.ninstr += 1
        self._commit((("c", 0), self.ccval), reads, writes)
        return ins

    def barrier(self, pool=False):
        toks = [(("p", e), self.cnt[e]) for e in self.sem if self.cnt[e] > 0 and (pool or e != "pool")]
        nd = self.NDMA if pool else self.NDMA - self.NPOOL
        toks += [(("d", i), self.dval[i]) for i in range(nd) if self.dval[i] > 0]
        for e in self.eng:
            if e == "pool" and not pool:
                continue
            for sk, v in toks:
                self._wait(e, sk, v)

    def sync_collectives(self, scratch):
        if self.ccval:
            self._wait("act", ("c", 0), self.ccval)
        self.op("act", lambda e: e.copy(out=scratch[:, 0:1], in_=scratch[:, 1:2]))
        self.barrier(pool=True)

    def finish(self):
        toks = [(("p", e), self.cnt[e]) for e in self.sem if self.cnt[e] > 0]
        toks += [(("d", i), self.dval[i]) for i in range(self.NDMA) if self.dval[i] > 0]
        if self.ccval:
            toks.append((("c", 0), self.ccval))
        for sk, v in toks:
            self._wait("sp", sk, v)


_uid = [0]


def uid(p):
    _uid[0] += 1
    return "%s#%d" % (p, _uid[0])


class Buf:
    def __init__(self, t, key):
        self.t = t
        self.k = key


STOP = [99]
DBGF = [0]


class _Stop(Exception):
    pass


def build(NTL, DEPTH, PAIR=False):
    NT = NTL + 1
    T = NT * 128
    TLAT = NTL * 128
    NA = (DEPTH + 1) // 2
    NBL = DEPTH // 2
    R = 2

    nc = bass.Bass("TRN2", target_bir_lowering=False)

    def din(name, shape, dt=F32):
        return nc.dram_tensor(name, shape, dt, kind="ExternalInput")

    def dint(name, shape, dt):
        return nc.dram_tensor(name, shape, dt)

    NV = 1 if PAIR else 2
    TP = 1 << (T - 1).bit_length()
    PAIRS = [[2 * i, 2 * i + 1] for i in range(4)]
    xin2 = din("xin", [T, D] if PAIR else [2, T, D])
    cT = din("cT", [D, R])
    sel = din("sel", [R, 256])
    ADA_SPLIT = PAIR and DEPTH % 2 == 0
    NADA = DEPTH // 2 if ADA_SPLIT else DEPTH
    ada_w = din("ada_w", [NADA, D, 6 * D])
    ada_b = din("ada_b", [DEPTH, 6 * D])
    norm_g = din("norm_g", [DEPTH, 2, D])
    m_win = din("m_win", [NA, D, 6144])
    m_wg2 = din("m_wg", [NA, D, 32] if PAIR else [2, NA, D, 32])
    m_gb2 = din("m_gb", [NA, 32] if PAIR else [2, NA, 32])
    m_hg = din("m_hg", [NA, D])
    m_wout = din("m_wout", [NA, D, D])
    if NBL:
        d_win = din("d_win", [NBL, D, 6144])
        d_wout = din("d_wout", [NBL, D, D])
        d_vec = din("d_vec", [NBL, 6, 128])
        d_sg = din("d_sg", [NBL, 256])
    f_wgu = din("f_wgu", [DEPTH, D, 2 * FF])
    f_wdn = din("f_wdn", [DEPTH, FF, D])
    rope2 = din("rope", [T, 128] if PAIR else [2, T, 128])
    if PAIR:
        pmask = din("pmask", [128, 2])
    consts = din("consts", [128, 512])
    yout2 = nc.dram_tensor("y", [TLAT, D] if PAIR else [2, TLAT, D], F32, kind="ExternalOutput")

    ada_full = dint("ada_full", [NADA * R, 6 * D], F32)
    if ADA_SPLIT:
        ada_all = dint("ada_all", [2 * NADA * R, 6 * D], F32)
    actT = dint("actT", [FF, T], BF16)
    EXW = 8 * 258

    class VC:
        pass

    VCS = []
    for v in range(NV):
        V = VC()
        V.idx = v
        V.xin = xin2.ap() if PAIR else xin2[v]
        V.rope = rope2.ap() if PAIR else rope2[v]
        V.y = yout2.ap() if PAIR else yout2[v]
        V.m_wg = m_wg2.ap() if PAIR else m_wg2[v]
        V.m_gb = m_gb2.ap() if PAIR else m_gb2[v]
        V.xres = dint("xres%d" % v, [T, D], F32)
        V.mq = [dint("mq%d_%d" % (v, i), [T, 1024], BF16) for i in range(2)]
        V.mk = [dint("mk%d_%d" % (v, i), [T, 1024], BF16) for i in range(2)]
        V.mv = dint("mv%d" % v, [T, 2048], BF16)
        V.mo = dint("mo%d" % v, [T, 2048], BF16)
        V.mhA = dint("mhA%d" % v, [T, 2048], F32)
        V.st = [dint("st%d_%d" % (v, i), [128, EXW], F32) for i in range(3)]
        if NBL:
            V.aqT = dint("aqT%d" % v, [2048, T], BF16)
            V.akT = dint("akT%d" % v, [2048, TP], BF16)
            V.av = dint("av%d" % v, [TP, 2048], BF16)
        VCS.append(V)
    if PAIR:
        PV = VC()
        PV.st = [dint("pst%d" % i, [128, EXW], F32) for i in range(3)]
        EXP = 4096
        ex_in = [[dint("ex_in%d_%d" % (i, p), [128, EXP], F32) for p in range(2)] for i in range(2)]
        ex_out = [[dint("ex_out%d_%d" % (i, p), [256, EXP], F32) for p in range(2)] for i in range(2)]
        if NBL:
            NVC = (T + 255) // 256
            akc_in = [dint("akc_in%d" % k, [128, 4096], BF16) for k in range(16)]
            akc = [dint("akc%d" % k, [256, 4096], BF16) for k in range(16)]
            avc_in = [dint("avc_in%d" % k, [256, 2048], BF16) for k in range(NVC)]
            avc = [dint("avc%d" % k, [512, 2048], BF16) for k in range(NVC)]

    with ExitStack() as st:
        S = Sched(nc, st)

        def sb(stack, name, shape, dt):
            return Buf(stack.enter_context(nc.sbuf_tensor(uid(name), shape, dt)), uid(name))

        def ps(stack, name, shape, dt):
            return Buf(stack.enter_context(nc.psum_tensor(uid(name), shape, dt)), uid(name))

        cst = sb(st, "cst", [128, 512], F32)
        cstb = sb(st, "cstb", [128, 512], BF16)
        scr = sb(st, "scr", [128, 2], F32)
        selt = sb(st, "selt", [R, 256], F32)
        PF = [ps(st, "pf%d" % i, [128, 512], F32) for i in range(6)]
        PB = [ps(st, "pb%d" % i, [128, 1024], BF16) for i in range(2)]
        S.dma("sp", cst.t[:], consts[:, :], writes=[cst.k])
        for V in VCS:
            V.GF = sb(st, "GF", [128, NT, 48], F32)
        if PAIR:
            pmk = sb(st, "pmk", [128, 2], F32)
            S.dma("sp", pmk.t[:], pmask[:, :], writes=[pmk.k])
        S.dma("sp", selt.t[:], sel[:, :], writes=[selt.k])
        S.op("dve", lambda e: e.tensor_copy(out=cstb.t[:], in_=cst.t[:]), reads=[cst.k], writes=[cstb.k])
        ident = cstb.t[:, 0:128]
        triU_f, triL_f, ones_f = cst.t[:, 128:256], cst.t[:, 256:384], cst.t[:, 384:512]
        CK = [cst.k, cstb.k]

        stopped = [False]

        def stop(level):
            if STOP[0] <= level:
                stopped[0] = True
            return stopped[0]

        wfull = {}

        def layer_weights(i):
            j = i // 2
            if i % 2 == 0:
                return [("win%d" % i, m_win[j], D, 6144), ("wout%d" % i, m_wout[j], D, D),
                        ("wgu%d" % i, f_wgu[i], D, 2 * FF), ("wdn%d" % i, f_wdn[i], FF, D)]
            return [("win%d" % i, d_win[j], D, 6144), ("wout%d" % i, d_wout[j], D, D),
                    ("wgu%d" % i, f_wgu[i], D, 2 * FF), ("wdn%d" % i, f_wdn[i], FF, D)]

        def cast_layer(i):
            for (tag, src, rows, ncols) in layer_weights(i):
                full = dint(tag + "_bf", [rows, ncols], BF16)
                kfull = uid(tag + "bf")
                step = 128
                keys = []
                for r0 in range(0, rows, step):
                    r1 = min(rows, r0 + step)
                    S.dma("pool", full[r0:r1, :], src[r0:r1, :], writes=[kfull + str(r0)])
                    keys.append(kfull + str(r0))
                wfull[tag] = (full, keys)

        def ada_phase():
            with ExitStack() as ph:
                cTt = sb(ph, "cTt", [128, 16, R], F32)
                sil = sb(ph, "sil", [128, 16, R], F32)
                S.dma("sp", cTt.t[:], cT.ap().rearrange("(k p) r -> p k r", p=128), writes=[cTt.k])
                S.op("act", lambda e: e.activation(out=sil.t[:], in_=cTt.t[:], func=AF.Silu), reads=[cTt.k], writes=[sil.k])
                wts = [sb(ph, "adaw%d" % i, [128, 16, 512], F32) for i in range(2)]
                outb = [sb(ph, "adao%d" % i, [R, 512], F32) for i in range(2)]
                n = 0
                for i in range(NADA):
                    for cb in range(24):
                        w = wts[n % 2]
                        ob = outb[n % 2]
                        pf = PF[n % 4]
                        n += 1
                        S.dma("sp", w.t[:], ada_w[i].rearrange("(k p) n -> p k n", p=128)[:, :, cb * 512:(cb + 1) * 512], writes=[w.k])
                        for kc in range(16):
                            S.op("pe", lambda e, pf=pf, w=w, kc=kc: e.matmul(pf.t[0:R, :], lhsT=sil.t[:, kc, :], rhs=w.t[:, kc, :],
                                                                             start=(kc == 0), stop=(kc == 15)), reads=[sil.k, w.k], writes=[pf.k])
                        S.op("dve", lambda e, pf=pf, ob=ob: e.tensor_copy(out=ob.t[:], in_=pf.t[0:R, :]), reads=[pf.k], writes=[ob.k])
                        S.dma("act", ada_full[i * R:(i + 1) * R, cb * 512:(cb + 1) * 512], ob.t[:], reads=[ob.k], writes=[])
                if ADA_SPLIT:
                    S.barrier(pool=True)
                    S.collective("AllGather", ALU.bypass, PAIRS, ada_full.ap().opt(), ada_all.ap().opt(), writes=["ada_all"])
                else:
                    S.barrier()

        def mod_tmps(ph):
            return (sb(ph, "modrow", [R, 2048], F32), sb(ph, "modb", [R, 2048], F32), sb(ph, "modg", [128, 2048], F32))

        def mod_tile(ph, tmps, i, which, vec, kind, gidx=None):
            out = sb(ph, "mod", [128, 2048], F32)
            row, brow, gt = tmps
            if ADA_SPLIT:
                r0 = (i % 2) * NADA * R + (i // 2) * R
                S.dma("sp", row.t[:], ada_all[r0:r0 + R, vec * 2048:(vec + 1) * 2048], reads=["ada_all"], writes=[row.k])
            else:
                S.dma("sp", row.t[:], ada_full[i * R:(i + 1) * R, vec * 2048:(vec + 1) * 2048], reads=[], writes=[row.k])
            S.dma("sp", brow.t[:], ada_b[i, vec * 2048:(vec + 1) * 2048].partition_broadcast(R), writes=[brow.k])
            S.op("dve", lambda e: e.tensor_tensor(out=row.t[:], in0=row.t[:], in1=brow.t[:], op=ALU.add), reads=[row.k, brow.k], writes=[row.k])
            if kind == "scale":
                S.dma("sp", gt.t[:], norm_g[i, gidx, :].partition_broadcast(128), writes=[gt.k])
            for q in range(4):
                pf = PF[q]
                S.op("pe", lambda e, pf=pf, q=q: e.matmul(pf.t[:], lhsT=selt.t[:, which * 128:(which + 1) * 128],
                                                          rhs=row.t[:, q * 512:(q + 1) * 512], start=True, stop=True),
                     reads=[selt.k, row.k], writes=[pf.k])
                if kind == "scale":
                    S.op("dve", lambda e, pf=pf, q=q: e.scalar_tensor_tensor(
                        out=out.t[:, q * 512:(q + 1) * 512], in0=pf.t[:], scalar=1.0, in1=gt.t[:, q * 512:(q + 1) * 512],
                        op0=ALU.add, op1=ALU.mult), reads=[pf.k, gt.k], writes=[out.k])
                else:
                    S.op("dve", lambda e, pf=pf, q=q: e.tensor_copy(out=out.t[:, q * 512:(q + 1) * 512], in_=pf.t[:]),
                         reads=[pf.k], writes=[out.k])
            return out

        def norm_phase(ph, XT, i, which_norm, src):
            vs, vsh = (1, 0) if which_norm == 0 else (4, 3)
            tmps = mod_tmps(ph)
            A = [mod_tile(ph, tmps, i, w, vs, "scale", which_norm) for w in range(2)]
            Sh = [mod_tile(ph, tmps, i, w, vsh, "raw") for w in range(2)]
            xt = [sb(ph, "nx", [128, 2048], F32) for _ in range(2)]
            junk = sb(ph, "njunk", [128, 2048], BF16)
            tmp = sb(ph, "ntmp", [128, 2048], F32)
            hb = [sb(ph, "nhb", [128, 2048], BF16) for _ in range(2)]
            ssq = [sb(ph, "nss", [128, 1], F32) for _ in range(2)]
            for j in range(NT):
                w = 0 if j < NTL else 1
                x = xt[j % 2]
                h = hb[j % 2]
                ss = ssq[j % 2]
                S.dma("sp", x.t[:], src[j * 128:(j + 1) * 128, :], writes=[x.k])
                S.op("act", lambda e, x=x, ss=ss: e.activation(out=junk.t[:], in_=x.t[:], func=AF.Square, accum_out=ss.t[:]),
                     reads=[x.k], writes=[junk.k, ss.k])
                S.op("act", lambda e, ss=ss: e.activation(out=ss.t[:], in_=ss.t[:], func=AF.Sqrt, scale=1.0 / D, bias=EPS),
                     reads=[ss.k], writes=[ss.k])
                S.op("dve", lambda e, ss=ss: e.reciprocal(out=ss.t[:], in_=ss.t[:]),
                     reads=[ss.k], writes=[ss.k])
                S.op("dve", lambda e, x=x, ss=ss, w=w: e.scalar_tensor_tensor(out=tmp.t[:], in0=x.t[:], scalar=ss.t[:, 0:1], in1=A[w].t[:],
                                                                               op0=ALU.mult, op1=ALU.mult),
                     reads=[x.k, ss.k, A[w].k], writes=[tmp.k])
                S.op("dve", lambda e, h=h, w=w: e.tensor_tensor(out=h.t[:], in0=tmp.t[:], in1=Sh[w].t[:], op=ALU.add),
                     reads=[tmp.k, Sh[w].k], writes=[h.k])
                transpose_into(XT, h, j)

        def transpose_into(XT, h, j, nchunks=16, kc0=0):
            for half in range(0, nchunks, 8):
                n = min(8, nchunks - half)
                pb = PB[(half // 8) % 2]
                for c in range(n):
                    S.op("pe", lambda e, pb=pb, c=c, half=half: e.transpose(pb.t[:, c * 128:(c + 1) * 128], h.t[:, (half + c) * 128:(half + c + 1) * 128], ident),
                         reads=[h.k] + CK, writes=[pb.k])
                S.op("act", lambda e, pb=pb, n=n, half=half: e.copy(
                    out=XT.t[:, kc0 + half:kc0 + half + n, j * 128:(j + 1) * 128],
                    in_=pb.t[:, 0:n * 128].rearrange("p (c t) -> p c t", c=n)),
                    reads=[pb.k], writes=[XT.k + "_%d" % j])

        def lin_tm(ph, XT, wtag, col_blocks, epilogue, kchunks=16, tiles=None, bw=512):
            full, wkeys = wfull[wtag]
            slabs = [sb(ph, "slab", [128, kchunks, bw], BF16) for _ in range(2)]
            wv = full.ap().rearrange("(k p) n -> p k n", p=128)
            n = 0
            for cb in col_blocks:
                sl = slabs[n % 2]
                n += 1
                S.dma("sp", sl.t[:], wv[:, :, cb * bw:(cb + 1) * bw], reads=wkeys, writes=[sl.k])
                for j in (tiles if tiles is not None else range(NT)):
                    pf = PF[(j + n) % 4]
                    if DBGF[0] >= 2:
                        continue
                    for kc in range(kchunks):
                        S.op("pe", lambda e, pf=pf, sl=sl, kc=kc, j=j: e.matmul(
                            pf.t[:, 0:bw], lhsT=XT.t[:, kc, j * 128:(j + 1) * 128], rhs=sl.t[:, kc, :],
                            start=(kc == 0), stop=(kc == kchunks - 1)),
                            reads=[XT.k + "_%d" % j, sl.k], writes=[pf.k])
                    if DBGF[0] >= 1:
                        continue
                    epilogue(j, cb, pf)

        def make_resid_epilogue(ph, V, i, gvec, src, dst, last=False):
            tmps = mod_tmps(ph)
            G = [mod_tile(ph, tmps, i, w, gvec, "raw") for w in range(2)]
            xp = [sb(ph, "rx", [128, 512], F32) for _ in range(3)]
            cnt = [0]

            def epi(j, cb, pf):
                w = 0 if j < NTL else 1
                x = xp[cnt[0] % 3]
                cnt[0] += 1
                key = "xres_%d_%d" % (j, cb)
                S.dma("sp", x.t[:], src[j * 128:(j + 1) * 128, cb * 512:(cb + 1) * 512], reads=[key], writes=[x.k])
                S.op("dve", lambda e: e.tensor_tensor(out=pf.t[:], in0=pf.t[:], in1=G[w].t[:, cb * 512:(cb + 1) * 512], op=ALU.mult),
                     reads=[pf.k, G[w].k], writes=[pf.k])
                S.op("dve", lambda e: e.tensor_tensor(out=x.t[:], in0=pf.t[:], in1=x.t[:], op=ALU.add),
                     reads=[pf.k, x.k], writes=[x.k])
                if last:
                    if j < NTL:
                        S.dma("act", V.y[j * 128:(j + 1) * 128, cb * 512:(cb + 1) * 512], x.t[:], reads=[x.k], writes=[key + "y"])
                else:
                    S.dma("act", dst[j * 128:(j + 1) * 128, cb * 512:(cb + 1) * 512], x.t[:], reads=[x.k], writes=[key])
            return epi

        def ffn_gu(ph, XT, i):
            full, wkeys = wfull["wgu%d" % i]
            wv = full.ap().rearrange("(k p) n -> p k n", p=128)
            slg = [sb(ph, "slg", [128, 16, 512], BF16) for _ in range(2)]
            slu = [sb(ph, "slu", [128, 16, 512], BF16) for _ in range(2)]
            sg = [sb(ph, "sg", [128, 512], F32) for _ in range(2)]
            ao = [sb(ph, "ao", [128, 512], BF16) for _ in range(3)]
            tblocks = [(t0, min(512, T - t0)) for t0 in range(0, T, 512)]
            n = 0
            m = 0
            for cb in range(FF // 512):
                g = slg[n % 2]
                u = slu[n % 2]
                n += 1
                S.dma("sp", g.t[:], wv[:, :, cb * 512:(cb + 1) * 512], reads=wkeys, writes=[g.k])
                S.dma("sp", u.t[:], wv[:, :, FF + cb * 512:FF + (cb + 1) * 512], reads=wkeys, writes=[u.k])
                for (t0, tw) in tblocks:
                    tkeys = [XT.k + "_%d" % j for j in range(t0 // 128, (t0 + tw) // 128)]
                    for fc in range(4):
                        pg = PF[(2 * m) % 6]
                        pu = PF[(2 * m + 1) % 6]
                        s_ = sg[m % 2]
                        a = ao[m % 3]
                        m += 1
                        for (pp, sl) in ((pg, g), (pu, u)):
                            for kc in range(16):
                                S.op("pe", lambda e, pp=pp, sl=sl, kc=kc: e.matmul(
                                    pp.t[:, 0:tw], lhsT=sl.t[:, kc, fc * 128:(fc + 1) * 128], rhs=XT.t[:, kc, t0:t0 + tw],
                                    start=(kc == 0), stop=(kc == 15)), reads=[sl.k] + tkeys, writes=[pp.k])
                        S.op("act", lambda e: e.activation(out=s_.t[:, 0:tw], in_=pg.t[:, 0:tw], func=AF.Silu), reads=[pg.k], writes=[s_.k])
                        S.op("dve", lambda e: e.tensor_tensor(out=a.t[:, 0:tw], in0=s_.t[:, 0:tw], in1=pu.t[:, 0:tw], op=ALU.mult),
                             reads=[s_.k, pu.k], writes=[a.k])
                        f0 = cb * 512 + fc * 128
                        S.dma("act", actT[f0:f0 + 128, t0:t0 + tw], a.t[:, 0:tw], reads=[a.k], writes=[])

        def ffn_down(ph, V, i, src, last):
            full, wkeys = wfull["wdn%d" % i]
            wv = full.ap().rearrange("(k p) n -> p k n", p=128)
            KC = FF // 128
            slabs = [sb(ph, "dslab", [128, KC, 512], BF16) for _ in range(2)]
            at = [sb(ph, "dact", [128, KC, 128], BF16) for _ in range(2)]
            epi = make_resid_epilogue(ph, V, i, 5, src, V.xres, last)
            av_ = actT.ap().rearrange("(k p) t -> p k t", p=128)
            n = 0
            m = 0
            for cb in range(4):
                sl = slabs[n % 2]
                n += 1
                S.dma("sp", sl.t[:], wv[:, :, cb * 512:(cb + 1) * 512], reads=wkeys, writes=[sl.k])
                for j in range(NT):
                    if last and j >= NTL:
                        continue
                    a = at[m % 2]
                    pf = PF[m % 4]
                    m += 1
                    S.dma("sp", a.t[:], av_[:, :, j * 128:(j + 1) * 128], reads=[], writes=[a.k])
                    for kc in range(KC):
                        S.op("pe", lambda e, kc=kc: e.matmul(pf.t[:], lhsT=a.t[:, kc, :], rhs=sl.t[:, kc, :], start=(kc == 0), stop=(kc == KC - 1)),
                             reads=[a.k, sl.k], writes=[pf.k])
                    epi(j, cb, pf)

        def mlstm_pre(V, i, src):
            j_ = i // 2
            with ExitStack() as ph:
                XT = sb(ph, "XT", [128, 16, T], BF16)
                with ExitStack() as ph1:
                    norm_phase(ph1, XT, i, 0, src)
                    S.barrier()
                if stop(2):
                    return
                GF = V.GF
                mq, mk, mv, mo = V.mq, V.mk, V.mv, V.mo
                with ExitStack() as ph2:
                    wgf = sb(ph2, "wgf", [128, 16, 32], F32)
                    wgb = sb(ph2, "wgb", [128, 16, 32], BF16)
                    gbt = sb(ph2, "gbt", [128, 32], F32)
                    S.dma("sp", wgf.t[:], V.m_wg[j_].rearrange("(k p) n -> p k n", p=128), writes=[wgf.k])
                    S.dma("sp", gbt.t[:], V.m_gb[j_, :].partition_broadcast(128), writes=[gbt.k])
                    S.op("dve", lambda e: e.tensor_copy(out=wgb.t[:], in_=wgf.t[:]), reads=[wgf.k], writes=[wgb.k])
                    gr = sb(ph2, "gr", [128, 32], F32)
                    ex = sb(ph2, "gex", [128, 16], F32)
                    cum = sb(ph2, "cum", [128, 32], F32)
                    for j in range(NT):
                        pf = PF[j % 2]
                        for kc in range(16):
                            S.op("pe", lambda e, kc=kc: e.matmul(pf.t[:, 0:32], lhsT=XT.t[:, kc, j * 128:(j + 1) * 128], rhs=wgb.t[:, kc, :],
                                                                  start=(kc == 0), stop=(kc == 15)),
                                 reads=[XT.k + "_%d" % j, wgb.k], writes=[pf.k])
                        S.op("dve", lambda e: e.tensor_tensor(out=gr.t[:], in0=pf.t[:, 0:32], in1=gbt.t[:], op=ALU.add), reads=[pf.k, gbt.k], writes=[gr.k])
                        S.op("act", lambda e: e.activation(out=gr.t[:], in_=gr.t[:], func=AF.Tanh, scale=1.0 / GATE_CAP), reads=[gr.k], writes=[gr.k])
                        S.op("dve", lambda e: e.tensor_single_scalar(out=gr.t[:], in_=gr.t[:], scalar=GATE_CAP, op=ALU.mult), reads=[gr.k], writes=[gr.k])
                        fv = gr.t[:, :].rearrange("p (a b) -> p a b", a=2)[:, :, 8:16]
                        exv = ex.t[:, :].rearrange("p (a b) -> p a b", a=2)
                        S.op("act", lambda e: e.activation(out=exv, in_=fv, func=AF.Exp, scale=-1.0), reads=[gr.k], writes=[ex.k])
                        S.op("act", lambda e: e.activation(out=exv, in_=exv, func=AF.Ln, bias=1.0), reads=[ex.k], writes=[ex.k])
                        S.op("dve", lambda e: e.tensor_single_scalar(out=fv, in_=exv, scalar=-1.0, op=ALU.mult), reads=[ex.k], writes=[gr.k])
                        pc = PF[2 + j % 2]
                        S.op("pe", lambda e: e.matmul(pc.t[:, 0:8], lhsT=triU_f, rhs=gr.t[:, 8:16], start=True, stop=True), reads=[gr.k] + CK, writes=[pc.k])
                        S.op("pe", lambda e: e.matmul(pc.t[:, 8:16], lhsT=triL_f, rhs=gr.t[:, 24:32], start=True, stop=True), reads=[gr.k] + CK, writes=[pc.k])
                        S.op("pe", lambda e: e.matmul(pc.t[:, 16:24], lhsT=ones_f, rhs=gr.t[:, 8:16], start=True, stop=True), reads=[gr.k] + CK, writes=[pc.k])
                        S.op("pe", lambda e: e.matmul(pc.t[:, 24:32], lhsT=ones_f, rhs=gr.t[:, 24:32], start=True, stop=True), reads=[gr.k] + CK, writes=[pc.k])
                        S.op("dve", lambda e: e.tensor_copy(out=cum.t[:], in_=pc.t[:, 0:32]), reads=[pc.k], writes=[cum.k])
                        g = GF.t[:, j, :]
                        gk = GF.k + "_%d" % j
                        S.op("act", lambda e: e.activation(out=g[:, 0:8], in_=cum.t[:, 0:8], func=AF.Exp), reads=[cum.k], writes=[gk])
                        S.op("act", lambda e: e.activation(out=g[:, 16:24], in_=cum.t[:, 8:16], func=AF.Exp), reads=[cum.k], writes=[gk])
                        S.op("act", lambda e: e.activation(out=g[:, 32:48], in_=cum.t[:, 16:32], func=AF.Exp), reads=[cum.k], writes=[gk])
                        S.op("dve", lambda e: e.tensor_single_scalar(out=g[:, 0:8], in_=g[:, 0:8], scalar=128.0 ** -0.5, op=ALU.mult), reads=[gk], writes=[gk])
                        S.op("dve", lambda e: e.tensor_single_scalar(out=g[:, 16:24], in_=g[:, 16:24], scalar=128.0 ** -0.5, op=ALU.mult), reads=[gk], writes=[gk])
                        S.op("dve", lambda e: e.tensor_tensor(out=cum.t[:, 0:8], in0=gr.t[:, 0:8], in1=cum.t[:, 0:8], op=ALU.subtract), reads=[cum.k, gr.k], writes=[cum.k])
                        S.op("dve", lambda e: e.tensor_tensor(out=cum.t[:, 8:16], in0=gr.t[:, 16:24], in1=cum.t[:, 8:16], op=ALU.subtract), reads=[cum.k, gr.k], writes=[cum.k])
                        S.op("act", lambda e: e.activation(out=g[:, 8:16], in_=cum.t[:, 0:8], func=AF.Exp), reads=[cum.k], writes=[gk])
                        S.op("act", lambda e: e.activation(out=g[:, 24:32], in_=cum.t[:, 8:16], func=AF.Exp), reads=[cum.k], writes=[gk])
                    S.barrier()
                if stop(3):
                    return
                with ExitStack() as ph3:
                    ob = [sb(ph3, "pob", [128, 2, 512], BF16) for _ in range(3)]
                    cnt = [0]

                    def epi(j, cb, pf):
                        o = ob[cnt[0] % 3]
                        cnt[0] += 1
                        gk = GF.k + "_%d" % j
                        if cb < 4:
                            isk = cb >= 2
                            h0 = (cb % 2) * 4
                            for sty in range(2):
                                c0 = sty * 16 + (8 if isk else 0) + h0
                                S.op("dve", lambda e, sty=sty, c0=c0: e.tensor_tensor(
                                    out=o.t[:, sty, :].rearrange("p (h d) -> p h d", h=4),
                                    in0=pf.t[:, :].rearrange("p (h d) -> p h d", h=4),
                                    in1=GF.t[:, j, c0:c0 + 4].unsqueeze(2).broadcast_to([128, 4, 128]), op=ALU.mult),
                                    reads=[pf.k, gk], writes=[o.k])
                                dst = (mk if isk else mq)[sty]
                                S.dma("act", dst[j * 128:(j + 1) * 128, (cb % 2) * 512:(cb % 2 + 1) * 512], o.t[:, sty, :], reads=[o.k], writes=[])
                        elif cb < 8:
                            S.op("act", lambda e: e.copy(out=o.t[:, 0, :], in_=pf.t[:]), reads=[pf.k], writes=[o.k])
                            S.dma("act", mv[j * 128:(j + 1) * 128, (cb - 4) * 512:(cb - 3) * 512], o.t[:, 0, :], reads=[o.k], writes=[])
                        else:
                            S.op("act", lambda e: e.activation(out=o.t[:, 0, :], in_=pf.t[:], func=AF.Sigmoid), reads=[pf.k], writes=[o.k])
                            S.dma("act", mo[j * 128:(j + 1) * 128, (cb - 8) * 512:(cb - 7) * 512], o.t[:, 0, :], reads=[o.k], writes=[])
                    lin_tm(ph3, XT, "win%d" % i, range(12), epi)
                    S.barrier()

        def mlstm_stage3(V, W, i):
            with ExitStack() as outer:
                XT = sb(outer, "XT", [128, 16, T], BF16)
                mlstm_scan(V, W, i, 3, XT)
                with ExitStack() as ph5:
                    epi = make_resid_epilogue(ph5, V, i, 2, (V.xin if i == 0 else V.xres), V.xres)
                    lin_tm(ph5, XT, "wout%d" % i, range(4), epi)
                    S.barrier()

        def mlstm_scan(V, W, i, stage, XT=None):
            j_ = i // 2
            GF = V.GF
            mq, mk, mv, mo, mhA = V.mq, V.mk, V.mv, V.mo, V.mhA
            with ExitStack() as ps_:
                Cf = sb(ps_, "Cf", [128, 8, 258], F32)
                Cb = sb(ps_, "Cb", [128, 8, 257], BF16)
                qt = [sb(ps_, "sq", [128, 1024], BF16) for _ in range(2)]
                kt = [sb(ps_, "sk", [128, 1024], BF16) for _ in range(2)]
                va = [sb(ps_, "sva", [128, 8, 257], BF16) for _ in range(2)]
                qkT = [sb(ps_, "qkT", [128, 4, 128], BF16) for _ in range(2)]
                stm = [sb(ps_, "stm", [128, 2, 128], BF16) for _ in range(2)]
                den = sb(ps_, "den", [128, 2], F32)
                hacc = sb(ps_, "hacc", [128, 2048], F32)
                hprev = sb(ps_, "hprev", [128, 2048], F32)
                tmpc = sb(ps_, "tmpc", [128, 2, 257], F32)
                stg = sb(ps_, "stg", [128, 8, 258], F32)
                X1 = sb(ps_, "X1", [128, 8, 258], F32)
                if stage == 3:
                    hg = sb(ps_, "hg", [128, 2048], F32)
                    og = sb(ps_, "og", [128, 2048], BF16)
                    hsq = sb(ps_, "hsq", [128, 2048], F32)
                    hss = sb(ps_, "hss", [128, 8], F32)
                    hb16 = sb(ps_, "hb16", [128, 2048], BF16)
                    S.dma("sp", hg.t[:], m_hg[j_, :].partition_broadcast(128), writes=[hg.k])
                for v_ in va:
                    S.op("dve", lambda e, v_=v_: e.memset(v_.t[:, :, 256:257], 1.0), writes=[v_.k])
                maskA = cstb.t[:, 128:256]
                maskB = cstb.t[:, 256:384]
                ntile = [0]

                def refresh_cb():
                    S.op("act", lambda e: e.copy(out=Cb.t[:], in_=Cf.t[:, :, 0:257]), reads=[Cf.k], writes=[Cb.k])

                def load_state(src_dram):
                    S.dma("sp", Cf.t[:, :, :].rearrange("p h w -> p (h w)"), src_dram[:, :], writes=[Cf.k])
                    refresh_cb()

                def save_state(dst_dram, buf):
                    S.dma("act", dst_dram[:, :], buf.t[:, :, :].rearrange("p h w -> p (h w)"), reads=[buf.k], writes=[])

                def chunk(j, sty, out_final):
                    n = ntile[0]
                    ntile[0] += 1
                    q, k, v = qt[n % 2], kt[n % 2], va[n % 2]
                    S.dma("sp", q.t[:], mq[sty][j * 128:(j + 1) * 128, :], reads=[], writes=[q.k])
                    S.dma("sp", k.t[:], mk[sty][j * 128:(j + 1) * 128, :], reads=[], writes=[k.k])
                    S.dma("sp", v.t[:, :, 0:256], mv[j * 128:(j + 1) * 128, :].rearrange("p (h d) -> p h d", h=8), reads=[], writes=[v.k])
                    if out_final:
                        S.dma("sp", hprev.t[:], mhA[j * 128:(j + 1) * 128, :], reads=["mhA_%d" % j], writes=[hprev.k])
                        S.dma("sp", og.t[:], mo[j * 128:(j + 1) * 128, :], reads=[], writes=[og.k])
                    mask = maskA if sty == 0 else maskB
                    dcol = 32 + sty * 8
                    gk = GF.k + "_%d" % j
                    for hp in range(4):
                        qk_ = qkT[hp % 2]
                        sm = stm[hp % 2]
                        pb = PB[hp % 2]
                        for c, (srcb, col) in enumerate(((q, 2 * hp), (q, 2 * hp + 1), (k, 2 * hp), (k, 2 * hp + 1))):
                            S.op("pe", lambda e, c=c, srcb=srcb, col=col: e.transpose(pb.t[:, c * 128:(c + 1) * 128], srcb.t[:, col * 128:(col + 1) * 128], ident),
                                 reads=[srcb.k] + CK, writes=[pb.k])
                        S.op("act", lambda e: e.copy(out=qk_.t[:], in_=pb.t[:, 0:512].rearrange("p (c t) -> p c t", c=4)), reads=[pb.k], writes=[qk_.k])
                        pst = PF[0]
                        for hh in range(2):
                            S.op("pe", lambda e, hh=hh: e.matmul(pst.t[:, hh * 128:(hh + 1) * 128], lhsT=qk_.t[:, 2 + hh, :], rhs=qk_.t[:, hh, :], start=True, stop=True),
                                 reads=[qk_.k], writes=[pst.k])
                        S.op("dve", lambda e: e.tensor_tensor(out=sm.t[:], in0=pst.t[:, 0:256].rearrange("p (h t) -> p h t", h=2),
                                                              in1=mask.unsqueeze(1).broadcast_to([128, 2, 128]), op=ALU.mult),
                             reads=[pst.k] + CK, writes=[sm.k])
                        for hh in range(2):
                            h = 2 * hp + hh
                            pn = PF[1 + hh]
                            S.op("pe", lambda e, hh=hh, h=h, pn=pn: e.matmul(pn.t[:, 0:257], lhsT=sm.t[:, hh, :], rhs=v.t[:, h, :], start=True, stop=False),
                                 reads=[sm.k, v.k], writes=[pn.k])
                            S.op("pe", lambda e, hh=hh, h=h, pn=pn: e.matmul(pn.t[:, 0:257], lhsT=qk_.t[:, hh, :], rhs=Cb.t[:, h, :], start=False, stop=True),
                                 reads=[qk_.k, Cb.k], writes=[pn.k])
                            pd = PF[3 + hh]
                            S.op("pe", lambda e, h=h, pd=pd: e.matmul(pd.t[:, 0:257], lhsT=k.t[:, h * 128:(h + 1) * 128], rhs=v.t[:, h, :], start=True, stop=True),
                                 reads=[k.k, v.k], writes=[pd.k])
                            S.op("act", lambda e, hh=hh, pn=pn: e.activation(out=den.t[:, hh:hh + 1], in_=pn.t[:, 256:257], func=AF.Abs), reads=[pn.k], writes=[den.k])
                            S.op("dve", lambda e, hh=hh: e.tensor_single_scalar(out=den.t[:, hh:hh + 1], in_=den.t[:, hh:hh + 1], scalar=1.0, op=ALU.max), reads=[den.k], writes=[den.k])
                            S.op("dve", lambda e, hh=hh: e.reciprocal(out=den.t[:, hh:hh + 1], in_=den.t[:, hh:hh + 1]), reads=[den.k], writes=[den.k])
                            if out_final:
                                S.op("dve", lambda e, hh=hh, h=h, pn=pn: e.scalar_tensor_tensor(
                                    out=hacc.t[:, h * 256:(h + 1) * 256], in0=pn.t[:, 0:256], scalar=den.t[:, hh:hh + 1], in1=hprev.t[:, h * 256:(h + 1) * 256],
                                    op0=ALU.mult, op1=ALU.add), reads=[pn.k, den.k, hprev.k], writes=[hacc.k])
                            else:
                                S.op("dve", lambda e, hh=hh, h=h, pn=pn: e.tensor_scalar(
                                    out=hacc.t[:, h * 256:(h + 1) * 256], in0=pn.t[:, 0:256], scalar1=den.t[:, hh:hh + 1], scalar2=None, op0=ALU.mult),
                                    reads=[pn.k, den.k], writes=[hacc.k])
                            S.op("dve", lambda e, hh=hh, h=h, pd=pd: e.tensor_tensor(out=tmpc.t[:, hh, :], in0=pd.t[:, 0:257], in1=Cf.t[:, h, 0:257], op=ALU.add),
                                 reads=[pd.k, Cf.k], writes=[tmpc.k])
                            S.op("dve", lambda e, hh=hh, h=h: e.tensor_scalar(out=Cf.t[:, h, 0:257], in0=tmpc.t[:, hh, :], scalar1=GF.t[:, j, dcol + h:dcol + h + 1],
                                                                          scalar2=None, op0=ALU.mult), reads=[tmpc.k, gk, Cb.k], writes=[Cf.k])
                    refresh_cb()
                    if out_final:
                        finalize(j)
                    else:
                        S.dma("act", mhA[j * 128:(j + 1) * 128, :], hacc.t[:], reads=[hacc.k], writes=["mhA_%d" % j])

                def finalize(j):
                    S.op("act", lambda e: e.activation(out=hsq.t[:], in_=hacc.t[:], func=AF.Square), reads=[hacc.k], writes=[hsq.k])
                    S.op("dve", lambda e: e.tensor_reduce(out=hss.t[:], in_=hsq.t[:, :].rearrange("p (h d) -> p h d", h=8), axis=AX.X, op=ALU.add),
                         reads=[hsq.k], writes=[hss.k])
                    S.op("act", lambda e: e.activation(out=hss.t[:], in_=hss.t[:], func=AF.Sqrt, scale=1.0 / 256, bias=EPS), reads=[hss.k], writes=[hss.k])
                    S.op("dve", lambda e: e.reciprocal(out=hss.t[:], in_=hss.t[:]), reads=[hss.k], writes=[hss.k])
                    S.op("dve", lambda e: e.tensor_tensor(out=hsq.t[:, :].rearrange("p (h d) -> p h d", h=8), in0=hacc.t[:, :].rearrange("p (h d) -> p h d", h=8),
                                                          in1=hss.t[:, :].unsqueeze(2).broadcast_to([128, 8, 256]), op=ALU.mult),
                         reads=[hacc.k, hss.k], writes=[hsq.k])
                    S.op("dve", lambda e: e.tensor_tensor(out=hsq.t[:], in0=hsq.t[:], in1=hg.t[:], op=ALU.mult), reads=[hsq.k, hg.k], writes=[hsq.k])
                    S.op("dve", lambda e: e.tensor_tensor(out=hb16.t[:], in0=hsq.t[:], in1=og.t[:], op=ALU.mult), reads=[hsq.k, og.k], writes=[hb16.k])
                    transpose_into(XT, hb16, j)


                jc = NTL
                if stage == 1:
                    S.op("dve", lambda e: e.memset(Cf.t[:], 0.0), writes=[Cf.k])
                    S.op("dve", lambda e: e.memset(Cb.t[:], 0.0), writes=[Cb.k])
                    chunk(jc, 0, False)
                    save_state(V.st[0], Cf)
                    n = ntile[0]
                    ntile[0] += 1
                    kB, vB = kt[n % 2], va[n % 2]
                    S.dma("sp", kB.t[:], mk[1][jc * 128:(jc + 1) * 128, :], writes=[kB.k])
                    S.dma("sp", vB.t[:, :, 0:256], mv[jc * 128:(jc + 1) * 128, :].rearrange("p (h d) -> p h d", h=8), writes=[vB.k])
                    for h in range(8):
                        pd = PF[3 + h % 2]
                        S.op("pe", lambda e, h=h, pd=pd: e.matmul(pd.t[:, 0:257], lhsT=kB.t[:, h * 128:(h + 1) * 128], rhs=vB.t[:, h, :], start=True, stop=True),
                             reads=[kB.k, vB.k], writes=[pd.k])
                        S.op("dve", lambda e, h=h, pd=pd: e.tensor_copy(out=stg.t[:, h, 0:257], in_=pd.t[:, 0:257]), reads=[pd.k], writes=[stg.k])
                    S.op("dve", lambda e: e.tensor_copy(out=stg.t[:, :, 257:258], in_=GF.t[:, jc, 40:48].unsqueeze(2)), reads=[GF.k + "_%d" % jc], writes=[stg.k])
                    save_state(V.st[1], stg)
                elif stage == 2:
                    S.dma("sp", Cf.t[:, :, :].rearrange("p h w -> p (h w)"), V.st[0][:, :], writes=[Cf.k])
                    S.dma("sp", X1.t[:, :, :].rearrange("p h w -> p (h w)"), W.st[1][:, :], writes=[X1.k])
                    S.op("dve", lambda e: e.tensor_tensor(out=Cf.t[:, :, 0:257], in0=Cf.t[:, :, 0:257], in1=X1.t[:, :, 0:257], op=ALU.add), reads=[Cf.k, X1.k], writes=[Cf.k])
                    S.op("dve", lambda e: e.tensor_tensor(out=Cf.t[:, :, 0:257], in0=Cf.t[:, :, 0:257], in1=X1.t[:, :, 257:258].broadcast_to([128, 8, 257]), op=ALU.mult),
                         reads=[Cf.k, X1.k], writes=[Cf.k])
                    refresh_cb()
                    for j in range(NTL):
                        chunk(j, 0, False)
                    save_state(V.st[2], Cf)
                else:
                    load_state(W.st[0])
                    chunk(jc, 1, True)
                    load_state(W.st[2])
                    for j in range(NTL - 1, -1, -1):
                        chunk(j, 1, True)
                S.barrier()

        def attn_pre(V, i, src):
            j_ = i // 2
            aqT, akT, av = V.aqT, V.akT, V.av
            rope = V.rope
            with ExitStack() as ph:
                XT = sb(ph, "XT", [128, 16, T], BF16)
                with ExitStack() as ph1:
                    norm_phase(ph1, XT, i, 0, src)
                    S.barrier()
                with ExitStack() as ph2:
                    gq = sb(ph2, "gq", [128, 2, 128], F32)
                    S.dma("sp", gq.t[:], d_vec[j_, 0:2, :].partition_broadcast(128), writes=[gq.k])
                    S.op("dve", lambda e: e.tensor_single_scalar(out=gq.t[:, 0, :], in_=gq.t[:, 0, :], scalar=128.0 ** -0.5, op=ALU.mult), reads=[gq.k], writes=[gq.k])
                    rp = sb(ph2, "rp", [128, NT, 128], F32)
                    S.dma("sp", rp.t[:], rope.rearrange("(j p) c -> p j c", p=128), writes=[rp.k])
                    sq_l = [sb(ph2, "asq", [128, 512], F32) for _ in range(2)]
                    qss_l = [sb(ph2, "qss", [128, 4], F32) for _ in range(2)]
                    qn_l = [sb(ph2, "qn", [128, 512], F32) for _ in range(2)]
                    t1_l = [sb(ph2, "t1", [128, 512], F32) for _ in range(2)]
                    t2_l = [sb(ph2, "t2", [128, 512], F32) for _ in range(2)]
                    qo = [sb(ph2, "qo", [128, 512], BF16) for _ in range(2)]
                    qTs = [sb(ph2, "qTs", [128, 4, 128], BF16) for _ in range(2)]
                    vo = [sb(ph2, "vo", [128, 512], BF16) for _ in range(2)]
                    cnt = [0]

                    def epi(j, cb, pf):
                        n = cnt[0]
                        cnt[0] += 1
                        sq_, qss, qn, t1, t2 = sq_l[n % 2], qss_l[n % 2], qn_l[n % 2], t1_l[n % 2], t2_l[n % 2]
                        if cb < 8:
                            isk = cb >= 4
                            S.op("act", lambda e: e.activation(out=sq_.t[:], in_=pf.t[:], func=AF.Square), reads=[pf.k], writes=[sq_.k])
                            S.op("dve", lambda e: e.tensor_reduce(out=qss.t[:], in_=sq_.t[:, :].rearrange("p (h d) -> p h d", h=4), axis=AX.X, op=ALU.add),
                                 reads=[sq_.k], writes=[qss.k])
                            S.op("act", lambda e: e.activation(out=qss.t[:], in_=qss.t[:], func=AF.Sqrt, scale=1.0 / 128, bias=EPS), reads=[qss.k], writes=[qss.k])
                            S.op("dve", lambda e: e.reciprocal(out=qss.t[:], in_=qss.t[:]), reads=[qss.k], writes=[qss.k])
                            S.op("dve", lambda e: e.tensor_tensor(out=qn.t[:, :].rearrange("p (h d) -> p h d", h=4), in0=pf.t[:, :].rearrange("p (h d) -> p h d", h=4),
                                                                  in1=qss.t[:, :].unsqueeze(2).broadcast_to([128, 4, 128]), op=ALU.mult), reads=[pf.k, qss.k], writes=[qn.k])
                            S.op("dve", lambda e: e.tensor_tensor(out=qn.t[:, :].rearrange("p (h d) -> p h d", h=4), in0=qn.t[:, :].rearrange("p (h d) -> p h d", h=4),
                                                                   in1=gq.t[:, 1 if isk else 0, :].unsqueeze(1).broadcast_to([128, 4, 128]), op=ALU.mult),
                                 reads=[qn.k, gq.k], writes=[qn.k])
                            cosv = rp.t[:, j, 0:64].rearrange("p (a f) -> p a f", a=2)
                            sinv = rp.t[:, j, 64:128].rearrange("p (a f) -> p a f", a=2)
                            qv5 = qn.t[:, :].rearrange("p (h a j f) -> p h a j f", h=4, a=2, j=2)
                            t15 = t1.t[:, :].rearrange("p (h a j f) -> p h a j f", h=4, a=2, j=2)
                            t25 = t2.t[:, :].rearrange("p (h a j f) -> p h a j f", h=4, a=2, j=2)
                            for jj in range(2):
                                S.op("dve", lambda e, jj=jj: e.tensor_tensor(out=t15[:, :, :, jj, :], in0=qv5[:, :, :, jj, :],
                                                                             in1=cosv.unsqueeze(1).broadcast_to([128, 4, 2, 32]), op=ALU.mult),
                                     reads=[qn.k, rp.k], writes=[t1.k])
                                S.op("dve", lambda e, jj=jj: e.tensor_tensor(out=t25[:, :, :, jj, :], in0=qv5[:, :, :, 1 - jj, :],
                                                                              in1=sinv.unsqueeze(1).broadcast_to([128, 4, 2, 32]), op=ALU.mult),
                                     reads=[qn.k, rp.k], writes=[t2.k])
                            o = qo[n % 2]
                            o5 = o.t[:, :].rearrange("p (h a j f) -> p h a j f", h=4, a=2, j=2)
                            S.op("dve", lambda e: e.tensor_tensor(out=o5[:, :, :, 0, :], in0=t15[:, :, :, 0, :], in1=t25[:, :, :, 0, :], op=ALU.subtract),
                                 reads=[t1.k, t2.k], writes=[o.k])
                            S.op("dve", lambda e: e.tensor_tensor(out=o5[:, :, :, 1, :], in0=t15[:, :, :, 1, :], in1=t25[:, :, :, 1, :], op=ALU.add),
                                 reads=[t1.k, t2.k], writes=[o.k])
                            pb = PB[n % 2]
                            for c in range(4):
                                S.op("pe", lambda e, c=c: e.transpose(pb.t[:, c * 128:(c + 1) * 128], o.t[:, c * 128:(c + 1) * 128], ident), reads=[o.k] + CK, writes=[pb.k])
                            qT_ = qTs[n % 2]
                            S.op("act", lambda e: e.copy(out=qT_.t[:], in_=pb.t[:, 0:512].rearrange("p (c t) -> p c t", c=4)), reads=[pb.k], writes=[qT_.k])
                            dst = akT if isk else aqT
                            r0 = (cb % 4) * 512
                            if isk and PAIR:
                                for c in range(4):
                                    S.dma("act", akc_in[(cb % 4) * 4 + c][:, j * 128:(j + 1) * 128], qT_.t[:, c, :], reads=[qT_.k], writes=[])
                            else:
                                S.dma("act", dst[r0:r0 + 512, j * 128:(j + 1) * 128].rearrange("(c p) t -> p c t", p=128), qT_.t[:], reads=[qT_.k], writes=[])
                        else:
                            o = vo[n % 2]
                            S.op("act", lambda e: e.copy(out=o.t[:], in_=pf.t[:]), reads=[pf.k], writes=[o.k])
                            if PAIR:
                                S.dma("act", avc_in[j // 2][(j % 2) * 128:(j % 2 + 1) * 128, (cb - 8) * 512:(cb - 7) * 512], o.t[:], reads=[o.k], writes=[])
                            else:
                                S.dma("act", av[j * 128:(j + 1) * 128, (cb - 8) * 512:(cb - 7) * 512], o.t[:], reads=[o.k], writes=[])
                    lin_tm(ph2, XT, "win%d" % i, range(12), epi)
                    S.barrier()

        def attn_core(V, i):
            j_ = i // 2
            lam_init = 0.8 - 0.6 * math.exp(-0.3 * i)
            aqT = V.aqT
            with ExitStack() as ph:
                XT = sb(ph, "XT", [128, 16, T], BF16)
                lamt = sb(ph, "lamt", [128, 2], F32)
                sgt = sb(ph, "sgt", [128, 256], F32)
                with ExitStack() as ph2:
                    lv = sb(ph2, "lv", [128, 4, 128], F32)
                    S.dma("sp", lv.t[:], d_vec[j_, 2:6, :].partition_broadcast(128), writes=[lv.k])
                    lj = sb(ph2, "lj", [128, 2, 128], F32)
                    ls = sb(ph2, "ls", [128, 2], F32)
                    lvv = lv.t[:, :, :].rearrange("p (a b) d -> p a b d", a=2)
                    S.op("dve", lambda e: e.tensor_tensor(out=lj.t[:], in0=lvv[:, :, 0, :], in1=lvv[:, :, 1, :], op=ALU.mult), reads=[lv.k], writes=[lj.k])
                    S.op("dve", lambda e: e.tensor_reduce(out=ls.t[:], in_=lj.t[:], axis=AX.X, op=ALU.add), reads=[lj.k], writes=[ls.k])
                    S.op("act", lambda e: e.activation(out=ls.t[:], in_=ls.t[:], func=AF.Exp), reads=[ls.k], writes=[ls.k])
                    S.op("dve", lambda e: e.tensor_tensor(out=lamt.t[:, 0:1], in0=ls.t[:, 1:2], in1=ls.t[:, 0:1], op=ALU.subtract), reads=[ls.k], writes=[lamt.k])
                    S.op("dve", lambda e: e.tensor_single_scalar(out=lamt.t[:, 0:1], in_=lamt.t[:, 0:1], scalar=-lam_init, op=ALU.add), reads=[lamt.k], writes=[lamt.k])
                    S.dma("sp", sgt.t[:], d_sg[j_, :].partition_broadcast(128), writes=[sgt.k])
                    S.op("dve", lambda e: e.tensor_single_scalar(out=sgt.t[:], in_=sgt.t[:], scalar=1.0 - lam_init, op=ALU.mult), reads=[sgt.k], writes=[sgt.k])
                    S.barrier()
                with ExitStack() as ph3:
                    NKT = 2 * NT
                    kTs = [sb(ph3, "kTs", [128, 2, 2 * T], BF16) for _ in range(2)]
                    vs_ = [sb(ph3, "vs", [128, NKT, 257], BF16) for _ in range(2)]
                    qs_ = [sb(ph3, "qs", [128, 2, T], BF16) for _ in range(2)]
                    pt = [sb(ph3, "pt", [128, 2, 256], BF16) for _ in range(3)]
                    rd = sb(ph3, "rd", [128, 2], F32)
                    o1 = sb(ph3, "o1", [128, 256], F32)
                    o2 = sb(ph3, "o2", [128, 256], F32)
                    oss = sb(ph3, "oss", [128, 1], F32)
                    ob16 = sb(ph3, "ob16", [128, 256], BF16)
                    for v_ in vs_:
                        S.op("dve", lambda e, v_=v_: e.memset(v_.t[:, :, 256:257], 1.0), writes=[v_.k])
                    if PAIR:
                        kalls = valls = None
                    else:
                        kalls = [VCS[r_].akT.ap().rearrange("(s d) t -> d s t", d=128) for r_ in range(2)]
                        valls = [VCS[r_].av.ap().rearrange("(k p) c -> p k c", p=128) for r_ in range(2)]
                        kvr = []
                    qall = aqT.ap().rearrange("(s d) t -> d s t", d=128)
                    npt = [0]
                    for h in range(8):
                        kT_ = kTs[h % 2]
                        v_ = vs_[h % 2]
                        q_ = qs_[h % 2]
                        for r_ in range(2):
                            if PAIR:
                                for e_ in range(2):
                                    k = 2 * h + e_
                                    S.dma("sp", kT_.t[:, e_, r_ * T:(r_ + 1) * T], akc[k][r_ * 128:(r_ + 1) * 128, 0:T], reads=["akc%d" % k], writes=[kT_.k])
                                for k in range(NVC):
                                    nt_ = min(2, NT - 2 * k)
                                    S.dma("sp", v_.t[:, r_ * NT + 2 * k:r_ * NT + 2 * k + nt_, 0:256],
                                          avc[k][r_ * 256:r_ * 256 + nt_ * 128, h * 256:(h + 1) * 256].rearrange("(k p) c -> p k c", p=128),
                                          reads=["avc%d" % k], writes=[v_.k])
                            else:
                                S.dma("sp", kT_.t[:, :, r_ * T:(r_ + 1) * T], kalls[r_][:, 2 * h:2 * h + 2, 0:T], reads=kvr, writes=[kT_.k])
                                S.dma("sp", v_.t[:, r_ * NT:(r_ + 1) * NT, 0:256], valls[r_][:, 0:NT, h * 256:(h + 1) * 256], reads=kvr, writes=[v_.k])
                        S.dma("sp", q_.t[:], qall[:, 2 * h:2 * h + 2, :], reads=[], writes=[q_.k])
                        qblocks = []
                        t0 = 0
                        while t0 < TLAT:
                            tw = min(256, TLAT - t0)
                            qblocks.append((t0, tw, [kt_ for kt_ in range(NKT)]))
                            t0 += tw
                        qblocks.append((TLAT, 128, [NTL, NT + NTL]))
                        for (q0, qw, ktiles) in qblocks:
                            nq = qw // 128
                            def qk(ki):
                                kt_ = ktiles[ki]
                                pst = PF[4 + ki % 2]
                                for e_ in range(2):
                                    S.op("pe", lambda e, e_=e_, kt_=kt_, pst=pst: e.matmul(pst.t[:, e_ * 256:e_ * 256 + qw], lhsT=kT_.t[:, e_, kt_ * 128:(kt_ + 1) * 128],
                                                                                        rhs=q_.t[:, e_, q0:q0 + qw], start=True, stop=True),
                                         reads=[kT_.k, q_.k], writes=[pst.k])

                            def exp_pv(ki):
                                kt_ = ktiles[ki]
                                pst = PF[4 + ki % 2]
                                p_ = pt[npt[0] % 3]
                                npt[0] += 1
                                S.op("act", lambda e: e.activation(out=p_.t[:, :, 0:qw], in_=pst.t[:, :].rearrange("p (e q) -> p e q", e=2)[:, :, 0:qw], func=AF.Exp),
                                     reads=[pst.k], writes=[p_.k])
                                for e_ in range(2):
                                    for qt_ in range(nq):
                                        po = PF[e_ * 2 + qt_]
                                        S.op("pe", lambda e, e_=e_, qt_=qt_, po=po, kt_=kt_: e.matmul(
                                            po.t[:, 0:257], lhsT=p_.t[:, e_, qt_ * 128:(qt_ + 1) * 128], rhs=v_.t[:, kt_, :],
                                            start=(ki == 0), stop=(ki == len(ktiles) - 1)), reads=[p_.k, v_.k], writes=[po.k])

                            qk(0)
                            for ki in range(len(ktiles)):
                                if ki + 1 < len(ktiles):
                                    qk(ki + 1)
                                exp_pv(ki)
                            for qt_ in range(nq):
                                j = q0 // 128 + qt_
                                p0 = PF[qt_]
                                p1 = PF[2 + qt_]
                                S.op("dve", lambda e: e.reciprocal(out=rd.t[:, 0:1], in_=p0.t[:, 256:257]), reads=[p0.k], writes=[rd.k])
                                S.op("dve", lambda e: e.reciprocal(out=rd.t[:, 1:2], in_=p1.t[:, 256:257]), reads=[p1.k], writes=[rd.k])
                                S.op("dve", lambda e: e.tensor_tensor(out=rd.t[:, 1:2], in0=rd.t[:, 1:2], in1=lamt.t[:, 0:1], op=ALU.mult), reads=[rd.k, lamt.k], writes=[rd.k])
                                S.op("dve", lambda e: e.tensor_scalar(out=o1.t[:], in0=p1.t[:, 0:256], scalar1=rd.t[:, 1:2], scalar2=None, op0=ALU.mult), reads=[p1.k, rd.k], writes=[o1.k])
                                S.op("dve", lambda e: e.scalar_tensor_tensor(out=o1.t[:], in0=p0.t[:, 0:256], scalar=rd.t[:, 0:1], in1=o1.t[:], op0=ALU.mult, op1=ALU.add),
                                     reads=[p0.k, rd.k, o1.k], writes=[o1.k])
                                S.op("act", lambda e: e.activation(out=o2.t[:], in_=o1.t[:], func=AF.Square, accum_out=oss.t[:]), reads=[o1.k], writes=[o2.k, oss.k])
                                S.op("act", lambda e: e.activation(out=oss.t[:], in_=oss.t[:], func=AF.Sqrt, scale=1.0 / 256, bias=EPS), reads=[oss.k], writes=[oss.k])
                                S.op("dve", lambda e: e.reciprocal(out=oss.t[:], in_=oss.t[:]), reads=[oss.k], writes=[oss.k])
                                S.op("dve", lambda e: e.scalar_tensor_tensor(out=ob16.t[:], in0=o1.t[:], scalar=oss.t[:, 0:1], in1=sgt.t[:], op0=ALU.mult, op1=ALU.mult),
                                     reads=[o1.k, oss.k, sgt.k], writes=[ob16.k])
                                transpose_into(XT, ob16, j, nchunks=2, kc0=2 * h)
                    S.barrier()
                with ExitStack() as ph5:
                    epi = make_resid_epilogue(ph5, V, i, 2, V.xres, V.xres)
                    lin_tm(ph5, XT, "wout%d" % i, range(4), epi)
                    S.barrier()

        def ffn_layer(V, i, last):
            with ExitStack() as ph:
                XT = sb(ph, "XT", [128, 16, T], BF16)
                with ExitStack() as ph1:
                    norm_phase(ph1, XT, i, 1, V.xres)
                    S.barrier()
                with ExitStack() as ph2:
                    ffn_gu(ph2, XT, i)
                    S.barrier()
            with ExitStack() as ph3:
                ffn_down(ph3, V, i, V.xres, last)
                S.barrier()

        def pair_exchange(slot, parts):
            for p_, (src_, dst_) in enumerate(parts):
                S.dma("sp", ex_in[slot][p_][:, 0:EXW], src_[:, :])
            S.barrier(pool=True)
            for p_ in range(len(parts)):
                S.collective("AllGather", ALU.bypass, PAIRS, ex_in[slot][p_].ap().opt(), ex_out[slot][p_].ap().opt(), writes=["exout%d_%d" % (slot, p_)])
            npart = len(parts)
            with ExitStack() as ph:
                exg = sb(ph, "exg", [128, 2, EXW], F32)
                exo = sb(ph, "exo", [128, EXW], F32)
                for p_, (src_, dst_) in enumerate(parts):
                    for r_ in range(2):
                        S.dma("sp", exg.t[:, r_, :], ex_out[slot][p_][r_ * 128:(r_ + 1) * 128, 0:EXW], reads=["exout%d_%d" % (slot, p_)], writes=[exg.k])
                    S.op("dve", lambda e: e.tensor_scalar(out=exo.t[:], in0=exg.t[:, 0, :], scalar1=pmk.t[:, 0:1], scalar2=None, op0=ALU.mult),
                         reads=[exg.k, pmk.k], writes=[exo.k])
                    S.op("dve", lambda e: e.scalar_tensor_tensor(out=exo.t[:], in0=exg.t[:, 1, :], scalar=pmk.t[:, 1:2], in1=exo.t[:], op0=ALU.mult, op1=ALU.add),
                         reads=[exg.k, pmk.k, exo.k], writes=[exo.k])
                    S.dma("act", dst_[:, :], exo.t[:], reads=[exo.k])
                S.barrier(pool=True)

        def program():
            cast_layer(0)
            ada_phase()
            if stop(1):
                return
            for i in range(DEPTH):
                if i % 2 == 0:
                    for V in VCS:
                        mlstm_pre(V, i, V.xin if i == 0 else V.xres)
                        if stopped[0]:
                            return
                    if stop(4):
                        return
                    if PAIR:
                        V = VCS[0]
                        mlstm_scan(V, PV, i, 1)
                        pair_exchange(0, [(V.st[0], PV.st[0]), (V.st[1], PV.st[1])])
                        mlstm_scan(V, PV, i, 2)
                        pair_exchange(1, [(V.st[2], PV.st[2])])
                        mlstm_stage3(V, PV, i)
                    else:
                        for st_ in (1, 2):
                            for V in VCS:
                                mlstm_scan(V, VCS[1 - V.idx], i, st_)
                        for V in VCS:
                            mlstm_stage3(V, VCS[1 - V.idx], i)
                else:
                    for V in VCS:
                        attn_pre(V, i, V.xres)
                    if PAIR:
                        S.barrier(pool=True)
                        for k in range(16):
                            S.collective("AllGather", ALU.bypass, PAIRS, akc_in[k].ap().opt(), akc[k].ap().opt(), writes=["akc%d" % k])
                        for k in range(NVC):
                            S.collective("AllGather", ALU.bypass, PAIRS, avc_in[k].ap().opt(), avc[k].ap().opt(), writes=["avc%d" % k])
                    for V in VCS:
                        attn_core(V, i)
                if stop(6):
                    return
                if i + 1 < DEPTH:
                    S.barrier(pool=True)
                    cast_layer(i + 1)
                for V in VCS:
                    ffn_layer(V, i, i == DEPTH - 1)

        program()
        S.finish()
    build.ninstr = S.ninstr
    return nc


def _prep(inputs, S_len, CTX, DEPTH, ncore, pair=False):
    x = np.asarray(inputs["x"], np.float32)
    ctx = np.asarray(inputs["ctx"], np.float32)
    c = np.asarray(inputs["c"], np.float32)
    c_ctx = np.asarray(inputs["c_ctx"], np.float32)
    NTL = S_len // 2 // 128
    HL = S_len // 2
    HC = CTX // 2
    assert HC == 128
    NA = (DEPTH + 1) // 2
    NBL = DEPTH // 2
    g = lambda k: np.asarray(inputs[k], np.float32)
    n_freq = 32
    freqs = (ROPE_BASE ** (-np.arange(n_freq, dtype=np.float32) / n_freq)).astype(np.float32)
    tok = np.arange(S_len)
    ang = np.stack([tok // GRID_W, tok % GRID_W], -1).astype(np.float32)[:, :, None] * freqs
    cos = np.cos(ang).astype(np.float32).reshape(S_len, 64)
    sin = np.sin(ang).astype(np.float32).reshape(S_len, 64)
    rope_full = np.concatenate([cos, sin], 1)
    rope_ctx = np.concatenate([np.ones((HC, 64), np.float32), np.zeros((HC, 64), np.float32)], 1)
    ident = np.eye(128, dtype=np.float32)
    triU = np.triu(np.ones((128, 128), np.float32))
    triL = np.tril(np.ones((128, 128), np.float32))
    consts = np.concatenate([ident, triU, triL, np.ones((128, 128), np.float32)], 1)
    mwin = g("mlstm_w_in")
    selm = np.zeros((2, 256), np.float32)
    selm[0, 0:128] = 1.0
    selm[1, 128:256] = 1.0
    perm1 = np.concatenate([np.arange(16, 32), np.arange(0, 16)])
    wg0 = mwin[:NA, :, 6144:]
    m_wg = np.ascontiguousarray(np.stack([wg0, wg0[:, :, perm1]], 0))
    gb0 = g("mlstm_gate_b")[:NA]
    m_gb = np.ascontiguousarray(np.stack([gb0, gb0[:, perm1]], 0))
    shared = {
        "sel": selm,
        "ada_w": g("ada_w")[:DEPTH],
        "ada_b": g("ada_b")[:DEPTH],
        "norm_g": g("norm_g")[:DEPTH],
        "m_win": np.ascontiguousarray(mwin[:NA, :, 0:6144]),
        "m_wg": m_wg,
        "m_gb": m_gb,
        "m_hg": g("mlstm_head_g")[:NA],
        "m_wout": g("mlstm_w_out")[:NA],
        "f_wgu": g("ffn_w_gu")[:DEPTH],
        "f_wdn": g("ffn_w_down")[:DEPTH],
        "consts": consts,
    }
    if NBL:
        shared["d_win"] = g("diff_w_in")[:NBL]
        shared["d_wout"] = g("diff_w_out")[:NBL]
        shared["d_vec"] = np.ascontiguousarray(np.stack([g("diff_q_g")[:NBL], g("diff_k_g")[:NBL], g("diff_lq1")[:NBL], g("diff_lk1")[:NBL],
                                                         g("diff_lq2")[:NBL], g("diff_lk2")[:NBL]], 1))
        shared["d_sg"] = g("diff_subln_g")[:NBL]
    in_maps = []
    if pair:
        if DEPTH % 2 == 0:
            ada_split = [np.ascontiguousarray(g("ada_w")[s_:DEPTH:2]) for s_ in range(2)]
        for r in range(ncore):
            b, s_ = r // 2, r % 2
            xl = x[b, s_ * HL:(s_ + 1) * HL]
            cl = ctx[b, s_ * HC:(s_ + 1) * HC]
            rl = rope_full[s_ * HL:(s_ + 1) * HL]
            if s_ == 1:
                xl, cl, rl = xl[::-1], cl[::-1], rl[::-1]
            m = dict(shared)
            if DEPTH % 2 == 0:
                m["ada_w"] = ada_split[s_]
            m["m_wg"] = np.ascontiguousarray(m_wg[s_])
            m["m_gb"] = np.ascontiguousarray(m_gb[s_])
            m["xin"] = np.ascontiguousarray(np.concatenate([xl, cl], 0))
            m["rope"] = np.ascontiguousarray(np.concatenate([rl, rope_ctx], 0))
            m["cT"] = np.ascontiguousarray(np.stack([c[b], c_ctx], 1))
            pm = np.zeros((128, 2), np.float32)
            pm[:, 1 - s_] = 1.0
            m["pmask"] = pm
            in_maps.append(m)
        return in_maps, NTL
    for b in range(ncore):
        xs, rs = [], []
        for s_ in range(2):
            xl = x[b, s_ * HL:(s_ + 1) * HL]
            cl = ctx[b, s_ * HC:(s_ + 1) * HC]
            rl = rope_full[s_ * HL:(s_ + 1) * HL]
            if s_ == 1:
                xl, cl, rl = xl[::-1], cl[::-1], rl[::-1]
            xs.append(np.concatenate([xl, cl], 0))
            rs.append(np.concatenate([rl, rope_ctx], 0))
        m = dict(shared)
        m["xin"] = np.ascontiguousarray(np.stack(xs, 0))
        m["rope"] = np.ascontiguousarray(np.stack(rs, 0))
        m["cT"] = np.ascontiguousarray(np.stack([c[b], c_ctx], 1))
        in_maps.append(m)
    return in_maps, NTL


_cache = {}


def run(inputs, DEPTH=4, ncore=None, pair=True):
    x = np.asarray(inputs["x"])
    B, S_len, _ = x.shape
    if ncore is None:
        ncore = 2 * B if pair else B
    CTX = np.asarray(inputs["ctx"]).shape[1]
    in_maps, NTL = _prep(inputs, S_len, CTX, DEPTH, ncore, pair)
    key = (NTL, DEPTH, pair)
    if key not in _cache:
        _cache[key] = build(NTL, DEPTH, pair)
    nc = _cache[key]
    res = run_bass_kernel_spmd(nc, in_maps, core_ids=list(range(ncore)))
    HL = S_len // 2
    out = np.zeros((B, S_len, D), np.float32)
    if pair:
        for r in range(ncore):
            b, s_ = r // 2, r % 2
            y = res.results[r]["y"]
            out[b, s_ * HL:(s_ + 1) * HL] = y if s_ == 0 else y[::-1]
        return out
    for b in range(ncore):
        y = res.results[b]["y"]
        out[b, 0:HL] = y[0]
        out[b, HL:] = y[1][::-1]
    return out


def kernel(**inputs):
    return run(inputs, DEPTH=4)
```

```python
import math
from contextlib import ExitStack

import numpy as np
import ml_dtypes

import concourse.bass as bass
import concourse.mybir as mybir
from concourse.bass_utils import run_bass_kernel_spmd

F32 = mybir.dt.float32
BF16 = mybir.dt.bfloat16
AF = mybir.ActivationFunctionType
ALU = mybir.AluOpType
AX = mybir.AxisListType

D = 2048
FF = 5632
EPS = 1e-6
GATE_CAP = 15.0
GRID_W = 64
ROPE_BASE = 10000.0
NB = 4


class Sched:
    NDMA = 28

    def __init__(self, nc, stack):
        self.nc = nc
        self.eng = {"pe": nc.tensor, "act": nc.scalar, "dve": nc.vector, "pool": nc.gpsimd, "sp": nc.sync}
        self.sem = {}
        self.cnt = {}
        for e in ("pe", "act", "dve", "pool"):
            self.sem[e] = stack.enter_context(nc.semaphore("prog_" + e))
            self.cnt[e] = 0
        self.dsem = [stack.enter_context(nc.semaphore("dma%d" % i)) for i in range(self.NDMA)]
        self.dval = [0] * self.NDMA
        self.dnext = 0
        self.NPOOL = 2
        self.pnext = 0
        self.ccsem = stack.enter_context(nc.semaphore("cc"))
        self.ccval = 0
        self.known = {e: {} for e in self.eng}
        self.semobj = {}
        for e in self.sem:
            self.semobj[("p", e)] = self.sem[e]
        for i in range(self.NDMA):
            self.semobj[("d", i)] = self.dsem[i]
        self.semobj[("c", 0)] = self.ccsem
        self.lastw = {}
        self.readers = {}
        self.ninstr = 0

    def _wait(self, e, semkey, val):
        if semkey == ("p", "pe") and e == "pe":
            return
        k = self.known[e]
        if k.get(semkey, 0) >= val:
            return
        self.eng[e].wait_ge(self.semobj[semkey], val)
        k[semkey] = val
        self.ninstr += 1

    def _deps(self, e, reads, writes):
        for r in reads:
            w = self.lastw.get(r)
            if w is not None:
                self._wait(e, w[0], w[1])
        for wkey in writes:
            w = self.lastw.get(wkey)
            if w is not None:
                self._wait(e, w[0], w[1])
            rd = self.readers.get(wkey)
            if rd:
                for sk, v in rd.items():
                    self._wait(e, sk, v)

    def _commit(self, tok, reads, writes):
        for r in reads:
            d = self.readers.setdefault(r, {})
            if d.get(tok[0], 0) < tok[1]:
                d[tok[0]] = tok[1]
        for w in writes:
            self.lastw[w] = tok
            self.readers[w] = {}

    def op(self, e, fn, reads=(), writes=()):
        self._deps(e, reads, writes)
        ins = fn(self.eng[e])
        self.cnt[e] += 1
        ins.then_inc(self.sem[e], 1)
        self.ninstr += 1
        self._commit((("p", e), self.cnt[e]), reads, writes)
        return ins

    def dma(self, e, out, in_, reads=(), writes=(), **kw):
        self._deps(e, reads, writes)
        if e == "pool":
            i = self.NDMA - self.NPOOL + self.pnext
            self.pnext = (self.pnext + 1) % self.NPOOL
        else:
            i = self.dnext
            self.dnext = (self.dnext + 1) % (self.NDMA - self.NPOOL)
        if self.dval[i] > 0:
            self._wait(e, ("d", i), self.dval[i])
        ins = self.eng[e].dma_start(out=out, in_=in_, **kw)
        self.dval[i] += 16
        ins.then_inc(self.dsem[i], 16)
        self.ninstr += 1
        self._commit((("d", i), self.dval[i]), reads, writes)
        return ins

    def collective(self, kind, op, groups, in_ap, out_ap, reads=(), writes=()):
        e = "pool"
        self._deps(e, reads, writes)
        ins = self.eng[e].collective_compute(kind, op, replica_groups=groups, ins=[in_ap], outs=[out_ap])
        self.ccval += 1
        ins.then_inc(self.ccsem, 1)
        self.ninstr += 1
        self._commit((("c", 0), self.ccval), reads, writes)
        return ins

    def barrier(self, pool=False):
        toks = [(("p", e), self.cnt[e]) for e in self.sem if self.cnt[e] > 0 and (pool or e != "pool")]
        nd = self.NDMA if pool else self.NDMA - self.NPOOL
        toks += [(("d", i), self.dval[i]) for i in range(nd) if self.dval[i] > 0]
        for e in self.eng:
            if e == "pool" and not pool:
                continue
            for sk, v in toks:
                self._wait(e, sk, v)

    def sync_collectives(self, scratch):
        if self.ccval:
            self._wait("act", ("c", 0), self.ccval)
        self.op("act", lambda e: e.copy(out=scratch[:, 0:1], in_=scratch[:, 1:2]))
        self.barrier(pool=True)

    def finish(self):
        toks = [(("p", e), self.cnt[e]) for e in self.sem if self.cnt[e] > 0]
        toks += [(("d", i), self.dval[i]) for i in range(self.NDMA) if self.dval[i] > 0]
        if self.ccval:
            toks.append((("c", 0), self.ccval))
        for sk, v in toks:
            self._wait("sp", sk, v)


_uid = [0]


def uid(p):
    _uid[0] += 1
    return "%s#%d" % (p, _uid[0])


class Buf:
    def __init__(self, t, key):
        self.t = t
        self.k = key


STOP = [99]
DBGF = [0]


class _Stop(Exception):
    pass


def build(NTL, DEPTH, PAIR=False):
    NT = NTL + 1
    T = NT * 128
    TLAT = NTL * 128
    NA = (DEPTH + 1) // 2
    NBL = DEPTH // 2
    R = 2

    nc = bass.Bass("TRN2", target_bir_lowering=False)

    def din(name, shape, dt=F32):
        return nc.dram_tensor(name, shape, dt, kind="ExternalInput")

    def dint(name, shape, dt):
        return nc.dram_tensor(name, shape, dt)

    NV = 1 if PAIR else 2
    TP = 1 << (T - 1).bit_length()
    PAIRS = [[2 * i, 2 * i + 1] for i in range(4)]
    xin2 = din("xin", [T, D] if PAIR else [2, T, D])
    cT = din("cT", [D, R])
    sel = din("sel", [R, 256])
    ADA_SPLIT = PAIR and DEPTH % 2 == 0
    NADA = DEPTH // 2 if ADA_SPLIT else DEPTH
    ada_w = din("ada_w", [NADA, D, 6 * D])
    ada_b = din("ada_b", [DEPTH, 6 * D])
    norm_g = din("norm_g", [DEPTH, 2, D])
    m_win = din("m_win", [NA, D, 6144])
    m_wg2 = din("m_wg", [NA, D, 32] if PAIR else [2, NA, D, 32])
    m_gb2 = din("m_gb", [NA, 32] if PAIR else [2, NA, 32])
    m_hg = din("m_hg", [NA, D])
    m_wout = din("m_wout", [NA, D, D])
    if NBL:
        d_win = din("d_win", [NBL, D, 6144])
        d_wout = din("d_wout", [NBL, D, D])
        d_vec = din("d_vec", [NBL, 6, 128])
        d_sg = din("d_sg", [NBL, 256])
    f_wgu = din("f_wgu", [DEPTH, D, 2 * FF])
    f_wdn = din("f_wdn", [DEPTH, FF, D])
    rope2 = din("rope", [T, 128] if PAIR else [2, T, 128])
    if PAIR:
        pmask = din("pmask", [128, 2])
    consts = din("consts", [128, 512])
    yout2 = nc.dram_tensor("y", [TLAT, D] if PAIR else [2, TLAT, D], F32, kind="ExternalOutput")

    ada_full = dint("ada_full", [NADA * R, 6 * D], F32)
    if ADA_SPLIT:
        ada_all = dint("ada_all", [2 * NADA * R, 6 * D], F32)
    actT = dint("actT", [FF, T], BF16)
    EXW = 8 * 258

    class VC:
        pass

    VCS = []
    for v in range(NV):
        V = VC()
        V.idx = v
        V.xin = xin2.ap() if PAIR else xin2[v]
        V.rope = rope2.ap() if PAIR else rope2[v]
        V.y = yout2.ap() if PAIR else yout2[v]
        V.m_wg = m_wg2.ap() if PAIR else m_wg2[v]
        V.m_gb = m_gb2.ap() if PAIR else m_gb2[v]
        V.xres = dint("xres%d" % v, [T, D], F32)
        V.mq = [dint("mq%d_%d" % (v, i), [T, 1024], BF16) for i in range(2)]
        V.mk = [dint("mk%d_%d" % (v, i), [T, 1024], BF16) for i in range(2)]
        V.mv = dint("mv%d" % v, [T, 2048], BF16)
        V.mo = dint("mo%d" % v, [T, 2048], BF16)
        V.mhA = dint("mhA%d" % v, [T, 2048], F32)
        V.st = [dint("st%d_%d" % (v, i), [128, EXW], F32) for i in range(3)]
        if NBL:
            V.aqT = dint("aqT%d" % v, [2048, T], BF16)
            V.akT = dint("akT%d" % v, [2048, TP], BF16)
            V.av = dint("av%d" % v, [TP, 2048], BF16)
        VCS.append(V)
    if PAIR:
        PV = VC()
        PV.st = [dint("pst%d" % i, [128, EXW], F32) for i in range(3)]
        EXP = 4096
        ex_in = [[dint("ex_in%d_%d" % (i, p), [128, EXP], F32) for p in range(2)] for i in range(2)]
        ex_out = [[dint("ex_out%d_%d" % (i, p), [256, EXP], F32) for p in range(2)] for i in range(2)]
        if NBL:
            NVC = (T + 255) // 256
            akc_in = [dint("akc_in%d" % k, [128, 4096], BF16) for k in range(16)]
            akc = [dint("akc%d" % k, [256, 4096], BF16) for k in range(16)]
            avc_in = [dint("avc_in%d" % k, [256, 2048], BF16) for k in range(NVC)]
            avc = [dint("avc%d" % k, [512, 2048], BF16) for k in range(NVC)]

    with ExitStack() as st:
        S = Sched(nc, st)

        def sb(stack, name, shape, dt):
            return Buf(stack.enter_context(nc.sbuf_tensor(uid(name), shape, dt)), uid(name))

        def ps(stack, name, shape, dt):
            return Buf(stack.enter_context(nc.psum_tensor(uid(name), shape, dt)), uid(name))

        cst = sb(st, "cst", [128, 512], F32)
        cstb = sb(st, "cstb", [128, 512], BF16)
        scr = sb(st, "scr", [128, 2], F32)
        selt = sb(st, "selt", [R, 256], F32)
        PF = [ps(st, "pf%d" % i, [128, 512], F32) for i in range(6)]
        PB = [ps(st, "pb%d" % i, [128, 1024], BF16) for i in range(2)]
        S.dma("sp", cst.t[:], consts[:, :], writes=[cst.k])
        for V in VCS:
            V.GF = sb(st, "GF", [128, NT, 48], F32)
        if PAIR:
            pmk = sb(st, "pmk", [128, 2], F32)
            S.dma("sp", pmk.t[:], pmask[:, :], writes=[pmk.k])
        S.dma("sp", selt.t[:], sel[:, :], writes=[selt.k])
        S.op("dve", lambda e: e.tensor_copy(out=cstb.t[:], in_=cst.t[:]), reads=[cst.k], writes=[cstb.k])
        ident = cstb.t[:, 0:128]
        triU_f, triL_f, ones_f = cst.t[:, 128:256], cst.t[:, 256:384], cst.t[:, 384:512]
        CK = [cst.k, cstb.k]

        stopped = [False]

        def stop(level):
            if STOP[0] <= level:
                stopped[0] = True
            return stopped[0]

        wfull = {}

        def layer_weights(i):
            j = i // 2
            if i % 2 == 0:
                return [("win%d" % i, m_win[j], D, 6144), ("wout%d" % i, m_wout[j], D, D),
                        ("wgu%d" % i, f_wgu[i], D, 2 * FF), ("wdn%d" % i, f_wdn[i], FF, D)]
            return [("win%d" % i, d_win[j], D, 6144), ("wout%d" % i, d_wout[j], D, D),
                    ("wgu%d" % i, f_wgu[i], D, 2 * FF), ("wdn%d" % i, f_wdn[i], FF, D)]

        def cast_layer(i):
            for (tag, src, rows, ncols) in layer_weights(i):
                full = dint(tag + "_bf", [rows, ncols], BF16)
                kfull = uid(tag + "bf")
                step = 32
                keys = []
                for r0 in range(0, rows, step):
                    r1 = min(rows, r0 + step)
                    S.dma("pool", full[r0:r1, :], src[r0:r1, :], writes=[kfull + str(r0)])
                    keys.append(kfull + str(r0))
                wfull[tag] = (full, keys)

        def ada_phase():
            with ExitStack() as ph:
                cTt = sb(ph, "cTt", [128, 16, R], F32)
                sil = sb(ph, "sil", [128, 16, R], F32)
                S.dma("sp", cTt.t[:], cT.ap().rearrange("(k p) r -> p k r", p=128), writes=[cTt.k])
                S.op("act", lambda e: e.activation(out=sil.t[:], in_=cTt.t[:], func=AF.Silu), reads=[cTt.k], writes=[sil.k])
                wts = [sb(ph, "adaw%d" % i, [128, 16, 512], F32) for i in range(2)]
                outb = [sb(ph, "adao%d" % i, [R, 512], F32) for i in range(2)]
                n = 0
                for i in range(NADA):
                    for cb in range(24):
                        w = wts[n % 2]
                        ob = outb[n % 2]
                        pf = PF[n % 4]
                        n += 1
                        S.dma("sp", w.t[:], ada_w[i].rearrange("(k p) n -> p k n", p=128)[:, :, cb * 512:(cb + 1) * 512], writes=[w.k])
                        for kc in range(16):
                            S.op("pe", lambda e, pf=pf, w=w, kc=kc: e.matmul(pf.t[0:R, :], lhsT=sil.t[:, kc, :], rhs=w.t[:, kc, :],
                                                                             start=(kc == 0), stop=(kc == 15)), reads=[sil.k, w.k], writes=[pf.k])
                        S.op("dve", lambda e, pf=pf, ob=ob: e.tensor_copy(out=ob.t[:], in_=pf.t[0:R, :]), reads=[pf.k], writes=[ob.k])
                        S.dma("act", ada_full[i * R:(i + 1) * R, cb * 512:(cb + 1) * 512], ob.t[:], reads=[ob.k], writes=[])
                if ADA_SPLIT:
                    S.barrier(pool=True)
                    S.collective("AllGather", ALU.bypass, PAIRS, ada_full.ap().opt(), ada_all.ap().opt(), writes=["ada_all"])
                else:
                    S.barrier()

        def mod_tmps(ph):
            return (sb(ph, "modrow", [R, 2048], F32), sb(ph, "modb", [R, 2048], F32), sb(ph, "modg", [128, 2048], F32))

        def mod_tile(ph, tmps, i, which, vec, kind, gidx=None):
            out = sb(ph, "mod", [128, 2048], F32)
            row, brow, gt = tmps
            if ADA_SPLIT:
                r0 = (i % 2) * NADA * R + (i // 2) * R
                S.dma("sp", row.t[:], ada_all[r0:r0 + R, vec * 2048:(vec + 1) * 2048], reads=["ada_all"], writes=[row.k])
            else:
                S.dma("sp", row.t[:], ada_full[i * R:(i + 1) * R, vec * 2048:(vec + 1) * 2048], reads=[], writes=[row.k])
            S.dma("sp", brow.t[:], ada_b[i, vec * 2048:(vec + 1) * 2048].partition_broadcast(R), writes=[brow.k])
            S.op("dve", lambda e: e.tensor_tensor(out=row.t[:], in0=row.t[:], in1=brow.t[:], op=ALU.add), reads=[row.k, brow.k], writes=[row.k])
            if kind == "scale":
                S.dma("sp", gt.t[:], norm_g[i, gidx, :].partition_broadcast(128), writes=[gt.k])
            for q in range(4):
                pf = PF[q]
                S.op("pe", lambda e, pf=pf, q=q: e.matmul(pf.t[:], lhsT=selt.t[:, which * 128:(which + 1) * 128],
                                                          rhs=row.t[:, q * 512:(q + 1) * 512], start=True, stop=True),
                     reads=[selt.k, row.k], writes=[pf.k])
                if kind == "scale":
                    S.op("dve", lambda e, pf=pf, q=q: e.scalar_tensor_tensor(
                        out=out.t[:, q * 512:(q + 1) * 512], in0=pf.t[:], scalar=1.0, in1=gt.t[:, q * 512:(q + 1) * 512],
                        op0=ALU.add, op1=ALU.mult), reads=[pf.k, gt.k], writes=[out.k])
                else:
                    S.op("dve", lambda e, pf=pf, q=q: e.tensor_copy(out=out.t[:, q * 512:(q + 1) * 512], in_=pf.t[:]),
                         reads=[pf.k], writes=[out.k])
            return out

        def norm_phase(ph, XT, i, which_norm, src):
            vs, vsh = (1, 0) if which_norm == 0 else (4, 3)
            tmps = mod_tmps(ph)
            A = [mod_tile(ph, tmps, i, w, vs, "scale", which_norm) for w in range(2)]
            Sh = [mod_tile(ph, tmps, i, w, vsh, "raw") for w in range(2)]
            xt = [sb(ph, "nx", [128, 2048], F32) for _ in range(2)]
            junk = sb(ph, "njunk", [128, 2048], BF16)
            tmp = sb(ph, "ntmp", [128, 2048], F32)
            hb = [sb(ph, "nhb", [128, 2048], BF16) for _ in range(2)]
            ssq = [sb(ph, "nss", [128, 1], F32) for _ in range(2)]
            for j in range(NT):
                w = 0 if j < NTL else 1
                x = xt[j % 2]
                h = hb[j % 2]
                ss = ssq[j % 2]
                S.dma("sp", x.t[:], src[j * 128:(j + 1) * 128, :], writes=[x.k])
                S.op("act", lambda e, x=x, ss=ss: e.activation(out=junk.t[:], in_=x.t[:], func=AF.Square, accum_out=ss.t[:]),
                     reads=[x.k], writes=[junk.k, ss.k])
                S.op("act", lambda e, ss=ss: e.activation(out=ss.t[:], in_=ss.t[:], func=AF.Sqrt, scale=1.0 / D, bias=EPS),
                     reads=[ss.k], writes=[ss.k])
                S.op("dve", lambda e, ss=ss: e.reciprocal(out=ss.t[:], in_=ss.t[:]),
                     reads=[ss.k], writes=[ss.k])
                S.op("dve", lambda e, x=x, ss=ss, w=w: e.scalar_tensor_tensor(out=tmp.t[:], in0=x.t[:], scalar=ss.t[:, 0:1], in1=A[w].t[:],
                                                                               op0=ALU.mult, op1=ALU.mult),
                     reads=[x.k, ss.k, A[w].k], writes=[tmp.k])
                S.op("dve", lambda e, h=h, w=w: e.tensor_tensor(out=h.t[:], in0=tmp.t[:], in1=Sh[w].t[:], op=ALU.add),
                     reads=[tmp.k, Sh[w].k], writes=[h.k])
                transpose_into(XT, h, j)

        def transpose_into(XT, h, j, nchunks=16, kc0=0):
            for half in range(0, nchunks, 8):
                n = min(8, nchunks - half)
                pb = PB[(half // 8) % 2]
                for c in range(n):
                    S.op("pe", lambda e, pb=pb, c=c, half=half: e.transpose(pb.t[:, c * 128:(c + 1) * 128], h.t[:, (half + c) * 128:(half + c + 1) * 128], ident),
                         reads=[h.k] + CK, writes=[pb.k])
                S.op("act", lambda e, pb=pb, n=n, half=half: e.copy(
                    out=XT.t[:, kc0 + half:kc0 + half + n, j * 128:(j + 1) * 128],
                    in_=pb.t[:, 0:n * 128].rearrange("p (c t) -> p c t", c=n)),
                    reads=[pb.k], writes=[XT.k + "_%d" % j])

        def lin_tm(ph, XT, wtag, col_blocks, epilogue, kchunks=16, tiles=None, bw=512):
            full, wkeys = wfull[wtag]
            slabs = [sb(ph, "slab", [128, kchunks, bw], BF16) for _ in range(2)]
            wv = full.ap().rearrange("(k p) n -> p k n", p=128)
            n = 0
            for cb in col_blocks:
                sl = slabs[n % 2]
                n += 1
                S.dma("sp", sl.t[:], wv[:, :, cb * bw:(cb + 1) * bw], reads=wkeys, writes=[sl.k])
                for j in (tiles if tiles is not None else range(NT)):
                    pf = PF[(j + n) % 4]
                    if DBGF[0] >= 2:
                        continue
                    for kc in range(kchunks):
                        S.op("pe", lambda e, pf=pf, sl=sl, kc=kc, j=j: e.matmul(
                            pf.t[:, 0:bw], lhsT=XT.t[:, kc, j * 128:(j + 1) * 128], rhs=sl.t[:, kc, :],
                            start=(kc == 0), stop=(kc == kchunks - 1)),
                            reads=[XT.k + "_%d" % j, sl.k], writes=[pf.k])
                    if DBGF[0] >= 1:
                        continue
                    epilogue(j, cb, pf)

        def make_resid_epilogue(ph, V, i, gvec, src, dst, last=False):
            tmps = mod_tmps(ph)
            G = [mod_tile(ph, tmps, i, w, gvec, "raw") for w in range(2)]
            xp = [sb(ph, "rx", [128, 512], F32) for _ in range(3)]
            cnt = [0]

            def epi(j, cb, pf):
                w = 0 if j < NTL else 1
                x = xp[cnt[0] % 3]
                cnt[0] += 1
                key = "xres_%d_%d" % (j, cb)
                S.dma("sp", x.t[:], src[j * 128:(j + 1) * 128, cb * 512:(cb + 1) * 512], reads=[key], writes=[x.k])
                S.op("dve", lambda e: e.tensor_tensor(out=pf.t[:], in0=pf.t[:], in1=G[w].t[:, cb * 512:(cb + 1) * 512], op=ALU.mult),
                     reads=[pf.k, G[w].k], writes=[pf.k])
                S.op("dve", lambda e: e.tensor_tensor(out=x.t[:], in0=pf.t[:], in1=x.t[:], op=ALU.add),
                     reads=[pf.k, x.k], writes=[x.k])
                if last:
                    if j < NTL:
                        S.dma("act", V.y[j * 128:(j + 1) * 128, cb * 512:(cb + 1) * 512], x.t[:], reads=[x.k], writes=[key + "y"])
                else:
                    S.dma("act", dst[j * 128:(j + 1) * 128, cb * 512:(cb + 1) * 512], x.t[:], reads=[x.k], writes=[key])
            return epi

        def ffn_gu(ph, XT, i):
            full, wkeys = wfull["wgu%d" % i]
            wv = full.ap().rearrange("(k p) n -> p k n", p=128)
            slg = [sb(ph, "slg", [128, 16, 512], BF16) for _ in range(2)]
            slu = [sb(ph, "slu", [128, 16, 512], BF16) for _ in range(2)]
            sg = [sb(ph, "sg", [128, 512], F32) for _ in range(2)]
            ao = [sb(ph, "ao", [128, 512], BF16) for _ in range(3)]
            tblocks = [(t0, min(512, T - t0)) for t0 in range(0, T, 512)]
            n = 0
            m = 0
            for cb in range(FF // 512):
                g = slg[n % 2]
                u = slu[n % 2]
                n += 1
                S.dma("sp", g.t[:], wv[:, :, cb * 512:(cb + 1) * 512], reads=wkeys, writes=[g.k])
                S.dma("sp", u.t[:], wv[:, :, FF + cb * 512:FF + (cb + 1) * 512], reads=wkeys, writes=[u.k])
                for (t0, tw) in tblocks:
                    tkeys = [XT.k + "_%d" % j for j in range(t0 // 128, (t0 + tw) // 128)]
                    for fc in range(4):
                        pg = PF[(2 * m) % 6]
                        pu = PF[(2 * m + 1) % 6]
                        s_ = sg[m % 2]
                        a = ao[m % 3]
                        m += 1
                        for (pp, sl) in ((pg, g), (pu, u)):
                            for kc in range(16):
                                S.op("pe", lambda e, pp=pp, sl=sl, kc=kc: e.matmul(
                                    pp.t[:, 0:tw], lhsT=sl.t[:, kc, fc * 128:(fc + 1) * 128], rhs=XT.t[:, kc, t0:t0 + tw],
                                    start=(kc == 0), stop=(kc == 15)), reads=[sl.k] + tkeys, writes=[pp.k])
                        S.op("act", lambda e: e.activation(out=s_.t[:, 0:tw], in_=pg.t[:, 0:tw], func=AF.Silu), reads=[pg.k], writes=[s_.k])
                        S.op("dve", lambda e: e.tensor_tensor(out=a.t[:, 0:tw], in0=s_.t[:, 0:tw], in1=pu.t[:, 0:tw], op=ALU.mult),
                             reads=[s_.k, pu.k], writes=[a.k])
                        f0 = cb * 512 + fc * 128
                        S.dma("act", actT[f0:f0 + 128, t0:t0 + tw], a.t[:, 0:tw], reads=[a.k], writes=[])

        def ffn_down(ph, V, i, src, last):
            full, wkeys = wfull["wdn%d" % i]
            wv = full.ap().rearrange("(k p) n -> p k n", p=128)
            KC = FF // 128
            slabs = [sb(ph, "dslab", [128, KC, 512], BF16) for _ in range(2)]
            at = [sb(ph, "dact", [128, KC, 128], BF16) for _ in range(2)]
            epi = make_resid_epilogue(ph, V, i, 5, src, V.xres, last)
            av_ = actT.ap().rearrange("(k p) t -> p k t", p=128)
            n = 0
            m = 0
            for cb in range(4):
                sl = slabs[n % 2]
                n += 1
                S.dma("sp", sl.t[:], wv[:, :, cb * 512:(cb + 1) * 512], reads=wkeys, writes=[sl.k])
                for j in range(NT):
                    if last and j >= NTL:
                        continue
                    a = at[m % 2]
                    pf = PF[m % 4]
                    m += 1
                    S.dma("sp", a.t[:], av_[:, :, j * 128:(j + 1) * 128], reads=[], writes=[a.k])
                    for kc in range(KC):
                        S.op("pe", lambda e, kc=kc: e.matmul(pf.t[:], lhsT=a.t[:, kc, :], rhs=sl.t[:, kc, :], start=(kc == 0), stop=(kc == KC - 1)),
                             reads=[a.k, sl.k], writes=[pf.k])
                    epi(j, cb, pf)

        def mlstm_pre(V, i, src):
            j_ = i // 2
            with ExitStack() as ph:
                XT = sb(ph, "XT", [128, 16, T], BF16)
                with ExitStack() as ph1:
                    norm_phase(ph1, XT, i, 0, src)
                    S.barrier()
                if stop(2):
                    return
                GF = V.GF
                mq, mk, mv, mo = V.mq, V.mk, V.mv, V.mo
                with ExitStack() as ph2:
                    wgf = sb(ph2, "wgf", [128, 16, 32], F32)
                    wgb = sb(ph2, "wgb", [128, 16, 32], BF16)
                    gbt = sb(ph2, "gbt", [128, 32], F32)
                    S.dma("sp", wgf.t[:], V.m_wg[j_].rearrange("(k p) n -> p k n", p=128), writes=[wgf.k])
                    S.dma("sp", gbt.t[:], V.m_gb[j_, :].partition_broadcast(128), writes=[gbt.k])
                    S.op("dve", lambda e: e.tensor_copy(out=wgb.t[:], in_=wgf.t[:]), reads=[wgf.k], writes=[wgb.k])
                    gr = sb(ph2, "gr", [128, 32], F32)
                    ex = sb(ph2, "gex", [128, 16], F32)
                    cum = sb(ph2, "cum", [128, 32], F32)
                    for j in range(NT):
                        pf = PF[j % 2]
                        for kc in range(16):
                            S.op("pe", lambda e, kc=kc: e.matmul(pf.t[:, 0:32], lhsT=XT.t[:, kc, j * 128:(j + 1) * 128], rhs=wgb.t[:, kc, :],
                                                                  start=(kc == 0), stop=(kc == 15)),
                                 reads=[XT.k + "_%d" % j, wgb.k], writes=[pf.k])
                        S.op("dve", lambda e: e.tensor_tensor(out=gr.t[:], in0=pf.t[:, 0:32], in1=gbt.t[:], op=ALU.add), reads=[pf.k, gbt.k], writes=[gr.k])
                        S.op("act", lambda e: e.activation(out=gr.t[:], in_=gr.t[:], func=AF.Tanh, scale=1.0 / GATE_CAP), reads=[gr.k], writes=[gr.k])
                        S.op("dve", lambda e: e.tensor_single_scalar(out=gr.t[:], in_=gr.t[:], scalar=GATE_CAP, op=ALU.mult), reads=[gr.k], writes=[gr.k])
                        fv = gr.t[:, :].rearrange("p (a b) -> p a b", a=2)[:, :, 8:16]
                        exv = ex.t[:, :].rearrange("p (a b) -> p a b", a=2)
                        S.op("act", lambda e: e.activation(out=exv, in_=fv, func=AF.Exp, scale=-1.0), reads=[gr.k], writes=[ex.k])
                        S.op("act", lambda e: e.activation(out=exv, in_=exv, func=AF.Ln, bias=1.0), reads=[ex.k], writes=[ex.k])
                        S.op("dve", lambda e: e.tensor_single_scalar(out=fv, in_=exv, scalar=-1.0, op=ALU.mult), reads=[ex.k], writes=[gr.k])
                        pc = PF[2 + j % 2]
                        S.op("pe", lambda e: e.matmul(pc.t[:, 0:8], lhsT=triU_f, rhs=gr.t[:, 8:16], start=True, stop=True), reads=[gr.k] + CK, writes=[pc.k])
                        S.op("pe", lambda e: e.matmul(pc.t[:, 8:16], lhsT=triL_f, rhs=gr.t[:, 24:32], start=True, stop=True), reads=[gr.k] + CK, writes=[pc.k])
                        S.op("pe", lambda e: e.matmul(pc.t[:, 16:24], lhsT=ones_f, rhs=gr.t[:, 8:16], start=True, stop=True), reads=[gr.k] + CK, writes=[pc.k])
                        S.op("pe", lambda e: e.matmul(pc.t[:, 24:32], lhsT=ones_f, rhs=gr.t[:, 24:32], start=True, stop=True), reads=[gr.k] + CK, writes=[pc.k])
                        S.op("dve", lambda e: e.tensor_copy(out=cum.t[:], in_=pc.t[:, 0:32]), reads=[pc.k], writes=[cum.k])
                        g = GF.t[:, j, :]
                        gk = GF.k + "_%d" % j
                        S.op("act", lambda e: e.activation(out=g[:, 0:8], in_=cum.t[:, 0:8], func=AF.Exp), reads=[cum.k], writes=[gk])
                        S.op("act", lambda e: e.activation(out=g[:, 16:24], in_=cum.t[:, 8:16], func=AF.Exp), reads=[cum.k], writes=[gk])
                        S.op("act", lambda e: e.activation(out=g[:, 32:48], in_=cum.t[:, 16:32], func=AF.Exp), reads=[cum.k], writes=[gk])
                        S.op("dve", lambda e: e.tensor_single_scalar(out=g[:, 0:8], in_=g[:, 0:8], scalar=128.0 ** -0.5, op=ALU.mult), reads=[gk], writes=[gk])
                        S.op("dve", lambda e: e.tensor_single_scalar(out=g[:, 16:24], in_=g[:, 16:24], scalar=128.0 ** -0.5, op=ALU.mult), reads=[gk], writes=[gk])
                        S.op("dve", lambda e: e.tensor_tensor(out=cum.t[:, 0:8], in0=gr.t[:, 0:8], in1=cum.t[:, 0:8], op=ALU.subtract), reads=[cum.k, gr.k], writes=[cum.k])
                        S.op("dve", lambda e: e.tensor_tensor(out=cum.t[:, 8:16], in0=gr.t[:, 16:24], in1=cum.t[:, 8:16], op=ALU.subtract), reads=[cum.k, gr.k], writes=[cum.k])
                        S.op("act", lambda e: e.activation(out=g[:, 8:16], in_=cum.t[:, 0:8], func=AF.Exp), reads=[cum.k], writes=[gk])
                        S.op("act", lambda e: e.activation(out=g[:, 24:32], in_=cum.t[:, 8:16], func=AF.Exp), reads=[cum.k], writes=[gk])
                    S.barrier()
                if stop(3):
                    return
                with ExitStack() as ph3:
                    ob = [sb(ph3, "pob", [128, 2, 512], BF16) for _ in range(3)]
                    cnt = [0]

                    def epi(j, cb, pf):
                        o = ob[cnt[0] % 3]
                        cnt[0] += 1
                        gk = GF.k + "_%d" % j
                        if cb < 4:
                            isk = cb >= 2
                            h0 = (cb % 2) * 4
                            for sty in range(2):
                                c0 = sty * 16 + (8 if isk else 0) + h0
                                S.op("dve", lambda e, sty=sty, c0=c0: e.tensor_tensor(
                                    out=o.t[:, sty, :].rearrange("p (h d) -> p h d", h=4),
                                    in0=pf.t[:, :].rearrange("p (h d) -> p h d", h=4),
                                    in1=GF.t[:, j, c0:c0 + 4].unsqueeze(2).broadcast_to([128, 4, 128]), op=ALU.mult),
                                    reads=[pf.k, gk], writes=[o.k])
                                dst = (mk if isk else mq)[sty]
                                S.dma("act", dst[j * 128:(j + 1) * 128, (cb % 2) * 512:(cb % 2 + 1) * 512], o.t[:, sty, :], reads=[o.k], writes=[])
                        elif cb < 8:
                            S.op("act", lambda e: e.copy(out=o.t[:, 0, :], in_=pf.t[:]), reads=[pf.k], writes=[o.k])
                            S.dma("act", mv[j * 128:(j + 1) * 128, (cb - 4) * 512:(cb - 3) * 512], o.t[:, 0, :], reads=[o.k], writes=[])
                        else:
                            S.op("act", lambda e: e.activation(out=o.t[:, 0, :], in_=pf.t[:], func=AF.Sigmoid), reads=[pf.k], writes=[o.k])
                            S.dma("act", mo[j * 128:(j + 1) * 128, (cb - 8) * 512:(cb - 7) * 512], o.t[:, 0, :], reads=[o.k], writes=[])
                    lin_tm(ph3, XT, "win%d" % i, range(12), epi)
                    S.barrier()

        def mlstm_stage3(V, W, i):
            with ExitStack() as outer:
                XT = sb(outer, "XT", [128, 16, T], BF16)
                mlstm_scan(V, W, i, 3, XT)
                with ExitStack() as ph5:
                    epi = make_resid_epilogue(ph5, V, i, 2, (V.xin if i == 0 else V.xres), V.xres)
                    lin_tm(ph5, XT, "wout%d" % i, range(4), epi)
                    S.barrier()

        def mlstm_scan(V, W, i, stage, XT=None):
            j_ = i // 2
            GF = V.GF
            mq, mk, mv, mo, mhA = V.mq, V.mk, V.mv, V.mo, V.mhA
            with ExitStack() as ps_:
                Cf = sb(ps_, "Cf", [128, 8, 258], F32)
                Cb = sb(ps_, "Cb", [128, 8, 257], BF16)
                qt = [sb(ps_, "sq", [128, 1024], BF16) for _ in range(2)]
                kt = [sb(ps_, "sk", [128, 1024], BF16) for _ in range(2)]
                va = [sb(ps_, "sva", [128, 8, 257], BF16) for _ in range(2)]
                qkT = [sb(ps_, "qkT", [128, 4, 128], BF16) for _ in range(2)]
                stm = [sb(ps_, "stm", [128, 2, 128], BF16) for _ in range(2)]
                den = sb(ps_, "den", [128, 2], F32)
                hacc = sb(ps_, "hacc", [128, 2048], F32)
                hprev = sb(ps_, "hprev", [128, 2048], F32)
                tmpc = sb(ps_, "tmpc", [128, 2, 257], F32)
                stg = sb(ps_, "stg", [128, 8, 258], F32)
                X1 = sb(ps_, "X1", [128, 8, 258], F32)
                if stage == 3:
                    hg = sb(ps_, "hg", [128, 2048], F32)
                    og = sb(ps_, "og", [128, 2048], BF16)
                    hsq = sb(ps_, "hsq", [128, 2048], F32)
                    hss = sb(ps_, "hss", [128, 8], F32)
                    hb16 = sb(ps_, "hb16", [128, 2048], BF16)
                    S.dma("sp", hg.t[:], m_hg[j_, :].partition_broadcast(128), writes=[hg.k])
                for v_ in va:
                    S.op("dve", lambda e, v_=v_: e.memset(v_.t[:, :, 256:257], 1.0), writes=[v_.k])
                maskA = cstb.t[:, 128:256]
                maskB = cstb.t[:, 256:384]
                ntile = [0]

                def refresh_cb():
                    S.op("act", lambda e: e.copy(out=Cb.t[:], in_=Cf.t[:, :, 0:257]), reads=[Cf.k], writes=[Cb.k])

                def load_state(src_dram):
                    S.dma("sp", Cf.t[:, :, :].rearrange("p h w -> p (h w)"), src_dram[:, :], writes=[Cf.k])
                    refresh_cb()

                def save_state(dst_dram, buf):
                    S.dma("act", dst_dram[:, :], buf.t[:, :, :].rearrange("p h w -> p (h w)"), reads=[buf.k], writes=[])

                def chunk(j, sty, out_final):
                    n = ntile[0]
                    ntile[0] += 1
                    q, k, v = qt[n % 2], kt[n % 2], va[n % 2]
                    S.dma("sp", q.t[:], mq[sty][j * 128:(j + 1) * 128, :], reads=[], writes=[q.k])
                    S.dma("sp", k.t[:], mk[sty][j * 128:(j + 1) * 128, :], reads=[], writes=[k.k])
                    S.dma("sp", v.t[:, :, 0:256], mv[j * 128:(j + 1) * 128, :].rearrange("p (h d) -> p h d", h=8), reads=[], writes=[v.k])
                    if out_final:
                        S.dma("sp", hprev.t[:], mhA[j * 128:(j + 1) * 128, :], reads=["mhA_%d" % j], writes=[hprev.k])
                        S.dma("sp", og.t[:], mo[j * 128:(j + 1) * 128, :], reads=[], writes=[og.k])
                    mask = maskA if sty == 0 else maskB
                    dcol = 32 + sty * 8
                    gk = GF.k + "_%d" % j
                    for hp in range(4):
                        qk_ = qkT[hp % 2]
                        sm = stm[hp % 2]
                        pb = PB[hp % 2]
                        for c, (srcb, col) in enumerate(((q, 2 * hp), (q, 2 * hp + 1), (k, 2 * hp), (k, 2 * hp + 1))):
                            S.op("pe", lambda e, c=c, srcb=srcb, col=col: e.transpose(pb.t[:, c * 128:(c + 1) * 128], srcb.t[:, col * 128:(col + 1) * 128], ident),
                                 reads=[srcb.k] + CK, writes=[pb.k])
                        S.op("act", lambda e: e.copy(out=qk_.t[:], in_=pb.t[:, 0:512].rearrange("p (c t) -> p c t", c=4)), reads=[pb.k], writes=[qk_.k])
                        pst = PF[0]
                        for hh in range(2):
                            S.op("pe", lambda e, hh=hh: e.matmul(pst.t[:, hh * 128:(hh + 1) * 128], lhsT=qk_.t[:, 2 + hh, :], rhs=qk_.t[:, hh, :], start=True, stop=True),
                                 reads=[qk_.k], writes=[pst.k])
                        S.op("dve", lambda e: e.tensor_tensor(out=sm.t[:], in0=pst.t[:, 0:256].rearrange("p (h t) -> p h t", h=2),
                                                              in1=mask.unsqueeze(1).broadcast_to([128, 2, 128]), op=ALU.mult),
                             reads=[pst.k] + CK, writes=[sm.k])
                        for hh in range(2):
                            h = 2 * hp + hh
                            pn = PF[1 + hh]
                            S.op("pe", lambda e, hh=hh, h=h, pn=pn: e.matmul(pn.t[:, 0:257], lhsT=sm.t[:, hh, :], rhs=v.t[:, h, :], start=True, stop=False),
                                 reads=[sm.k, v.k], writes=[pn.k])
                            S.op("pe", lambda e, hh=hh, h=h, pn=pn: e.matmul(pn.t[:, 0:257], lhsT=qk_.t[:, hh, :], rhs=Cb.t[:, h, :], start=False, stop=True),
                                 reads=[qk_.k, Cb.k], writes=[pn.k])
                            pd = PF[3 + hh]
                            S.op("pe", lambda e, h=h, pd=pd: e.matmul(pd.t[:, 0:257], lhsT=k.t[:, h * 128:(h + 1) * 128], rhs=v.t[:, h, :], start=True, stop=True),
                                 reads=[k.k, v.k], writes=[pd.k])
                            S.op("act", lambda e, hh=hh, pn=pn: e.activation(out=den.t[:, hh:hh + 1], in_=pn.t[:, 256:257], func=AF.Abs), reads=[pn.k], writes=[den.k])
                            S.op("dve", lambda e, hh=hh: e.tensor_single_scalar(out=den.t[:, hh:hh + 1], in_=den.t[:, hh:hh + 1], scalar=1.0, op=ALU.max), reads=[den.k], writes=[den.k])
                            S.op("dve", lambda e, hh=hh: e.reciprocal(out=den.t[:, hh:hh + 1], in_=den.t[:, hh:hh + 1]), reads=[den.k], writes=[den.k])
                            if out_final:
                                S.op("dve", lambda e, hh=hh, h=h, pn=pn: e.scalar_tensor_tensor(
                                    out=hacc.t[:, h * 256:(h + 1) * 256], in0=pn.t[:, 0:256], scalar=den.t[:, hh:hh + 1], in1=hprev.t[:, h * 256:(h + 1) * 256],
                                    op0=ALU.mult, op1=ALU.add), reads=[pn.k, den.k, hprev.k], writes=[hacc.k])
                            else:
                                S.op("dve", lambda e, hh=hh, h=h, pn=pn: e.tensor_scalar(
                                    out=hacc.t[:, h * 256:(h + 1) * 256], in0=pn.t[:, 0:256], scalar1=den.t[:, hh:hh + 1], scalar2=None, op0=ALU.mult),
                                    reads=[pn.k, den.k], writes=[hacc.k])
                            S.op("dve", lambda e, hh=hh, h=h, pd=pd: e.tensor_tensor(out=tmpc.t[:, hh, :], in0=pd.t[:, 0:257], in1=Cf.t[:, h, 0:257], op=ALU.add),
                                 reads=[pd.k, Cf.k], writes=[tmpc.k])
                            S.op("dve", lambda e, hh=hh, h=h: e.tensor_scalar(out=Cf.t[:, h, 0:257], in0=tmpc.t[:, hh, :], scalar1=GF.t[:, j, dcol + h:dcol + h + 1],
                                                                          scalar2=None, op0=ALU.mult), reads=[tmpc.k, gk, Cb.k], writes=[Cf.k])
                    refresh_cb()
                    if out_final:
                        finalize(j)
                    else:
                        S.dma("act", mhA[j * 128:(j + 1) * 128, :], hacc.t[:], reads=[hacc.k], writes=["mhA_%d" % j])

                def finalize(j):
                    S.op("act", lambda e: e.activation(out=hsq.t[:], in_=hacc.t[:], func=AF.Square), reads=[hacc.k], writes=[hsq.k])
                    S.op("dve", lambda e: e.tensor_reduce(out=hss.t[:], in_=hsq.t[:, :].rearrange("p (h d) -> p h d", h=8), axis=AX.X, op=ALU.add),
                         reads=[hsq.k], writes=[hss.k])
                    S.op("act", lambda e: e.activation(out=hss.t[:], in_=hss.t[:], func=AF.Sqrt, scale=1.0 / 256, bias=EPS), reads=[hss.k], writes=[hss.k])
                    S.op("dve", lambda e: e.reciprocal(out=hss.t[:], in_=hss.t[:]), reads=[hss.k], writes=[hss.k])
                    S.op("dve", lambda e: e.tensor_tensor(out=hsq.t[:, :].rearrange("p (h d) -> p h d", h=8), in0=hacc.t[:, :].rearrange("p (h d) -> p h d", h=8),
                                                          in1=hss.t[:, :].unsqueeze(2).broadcast_to([128, 8, 256]), op=ALU.mult),
                         reads=[hacc.k, hss.k], writes=[hsq.k])
                    S.op("dve", lambda e: e.tensor_tensor(out=hsq.t[:], in0=hsq.t[:], in1=hg.t[:], op=ALU.mult), reads=[hsq.k, hg.k], writes=[hsq.k])
                    S.op("dve", lambda e: e.tensor_tensor(out=hb16.t[:], in0=hsq.t[:], in1=og.t[:], op=ALU.mult), reads=[hsq.k, og.k], writes=[hb16.k])
                    transpose_into(XT, hb16, j)


                jc = NTL
                if stage == 1:
                    S.op("dve", lambda e: e.memset(Cf.t[:], 0.0), writes=[Cf.k])
                    S.op("dve", lambda e: e.memset(Cb.t[:], 0.0), writes=[Cb.k])
                    chunk(jc, 0, False)
                    save_state(V.st[0], Cf)
                    n = ntile[0]
                    ntile[0] += 1
                    kB, vB = kt[n % 2], va[n % 2]
                    S.dma("sp", kB.t[:], mk[1][jc * 128:(jc + 1) * 128, :], writes=[kB.k])
                    S.dma("sp", vB.t[:, :, 0:256], mv[jc * 128:(jc + 1) * 128, :].rearrange("p (h d) -> p h d", h=8), writes=[vB.k])
                    for h in range(8):
                        pd = PF[3 + h % 2]
                        S.op("pe", lambda e, h=h, pd=pd: e.matmul(pd.t[:, 0:257], lhsT=kB.t[:, h * 128:(h + 1) * 128], rhs=vB.t[:, h, :], start=True, stop=True),
                             reads=[kB.k, vB.k], writes=[pd.k])
                        S.op("dve", lambda e, h=h, pd=pd: e.tensor_copy(out=stg.t[:, h, 0:257], in_=pd.t[:, 0:257]), reads=[pd.k], writes=[stg.k])
                    S.op("dve", lambda e: e.tensor_copy(out=stg.t[:, :, 257:258], in_=GF.t[:, jc, 40:48].unsqueeze(2)), reads=[GF.k + "_%d" % jc], writes=[stg.k])
                    save_state(V.st[1], stg)
                elif stage == 2:
                    S.dma("sp", Cf.t[:, :, :].rearrange("p h w -> p (h w)"), V.st[0][:, :], writes=[Cf.k])
                    S.dma("sp", X1.t[:, :, :].rearrange("p h w -> p (h w)"), W.st[1][:, :], writes=[X1.k])
                    S.op("dve", lambda e: e.tensor_tensor(out=Cf.t[:, :, 0:257], in0=Cf.t[:, :, 0:257], in1=X1.t[:, :, 0:257], op=ALU.add), reads=[Cf.k, X1.k], writes=[Cf.k])
                    S.op("dve", lambda e: e.tensor_tensor(out=Cf.t[:, :, 0:257], in0=Cf.t[:, :, 0:257], in1=X1.t[:, :, 257:258].broadcast_to([128, 8, 257]), op=ALU.mult),
                         reads=[Cf.k, X1.k], writes=[Cf.k])
                    refresh_cb()
                    for j in range(NTL):
                        chunk(j, 0, False)
                    save_state(V.st[2], Cf)
                else:
                    load_state(W.st[0])
                    chunk(jc, 1, True)
                    load_state(W.st[2])
                    for j in range(NTL - 1, -1, -1):
                        chunk(j, 1, True)
                S.barrier()

        def attn_pre(V, i, src):
            j_ = i // 2
            aqT, akT, av = V.aqT, V.akT, V.av
            rope = V.rope
            with ExitStack() as ph:
                XT = sb(ph, "XT", [128, 16, T], BF16)
                with ExitStack() as ph1:
                    norm_phase(ph1, XT, i, 0, src)
                    S.barrier()
                with ExitStack() as ph2:
                    gq = sb(ph2, "gq", [128, 2, 128], F32)
                    S.dma("sp", gq.t[:], d_vec[j_, 0:2, :].partition_broadcast(128), writes=[gq.k])
                    S.op("dve", lambda e: e.tensor_single_scalar(out=gq.t[:, 0, :], in_=gq.t[:, 0, :], scalar=128.0 ** -0.5, op=ALU.mult), reads=[gq.k], writes=[gq.k])
                    rp = sb(ph2, "rp", [128, NT, 128], F32)
                    S.dma("sp", rp.t[:], rope.rearrange("(j p) c -> p j c", p=128), writes=[rp.k])
                    sq_l = [sb(ph2, "asq", [128, 512], F32) for _ in range(2)]
                    qss_l = [sb(ph2, "qss", [128, 4], F32) for _ in range(2)]
                    qn_l = [sb(ph2, "qn", [128, 512], F32) for _ in range(2)]
                    t1_l = [sb(ph2, "t1", [128, 512], F32) for _ in range(2)]
                    t2_l = [sb(ph2, "t2", [128, 512], F32) for _ in range(2)]
                    qo = [sb(ph2, "qo", [128, 512], BF16) for _ in range(2)]
                    qTs = [sb(ph2, "qTs", [128, 4, 128], BF16) for _ in range(2)]
                    vo = [sb(ph2, "vo", [128, 512], BF16) for _ in range(2)]
                    cnt = [0]

                    def epi(j, cb, pf):
                        n = cnt[0]
                        cnt[0] += 1
                        sq_, qss, qn, t1, t2 = sq_l[n % 2], qss_l[n % 2], qn_l[n % 2], t1_l[n % 2], t2_l[n % 2]
                        if cb < 8:
                            isk = cb >= 4
                            S.op("act", lambda e: e.activation(out=sq_.t[:], in_=pf.t[:], func=AF.Square), reads=[pf.k], writes=[sq_.k])
                            S.op("dve", lambda e: e.tensor_reduce(out=qss.t[:], in_=sq_.t[:, :].rearrange("p (h d) -> p h d", h=4), axis=AX.X, op=ALU.add),
                                 reads=[sq_.k], writes=[qss.k])
                            S.op("act", lambda e: e.activation(out=qss.t[:], in_=qss.t[:], func=AF.Sqrt, scale=1.0 / 128, bias=EPS), reads=[qss.k], writes=[qss.k])
                            S.op("dve", lambda e: e.reciprocal(out=qss.t[:], in_=qss.t[:]), reads=[qss.k], writes=[qss.k])
                            S.op("dve", lambda e: e.tensor_tensor(out=qn.t[:, :].rearrange("p (h d) -> p h d", h=4), in0=pf.t[:, :].rearrange("p (h d) -> p h d", h=4),
                                                                  in1=qss.t[:, :].unsqueeze(2).broadcast_to([128, 4, 128]), op=ALU.mult), reads=[pf.k, qss.k], writes=[qn.k])
                            S.op("dve", lambda e: e.tensor_tensor(out=qn.t[:, :].rearrange("p (h d) -> p h d", h=4), in0=qn.t[:, :].rearrange("p (h d) -> p h d", h=4),
                                                                   in1=gq.t[:, 1 if isk else 0, :].unsqueeze(1).broadcast_to([128, 4, 128]), op=ALU.mult),
                                 reads=[qn.k, gq.k], writes=[qn.k])
                            cosv = rp.t[:, j, 0:64].rearrange("p (a f) -> p a f", a=2)
                            sinv = rp.t[:, j, 64:128].rearrange("p (a f) -> p a f", a=2)
                            qv5 = qn.t[:, :].rearrange("p (h a j f) -> p h a j f", h=4, a=2, j=2)
                            t15 = t1.t[:, :].rearrange("p (h a j f) -> p h a j f", h=4, a=2, j=2)
                            t25 = t2.t[:, :].rearrange("p (h a j f) -> p h a j f", h=4, a=2, j=2)
                            for jj in range(2):
                                S.op("dve", lambda e, jj=jj: e.tensor_tensor(out=t15[:, :, :, jj, :], in0=qv5[:, :, :, jj, :],
                                                                             in1=cosv.unsqueeze(1).broadcast_to([128, 4, 2, 32]), op=ALU.mult),
                                     reads=[qn.k, rp.k], writes=[t1.k])
                                S.op("dve", lambda e, jj=jj: e.tensor_tensor(out=t25[:, :, :, jj, :], in0=qv5[:, :, :, 1 - jj, :],
                                                                              in1=sinv.unsqueeze(1).broadcast_to([128, 4, 2, 32]), op=ALU.mult),
                                     reads=[qn.k, rp.k], writes=[t2.k])
                            o = qo[n % 2]
                            o5 = o.t[:, :].rearrange("p (h a j f) -> p h a j f", h=4, a=2, j=2)
                            S.op("dve", lambda e: e.tensor_tensor(out=o5[:, :, :, 0, :], in0=t15[:, :, :, 0, :], in1=t25[:, :, :, 0, :], op=ALU.subtract),
                                 reads=[t1.k, t2.k], writes=[o.k])
                            S.op("dve", lambda e: e.tensor_tensor(out=o5[:, :, :, 1, :], in0=t15[:, :, :, 1, :], in1=t25[:, :, :, 1, :], op=ALU.add),
                                 reads=[t1.k, t2.k], writes=[o.k])
                            pb = PB[n % 2]
                            for c in range(4):
                                S.op("pe", lambda e, c=c: e.transpose(pb.t[:, c * 128:(c + 1) * 128], o.t[:, c * 128:(c + 1) * 128], ident), reads=[o.k] + CK, writes=[pb.k])
                            qT_ = qTs[n % 2]
                            S.op("act", lambda e: e.copy(out=qT_.t[:], in_=pb.t[:, 0:512].rearrange("p (c t) -> p c t", c=4)), reads=[pb.k], writes=[qT_.k])
                            dst = akT if isk else aqT
                            r0 = (cb % 4) * 512
                            if isk and PAIR:
                                for c in range(4):
                                    S.dma("act", akc_in[(cb % 4) * 4 + c][:, j * 128:(j + 1) * 128], qT_.t[:, c, :], reads=[qT_.k], writes=[])
                            else:
                                S.dma("act", dst[r0:r0 + 512, j * 128:(j + 1) * 128].rearrange("(c p) t -> p c t", p=128), qT_.t[:], reads=[qT_.k], writes=[])
                        else:
                            o = vo[n % 2]
                            S.op("act", lambda e: e.copy(out=o.t[:], in_=pf.t[:]), reads=[pf.k], writes=[o.k])
                            if PAIR:
                                S.dma("act", avc_in[j // 2][(j % 2) * 128:(j % 2 + 1) * 128, (cb - 8) * 512:(cb - 7) * 512], o.t[:], reads=[o.k], writes=[])
                            else:
                                S.dma("act", av[j * 128:(j + 1) * 128, (cb - 8) * 512:(cb - 7) * 512], o.t[:], reads=[o.k], writes=[])
                    lin_tm(ph2, XT, "win%d" % i, range(12), epi)
                    S.barrier()

        def attn_core(V, i):
            j_ = i // 2
            lam_init = 0.8 - 0.6 * math.exp(-0.3 * i)
            aqT = V.aqT
            with ExitStack() as ph:
                XT = sb(ph, "XT", [128, 16, T], BF16)
                lamt = sb(ph, "lamt", [128, 2], F32)
                sgt = sb(ph, "sgt", [128, 256], F32)
                with ExitStack() as ph2:
                    lv = sb(ph2, "lv", [128, 4, 128], F32)
                    S.dma("sp", lv.t[:], d_vec[j_, 2:6, :].partition_broadcast(128), writes=[lv.k])
                    lj = sb(ph2, "lj", [128, 2, 128], F32)
                    ls = sb(ph2, "ls", [128, 2], F32)
                    lvv = lv.t[:, :, :].rearrange("p (a b) d -> p a b d", a=2)
                    S.op("dve", lambda e: e.tensor_tensor(out=lj.t[:], in0=lvv[:, :, 0, :], in1=lvv[:, :, 1, :], op=ALU.mult), reads=[lv.k], writes=[lj.k])
                    S.op("dve", lambda e: e.tensor_reduce(out=ls.t[:], in_=lj.t[:], axis=AX.X, op=ALU.add), reads=[lj.k], writes=[ls.k])
                    S.op("act", lambda e: e.activation(out=ls.t[:], in_=ls.t[:], func=AF.Exp), reads=[ls.k], writes=[ls.k])
                    S.op("dve", lambda e: e.tensor_tensor(out=lamt.t[:, 0:1], in0=ls.t[:, 1:2], in1=ls.t[:, 0:1], op=ALU.subtract), reads=[ls.k], writes=[lamt.k])
                    S.op("dve", lambda e: e.tensor_single_scalar(out=lamt.t[:, 0:1], in_=lamt.t[:, 0:1], scalar=-lam_init, op=ALU.add), reads=[lamt.k], writes=[lamt.k])
                    S.dma("sp", sgt.t[:], d_sg[j_, :].partition_broadcast(128), writes=[sgt.k])
                    S.op("dve", lambda e: e.tensor_single_scalar(out=sgt.t[:], in_=sgt.t[:], scalar=1.0 - lam_init, op=ALU.mult), reads=[sgt.k], writes=[sgt.k])
                    S.barrier()
                with ExitStack() as ph3:
                    NKT = 2 * NT
                    kTs = [sb(ph3, "kTs", [128, 2, 2 * T], BF16) for _ in range(2)]
                    vs_ = [sb(ph3, "vs", [128, NKT, 257], BF16) for _ in range(2)]
                    qs_ = [sb(ph3, "qs", [128, 2, T], BF16) for _ in range(2)]
                    pt = [sb(ph3, "pt", [128, 2, 256], BF16) for _ in range(3)]
                    rd = sb(ph3, "rd", [128, 2], F32)
                    o1 = sb(ph3, "o1", [128, 256], F32)
                    o2 = sb(ph3, "o2", [128, 256], F32)
                    oss = sb(ph3, "oss", [128, 1], F32)
                    ob16 = sb(ph3, "ob16", [128, 256], BF16)
                    for v_ in vs_:
                        S.op("dve", lambda e, v_=v_: e.memset(v_.t[:, :, 256:257], 1.0), writes=[v_.k])
                    if PAIR:
                        kalls = valls = None
                    else:
                        kalls = [VCS[r_].akT.ap().rearrange("(s d) t -> d s t", d=128) for r_ in range(2)]
                        valls = [VCS[r_].av.ap().rearrange("(k p) c -> p k c", p=128) for r_ in range(2)]
                        kvr = []
                    qall = aqT.ap().rearrange("(s d) t -> d s t", d=128)
                    npt = [0]
                    for h in range(8):
                        kT_ = kTs[h % 2]
                        v_ = vs_[h % 2]
                        q_ = qs_[h % 2]
                        for r_ in range(2):
                            if PAIR:
                                for e_ in range(2):
                                    k = 2 * h + e_
                                    S.dma("sp", kT_.t[:, e_, r_ * T:(r_ + 1) * T], akc[k][r_ * 128:(r_ + 1) * 128, 0:T], reads=["akc%d" % k], writes=[kT_.k])
                                for k in range(NVC):
                                    nt_ = min(2, NT - 2 * k)
                                    S.dma("sp", v_.t[:, r_ * NT + 2 * k:r_ * NT + 2 * k + nt_, 0:256],
                                          avc[k][r_ * 256:r_ * 256 + nt_ * 128, h * 256:(h + 1) * 256].rearrange("(k p) c -> p k c", p=128),
                                          reads=["avc%d" % k], writes=[v_.k])
                            else:
                                S.dma("sp", kT_.t[:, :, r_ * T:(r_ + 1) * T], kalls[r_][:, 2 * h:2 * h + 2, 0:T], reads=kvr, writes=[kT_.k])
                                S.dma("sp", v_.t[:, r_ * NT:(r_ + 1) * NT, 0:256], valls[r_][:, 0:NT, h * 256:(h + 1) * 256], reads=kvr, writes=[v_.k])
                        S.dma("sp", q_.t[:], qall[:, 2 * h:2 * h + 2, :], reads=[], writes=[q_.k])
                        qblocks = []
                        t0 = 0
                        while t0 < TLAT:
                            tw = min(256, TLAT - t0)
                            qblocks.append((t0, tw, [kt_ for kt_ in range(NKT)]))
                            t0 += tw
                        qblocks.append((TLAT, 128, [NTL, NT + NTL]))
                        for (q0, qw, ktiles) in qblocks:
                            nq = qw // 128
                            def qk(ki):
                                kt_ = ktiles[ki]
                                pst = PF[4 + ki % 2]
                                for e_ in range(2):
                                    S.op("pe", lambda e, e_=e_, kt_=kt_, pst=pst: e.matmul(pst.t[:, e_ * 256:e_ * 256 + qw], lhsT=kT_.t[:, e_, kt_ * 128:(kt_ + 1) * 128],
                                                                                        rhs=q_.t[:, e_, q0:q0 + qw], start=True, stop=True),
                                         reads=[kT_.k, q_.k], writes=[pst.k])

                            def exp_pv(ki):
                                kt_ = ktiles[ki]
                                pst = PF[4 + ki % 2]
                                p_ = pt[npt[0] % 3]
                                npt[0] += 1
                                S.op("act", lambda e: e.activation(out=p_.t[:, :, 0:qw], in_=pst.t[:, :].rearrange("p (e q) -> p e q", e=2)[:, :, 0:qw], func=AF.Exp),
                                     reads=[pst.k], writes=[p_.k])
                                for e_ in range(2):
                                    for qt_ in range(nq):
                                        po = PF[e_ * 2 + qt_]
                                        S.op("pe", lambda e, e_=e_, qt_=qt_, po=po, kt_=kt_: e.matmul(
                                            po.t[:, 0:257], lhsT=p_.t[:, e_, qt_ * 128:(qt_ + 1) * 128], rhs=v_.t[:, kt_, :],
                                            start=(ki == 0), stop=(ki == len(ktiles) - 1)), reads=[p_.k, v_.k], writes=[po.k])

                            qk(0)
                            for ki in range(len(ktiles)):
                                if ki + 1 < len(ktiles):
                                    qk(ki + 1)
                                exp_pv(ki)
                            for qt_ in range(nq):
                                j = q0 // 128 + qt_
                                p0 = PF[qt_]
                                p1 = PF[2 + qt_]
                                S.op("dve", lambda e: e.reciprocal(out=rd.t[:, 0:1], in_=p0.t[:, 256:257]), reads=[p0.k], writes=[rd.k])
                                S.op("dve", lambda e: e.reciprocal(out=rd.t[:, 1:2], in_=p1.t[:, 256:257]), reads=[p1.k], writes=[rd.k])
                                S.op("dve", lambda e: e.tensor_tensor(out=rd.t[:, 1:2], in0=rd.t[:, 1:2], in1=lamt.t[:, 0:1], op=ALU.mult), reads=[rd.k, lamt.k], writes=[rd.k])
                                S.op("dve", lambda e: e.tensor_scalar(out=o1.t[:], in0=p1.t[:, 0:256], scalar1=rd.t[:, 1:2], scalar2=None, op0=ALU.mult), reads=[p1.k, rd.k], writes=[o1.k])
                                S.op("dve", lambda e: e.scalar_tensor_tensor(out=o1.t[:], in0=p0.t[:, 0:256], scalar=rd.t[:, 0:1], in1=o1.t[:], op0=ALU.mult, op1=ALU.add),
                                     reads=[p0.k, rd.k, o1.k], writes=[o1.k])
                                S.op("act", lambda e: e.activation(out=o2.t[:], in_=o1.t[:], func=AF.Square, accum_out=oss.t[:]), reads=[o1.k], writes=[o2.k, oss.k])
                                S.op("act", lambda e: e.activation(out=oss.t[:], in_=oss.t[:], func=AF.Sqrt, scale=1.0 / 256, bias=EPS), reads=[oss.k], writes=[oss.k])
                                S.op("dve", lambda e: e.reciprocal(out=oss.t[:], in_=oss.t[:]), reads=[oss.k], writes=[oss.k])
                                S.op("dve", lambda e: e.scalar_tensor_tensor(out=ob16.t[:], in0=o1.t[:], scalar=oss.t[:, 0:1], in1=sgt.t[:], op0=ALU.mult, op1=ALU.mult),
                                     reads=[o1.k, oss.k, sgt.k], writes=[ob16.k])
                                transpose_into(XT, ob16, j, nchunks=2, kc0=2 * h)
                    S.barrier()
                with ExitStack() as ph5:
                    epi = make_resid_epilogue(ph5, V, i, 2, V.xres, V.xres)
                    lin_tm(ph5, XT, "wout%d" % i, range(4), epi)
                    S.barrier()

        def ffn_layer(V, i, last):
            with ExitStack() as ph:
                XT = sb(ph, "XT", [128, 16, T], BF16)
                with ExitStack() as ph1:
                    norm_phase(ph1, XT, i, 1, V.xres)
                    S.barrier()
                with ExitStack() as ph2:
                    ffn_gu(ph2, XT, i)
                    S.barrier()
            with ExitStack() as ph3:
                ffn_down(ph3, V, i, V.xres, last)
                S.barrier()

        def pair_exchange(slot, parts):
            for p_, (src_, dst_) in enumerate(parts):
                S.dma("sp", ex_in[slot][p_][:, 0:EXW], src_[:, :])
            S.barrier(pool=True)
            for p_ in range(len(parts)):
                S.collective("AllGather", ALU.bypass, PAIRS, ex_in[slot][p_].ap().opt(), ex_out[slot][p_].ap().opt(), writes=["exout%d_%d" % (slot, p_)])
            npart = len(parts)
            with ExitStack() as ph:
                exg = sb(ph, "exg", [128, 2, EXW], F32)
                exo = sb(ph, "exo", [128, EXW], F32)
                for p_, (src_, dst_) in enumerate(parts):
                    for r_ in range(2):
                        S.dma("sp", exg.t[:, r_, :], ex_out[slot][p_][r_ * 128:(r_ + 1) * 128, 0:EXW], reads=["exout%d_%d" % (slot, p_)], writes=[exg.k])
                    S.op("dve", lambda e: e.tensor_scalar(out=exo.t[:], in0=exg.t[:, 0, :], scalar1=pmk.t[:, 0:1], scalar2=None, op0=ALU.mult),
                         reads=[exg.k, pmk.k], writes=[exo.k])
                    S.op("dve", lambda e: e.scalar_tensor_tensor(out=exo.t[:], in0=exg.t[:, 1, :], scalar=pmk.t[:, 1:2], in1=exo.t[:], op0=ALU.mult, op1=ALU.add),
                         reads=[exg.k, pmk.k, exo.k], writes=[exo.k])
                    S.dma("act", dst_[:, :], exo.t[:], reads=[exo.k])
                S.barrier(pool=True)

        def program():
            cast_layer(0)
            ada_phase()
            if stop(1):
                return
            for i in range(DEPTH):
                if i % 2 == 0:
                    for V in VCS:
                        mlstm_pre(V, i, V.xin if i == 0 else V.xres)
                        if stopped[0]:
                            return
                    if stop(4):
                        return
                    if PAIR:
                        V = VCS[0]
                        mlstm_scan(V, PV, i, 1)
                        pair_exchange(0, [(V.st[0], PV.st[0]), (V.st[1], PV.st[1])])
                        mlstm_scan(V, PV, i, 2)
                        pair_exchange(1, [(V.st[2], PV.st[2])])
                        mlstm_stage3(V, PV, i)
                    else:
                        for st_ in (1, 2):
                            for V in VCS:
                                mlstm_scan(V, VCS[1 - V.idx], i, st_)
                        for V in VCS:
                            mlstm_stage3(V, VCS[1 - V.idx], i)
                else:
                    for V in VCS:
                        attn_pre(V, i, V.xres)
                    if PAIR:
                        S.barrier(pool=True)
                        for k in range(16):
                            S.collective("AllGather", ALU.bypass, PAIRS, akc_in[k].ap().opt(), akc[k].ap().opt(), writes=["akc%d" % k])
                        for k in range(NVC):
                            S.collective("AllGather", ALU.bypass, PAIRS, avc_in[k].ap().opt(), avc[k].ap().opt(), writes=["avc%d" % k])
                    for V in VCS:
                        attn_core(V, i)
                if stop(6):
                    return
                if i + 1 < DEPTH:
                    S.barrier(pool=True)
                    cast_layer(i + 1)
                for V in VCS:
                    ffn_layer(V, i, i == DEPTH - 1)

        program()
        S.finish()
    build.ninstr = S.ninstr
    return nc


def _prep(inputs, S_len, CTX, DEPTH, ncore, pair=False):
    x = np.asarray(inputs["x"], np.float32)
    ctx = np.asarray(inputs["ctx"], np.float32)
    c = np.asarray(inputs["c"], np.float32)
    c_ctx = np.asarray(inputs["c_ctx"], np.float32)
    NTL = S_len // 2 // 128
    HL = S_len // 2
    HC = CTX // 2
    assert HC == 128
    NA = (DEPTH + 1) // 2
    NBL = DEPTH // 2
    g = lambda k: np.asarray(inputs[k], np.float32)
    n_freq = 32
    freqs = (ROPE_BASE ** (-np.arange(n_freq, dtype=np.float32) / n_freq)).astype(np.float32)
    tok = np.arange(S_len)
    ang = np.stack([tok // GRID_W, tok % GRID_W], -1).astype(np.float32)[:, :, None] * freqs
    cos = np.cos(ang).astype(np.float32).reshape(S_len, 64)
    sin = np.sin(ang).astype(np.float32).reshape(S_len, 64)
    rope_full = np.concatenate([cos, sin], 1)
    rope_ctx = np.concatenate([np.ones((HC, 64), np.float32), np.zeros((HC, 64), np.float32)], 1)
    ident = np.eye(128, dtype=np.float32)
    triU = np.triu(np.ones((128, 128), np.float32))
    triL = np.tril(np.ones((128, 128), np.float32))
    consts = np.concatenate([ident, triU, triL, np.ones((128, 128), np.float32)], 1)
    mwin = g("mlstm_w_in")
    selm = np.zeros((2, 256), np.float32)
    selm[0, 0:128] = 1.0
    selm[1, 128:256] = 1.0
    perm1 = np.concatenate([np.arange(16, 32), np.arange(0, 16)])
    wg0 = mwin[:NA, :, 6144:]
    m_wg = np.ascontiguousarray(np.stack([wg0, wg0[:, :, perm1]], 0))
    gb0 = g("mlstm_gate_b")[:NA]
    m_gb = np.ascontiguousarray(np.stack([gb0, gb0[:, perm1]], 0))
    shared = {
        "sel": selm,
        "ada_w": g("ada_w")[:DEPTH],
        "ada_b": g("ada_b")[:DEPTH],
        "norm_g": g("norm_g")[:DEPTH],
        "m_win": np.ascontiguousarray(mwin[:NA, :, 0:6144]),
        "m_wg": m_wg,
        "m_gb": m_gb,
        "m_hg": g("mlstm_head_g")[:NA],
        "m_wout": g("mlstm_w_out")[:NA],
        "f_wgu": g("ffn_w_gu")[:DEPTH],
        "f_wdn": g("ffn_w_down")[:DEPTH],
        "consts": consts,
    }
    if NBL:
        shared["d_win"] = g("diff_w_in")[:NBL]
        shared["d_wout"] = g("diff_w_out")[:NBL]
        shared["d_vec"] = np.ascontiguousarray(np.stack([g("diff_q_g")[:NBL], g("diff_k_g")[:NBL], g("diff_lq1")[:NBL], g("diff_lk1")[:NBL],
                                                         g("diff_lq2")[:NBL], g("diff_lk2")[:NBL]], 1))
        shared["d_sg"] = g("diff_subln_g")[:NBL]
    in_maps = []
    if pair:
        if DEPTH % 2 == 0:
            ada_split = [np.ascontiguousarray(g("ada_w")[s_:DEPTH:2]) for s_ in range(2)]
        for r in range(ncore):
            b, s_ = r // 2, r % 2
            xl = x[b, s_ * HL:(s_ + 1) * HL]
            cl = ctx[b, s_ * HC:(s_ + 1) * HC]
            rl = rope_full[s_ * HL:(s_ + 1) * HL]
            if s_ == 1:
                xl, cl, rl = xl[::-1], cl[::-1], rl[::-1]
            m = dict(shared)
            if DEPTH % 2 == 0:
                m["ada_w"] = ada_split[s_]
            m["m_wg"] = np.ascontiguousarray(m_wg[s_])
            m["m_gb"] = np.ascontiguousarray(m_gb[s_])
            m["xin"] = np.ascontiguousarray(np.concatenate([xl, cl], 0))
            m["rope"] = np.ascontiguousarray(np.concatenate([rl, rope_ctx], 0))
            m["cT"] = np.ascontiguousarray(np.stack([c[b], c_ctx], 1))
            pm = np.zeros((128, 2), np.float32)
            pm[:, 1 - s_] = 1.0
            m["pmask"] = pm
            in_maps.append(m)
        return in_maps, NTL
    for b in range(ncore):
        xs, rs = [], []
        for s_ in range(2):
            xl = x[b, s_ * HL:(s_ + 1) * HL]
            cl = ctx[b, s_ * HC:(s_ + 1) * HC]
            rl = rope_full[s_ * HL:(s_ + 1) * HL]
            if s_ == 1:
                xl, cl, rl = xl[::-1], cl[::-1], rl[::-1]
            xs.append(np.concatenate([xl, cl], 0))
            rs.append(np.concatenate([rl, rope_ctx], 0))
        m = dict(shared)
        m["xin"] = np.ascontiguousarray(np.stack(xs, 0))
        m["rope"] = np.ascontiguousarray(np.stack(rs, 0))
        m["cT"] = np.ascontiguousarray(np.stack([c[b], c_ctx], 1))
        in_maps.append(m)
    return in_maps, NTL


_cache = {}


def run(inputs, DEPTH=4, ncore=None, pair=True):
    x = np.asarray(inputs["x"])
    B, S_len, _ = x.shape
    if ncore is None:
        ncore = 2 * B if pair else B
    CTX = np.asarray(inputs["ctx"]).shape[1]
    in_maps, NTL = _prep(inputs, S_len, CTX, DEPTH, ncore, pair)
    key = (NTL, DEPTH, pair)
    if key not in _cache:
        _cache[key] = build(NTL, DEPTH, pair)
    nc = _cache[key]
    res = run_bass_kernel_spmd(nc, in_maps, core_ids=list(range(ncore)))
    HL = S_len // 2
    out = np.zeros((B, S_len, D), np.float32)
    if pair:
        for r in range(ncore):
            b, s_ = r // 2, r % 2
            y = res.results[r]["y"]
            out[b, s_ * HL:(s_ + 1) * HL] = y if s_ == 0 else y[::-1]
        return out
    for b in range(ncore):
        y = res.results[b]["y"]
        out[b, 0:HL] = y[0]
        out[b, HL:] = y[1][::-1]
    return out


def kernel(**inputs):
    return run(inputs, DEPTH=4)
```

```python
import math
from contextlib import ExitStack

import numpy as np
import ml_dtypes

import concourse.bass as bass
import concourse.mybir as mybir
from concourse.bass_utils import run_bass_kernel_spmd

F32 = mybir.dt.float32
BF16 = mybir.dt.bfloat16
AF = mybir.ActivationFunctionType
ALU = mybir.AluOpType
AX = mybir.AxisListType

D = 2048
FF = 5632
EPS = 1e-6
GATE_CAP = 15.0
GRID_W = 64
ROPE_BASE = 10000.0
NB = 4


class Sched:
    NDMA = 28

    def __init__(self, nc, stack):
        self.nc = nc
        self.eng = {"pe": nc.tensor, "act": nc.scalar, "dve": nc.vector, "pool": nc.gpsimd, "sp": nc.sync}
        self.sem = {}
        self.cnt = {}
        for e in ("pe", "act", "dve", "pool"):
            self.sem[e] = stack.enter_context(nc.semaphore("prog_" + e))
            self.cnt[e] = 0
        self.dsem = [stack.enter_context(nc.semaphore("dma%d" % i)) for i in range(self.NDMA)]
        self.dval = [0] * self.NDMA
        self.dnext = 0
        self.NPOOL = 2
        self.pnext = 0
        self.ccsem = stack.enter_context(nc.semaphore("cc"))
        self.ccval = 0
        self.known = {e: {} for e in self.eng}
        self.semobj = {}
        for e in self.sem:
            self.semobj[("p", e)] = self.sem[e]
        for i in range(self.NDMA):
            self.semobj[("d", i)] = self.dsem[i]
        self.semobj[("c", 0)] = self.ccsem
        self.lastw = {}
        self.readers = {}
        self.ninstr = 0

    def _wait(self, e, semkey, val):
        if semkey == ("p", "pe") and e == "pe":
            return
        k = self.known[e]
        if k.get(semkey, 0) >= val:
            return
        self.eng[e].wait_ge(self.semobj[semkey], val)
        k[semkey] = val
        self.ninstr += 1

    def _deps(self, e, reads, writes):
        for r in reads:
            w = self.lastw.get(r)
            if w is not None:
                self._wait(e, w[0], w[1])
        for wkey in writes:
            w = self.lastw.get(wkey)
            if w is not None:
                self._wait(e, w[0], w[1])
            rd = self.readers.get(wkey)
            if rd:
                for sk, v in rd.items():
                    self._wait(e, sk, v)

    def _commit(self, tok, reads, writes):
        for r in reads:
            d = self.readers.setdefault(r, {})
            if d.get(tok[0], 0) < tok[1]:
                d[tok[0]] = tok[1]
        for w in writes:
            self.lastw[w] = tok
            self.readers[w] = {}

    def op(self, e, fn, reads=(), writes=()):
        self._deps(e, reads, writes)
        ins = fn(self.eng[e])
        self.cnt[e] += 1
        ins.then_inc(self.sem[e], 1)
        self.ninstr += 1
        self._commit((("p", e), self.cnt[e]), reads, writes)
        return ins

    def dma(self, e, out, in_, reads=(), writes=(), **kw):
        self._deps(e, reads, writes)
        if e == "pool":
            i = self.NDMA - self.NPOOL + self.pnext
            self.pnext = (self.pnext + 1) % self.NPOOL
        else:
            i = self.dnext
            self.dnext = (self.dnext + 1) % (self.NDMA - self.NPOOL)
        if self.dval[i] > 0:
            self._wait(e, ("d", i), self.dval[i])
        ins = self.eng[e].dma_start(out=out, in_=in_, **kw)
        self.dval[i] += 16
        ins.then_inc(self.dsem[i], 16)
        self.ninstr += 1
        self._commit((("d", i), self.dval[i]), reads, writes)
        return ins

    def collective(self, kind, op, groups, in_ap, out_ap, reads=(), writes=()):
        e = "pool"
        self._deps(e, reads, writes)
        ins = self.eng[e].collective_compute(kind, op, replica_groups=groups, ins=[in_ap], outs=[out_ap])
        self.ccval += 1
        ins.then_inc(self.ccsem, 1)
        self.ninstr += 1
        self._commit((("c", 0), self.ccval), reads, writes)
        return ins

    def barrier(self, pool=False):
        toks = [(("p", e), self.cnt[e]) for e in self.sem if self.cnt[e] > 0 and (pool or e != "pool")]
        nd = self.NDMA if pool else self.NDMA - self.NPOOL
        toks += [(("d", i), self.dval[i]) for i in range(nd) if self.dval[i] > 0]
        for e in self.eng:
            if e == "pool" and not pool:
                continue
            for sk, v in toks:
                self._wait(e, sk, v)

    def sync_collectives(self, scratch):
        if self.ccval:
            self._wait("act", ("c", 0), self.ccval)
        self.op("act", lambda e: e.copy(out=scratch[:, 0:1], in_=scratch[:, 1:2]))
        self.barrier(pool=True)

    def finish(self):
        toks = [(("p", e), self.cnt[e]) for e in self.sem if self.cnt[e] > 0]
        toks += [(("d", i), self.dval[i]) for i in range(self.NDMA) if self.dval[i] > 0]
        if self.ccval:
            toks.append((("c", 0), self.ccval))
        for sk, v in toks:
            self._wait("sp", sk, v)


_uid = [0]


def uid(p):
    _uid[0] += 1
    return "%s#%d" % (p, _uid[0])


class Buf:
    def __init__(self, t, key):
        self.t = t
        self.k = key


STOP = [99]
DBGF = [0]


class _Stop(Exception):
    pass


def build(NTL, DEPTH, PAIR=False):
    NT = NTL + 1
    T = NT * 128
    TLAT = NTL * 128
    NA = (DEPTH + 1) // 2
    NBL = DEPTH // 2
    R = 2

    nc = bass.Bass("TRN2", target_bir_lowering=False)

    def din(name, shape, dt=F32):
        return nc.dram_tensor(name, shape, dt, kind="ExternalInput")

    def dint(name, shape, dt):
        return nc.dram_tensor(name, shape, dt)

    NV = 1 if PAIR else 2
    TP = 1 << (T - 1).bit_length()
    PAIRS = [[2 * i, 2 * i + 1] for i in range(4)]
    xin2 = din("xin", [T, D] if PAIR else [2, T, D])
    cT = din("cT", [D, R])
    sel = din("sel", [R, 256])
    ADA_SPLIT = PAIR and DEPTH % 2 == 0
    NADA = DEPTH // 2 if ADA_SPLIT else DEPTH
    ada_w = din("ada_w", [NADA, D, 6 * D])
    ada_b = din("ada_b", [DEPTH, 6 * D])
    norm_g = din("norm_g", [DEPTH, 2, D])
    m_win = din("m_win", [NA, D, 6144])
    m_wg2 = din("m_wg", [NA, D, 32] if PAIR else [2, NA, D, 32])
    m_gb2 = din("m_gb", [NA, 32] if PAIR else [2, NA, 32])
    m_hg = din("m_hg", [NA, D])
    m_wout = din("m_wout", [NA, D, D])
    if NBL:
        d_win = din("d_win", [NBL, D, 6144])
        d_wout = din("d_wout", [NBL, D, D])
        d_vec = din("d_vec", [NBL, 6, 128])
        d_sg = din("d_sg", [NBL, 256])
    f_wgu = din("f_wgu", [DEPTH, D, 2 * FF])
    f_wdn = din("f_wdn", [DEPTH, FF, D])
    rope2 = din("rope", [T, 128] if PAIR else [2, T, 128])
    if PAIR:
        pmask = din("pmask", [128, 2])
    consts = din("consts", [128, 512])
    yout2 = nc.dram_tensor("y", [TLAT, D] if PAIR else [2, TLAT, D], F32, kind="ExternalOutput")

    ada_full = dint("ada_full", [NADA * R, 6 * D], F32)
    if ADA_SPLIT:
        ada_all = dint("ada_all", [2 * NADA * R, 6 * D], F32)
    actT = dint("actT", [FF, T], BF16)
    EXW = 8 * 258

    class VC:
        pass

    VCS = []
    for v in range(NV):
        V = VC()
        V.idx = v
        V.xin = xin2.ap() if PAIR else xin2[v]
        V.rope = rope2.ap() if PAIR else rope2[v]
        V.y = yout2.ap() if PAIR else yout2[v]
        V.m_wg = m_wg2.ap() if PAIR else m_wg2[v]
        V.m_gb = m_gb2.ap() if PAIR else m_gb2[v]
        V.xres = dint("xres%d" % v, [T, D], F32)
        V.mq = [dint("mq%d_%d" % (v, i), [T, 1024], BF16) for i in range(2)]
        V.mk = [dint("mk%d_%d" % (v, i), [T, 1024], BF16) for i in range(2)]
        V.mv = dint("mv%d" % v, [T, 2048], BF16)
        V.mo = dint("mo%d" % v, [T, 2048], BF16)
        V.mhA = dint("mhA%d" % v, [T, 2048], F32)
        V.st = [dint("st%d_%d" % (v, i), [128, EXW], F32) for i in range(3)]
        if NBL:
            V.aqT = dint("aqT%d" % v, [2048, T], BF16)
            V.akT = dint("akT%d" % v, [2048, TP], BF16)
            V.av = dint("av%d" % v, [TP, 2048], BF16)
        VCS.append(V)
    if PAIR:
        PV = VC()
        PV.st = [dint("pst%d" % i, [128, EXW], F32) for i in range(3)]
        EXP = 4096
        ex_in = [[dint("ex_in%d_%d" % (i, p), [128, EXP], F32) for p in range(2)] for i in range(2)]
        ex_out = [[dint("ex_out%d_%d" % (i, p), [256, EXP], F32) for p in range(2)] for i in range(2)]
        if NBL:
            NVC = (T + 255) // 256
            akc_in = [dint("akc_in%d" % k, [128, 4096], BF16) for k in range(16)]
            akc = [dint("akc%d" % k, [256, 4096], BF16) for k in range(16)]
            avc_in = [dint("avc_in%d" % k, [256, 2048], BF16) for k in range(NVC)]
            avc = [dint("avc%d" % k, [512, 2048], BF16) for k in range(NVC)]

    with ExitStack() as st:
        S = Sched(nc, st)

        def sb(stack, name, shape, dt):
            return Buf(stack.enter_context(nc.sbuf_tensor(uid(name), shape, dt)), uid(name))

        def ps(stack, name, shape, dt):
            return Buf(stack.enter_context(nc.psum_tensor(uid(name), shape, dt)), uid(name))

        cst = sb(st, "cst", [128, 512], F32)
        cstb = sb(st, "cstb", [128, 512], BF16)
        scr = sb(st, "scr", [128, 2], F32)
        selt = sb(st, "selt", [R, 256], F32)
        PF = [ps(st, "pf%d" % i, [128, 512], F32) for i in range(6)]
        PB = [ps(st, "pb%d" % i, [128, 1024], BF16) for i in range(2)]
        S.dma("sp", cst.t[:], consts[:, :], writes=[cst.k])
        for V in VCS:
            V.GF = sb(st, "GF", [128, NT, 48], F32)
        if PAIR:
            pmk = sb(st, "pmk", [128, 2], F32)
            S.dma("sp", pmk.t[:], pmask[:, :], writes=[pmk.k])
        S.dma("sp", selt.t[:], sel[:, :], writes=[selt.k])
        S.op("dve", lambda e: e.tensor_copy(out=cstb.t[:], in_=cst.t[:]), reads=[cst.k], writes=[cstb.k])
        ident = cstb.t[:, 0:128]
        triU_f, triL_f, ones_f = cst.t[:, 128:256], cst.t[:, 256:384], cst.t[:, 384:512]
        CK = [cst.k, cstb.k]

        stopped = [False]

        def stop(level):
            if STOP[0] <= level:
                stopped[0] = True
            return stopped[0]

        wfull = {}

        def layer_weights(i):
            j = i // 2
            if i % 2 == 0:
                return [("win%d" % i, m_win[j], D, 6144), ("wout%d" % i, m_wout[j], D, D),
                        ("wgu%d" % i, f_wgu[i], D, 2 * FF), ("wdn%d" % i, f_wdn[i], FF, D)]
            return [("win%d" % i, d_win[j], D, 6144), ("wout%d" % i, d_wout[j], D, D),
                    ("wgu%d" % i, f_wgu[i], D, 2 * FF), ("wdn%d" % i, f_wdn[i], FF, D)]

        def cast_layer(i):
            for (tag, src, rows, ncols) in layer_weights(i):
                full = dint(tag + "_bf", [rows, ncols], BF16)
                kfull = uid(tag + "bf")
                step = 32
                keys = []
                for r0 in range(0, rows, step):
                    r1 = min(rows, r0 + step)
                    S.dma("pool", full[r0:r1, :], src[r0:r1, :], writes=[kfull + str(r0)])
                    keys.append(kfull + str(r0))
                wfull[tag] = (full, keys)

        def ada_phase():
            with ExitStack() as ph:
                cTt = sb(ph, "cTt", [128, 16, R], F32)
                sil = sb(ph, "sil", [128, 16, R], F32)
                S.dma("sp", cTt.t[:], cT.ap().rearrange("(k p) r -> p k r", p=128), writes=[cTt.k])
                S.op("act", lambda e: e.activation(out=sil.t[:], in_=cTt.t[:], func=AF.Silu), reads=[cTt.k], writes=[sil.k])
                wts = [sb(ph, "adaw%d" % i, [128, 16, 512], F32) for i in range(2)]
                outb = [sb(ph, "adao%d" % i, [R, 512], F32) for i in range(2)]
                n = 0
                for i in range(NADA):
                    for cb in range(24):
                        w = wts[n % 2]
                        ob = outb[n % 2]
                        pf = PF[n % 4]
                        n += 1
                        S.dma("sp", w.t[:], ada_w[i].rearrange("(k p) n -> p k n", p=128)[:, :, cb * 512:(cb + 1) * 512], writes=[w.k])
                        for kc in range(16):
                            S.op("pe", lambda e, pf=pf, w=w, kc=kc: e.matmul(pf.t[0:R, :], lhsT=sil.t[:, kc, :], rhs=w.t[:, kc, :],
                                                                             start=(kc == 0), stop=(kc == 15)), reads=[sil.k, w.k], writes=[pf.k])
                        S.op("dve", lambda e, pf=pf, ob=ob: e.tensor_copy(out=ob.t[:], in_=pf.t[0:R, :]), reads=[pf.k], writes=[ob.k])
                        S.dma("act", ada_full[i * R:(i + 1) * R, cb * 512:(cb + 1) * 512], ob.t[:], reads=[ob.k], writes=[])
                if ADA_SPLIT:
                    S.barrier(pool=True)
                    S.collective("AllGather", ALU.bypass, PAIRS, ada_full.ap().opt(), ada_all.ap().opt(), writes=["ada_all"])
                else:
                    S.barrier()

        def mod_tmps(ph):
            return (sb(ph, "modrow", [R, 2048], F32), sb(ph, "modb", [R, 2048], F32), sb(ph, "modg", [128, 2048], F32))

        def mod_tile(ph, tmps, i, which, vec, kind, gidx=None):
            out = sb(ph, "mod", [128, 2048], F32)
            row, brow, gt = tmps
            if ADA_SPLIT:
                r0 = (i % 2) * NADA * R + (i // 2) * R
                S.dma("sp", row.t[:], ada_all[r0:r0 + R, vec * 2048:(vec + 1) * 2048], reads=["ada_all"], writes=[row.k])
            else:
                S.dma("sp", row.t[:], ada_full[i * R:(i + 1) * R, vec * 2048:(vec + 1) * 2048], reads=[], writes=[row.k])
            S.dma("sp", brow.t[:], ada_b[i, vec * 2048:(vec + 1) * 2048].partition_broadcast(R), writes=[brow.k])
            S.op("dve", lambda e: e.tensor_tensor(out=row.t[:], in0=row.t[:], in1=brow.t[:], op=ALU.add), reads=[row.k, brow.k], writes=[row.k])
            if kind == "scale":
                S.dma("sp", gt.t[:], norm_g[i, gidx, :].partition_broadcast(128), writes=[gt.k])
            for q in range(4):
                pf = PF[q]
                S.op("pe", lambda e, pf=pf, q=q: e.matmul(pf.t[:], lhsT=selt.t[:, which * 128:(which + 1) * 128],
                                                          rhs=row.t[:, q * 512:(q + 1) * 512], start=True, stop=True),
                     reads=[selt.k, row.k], writes=[pf.k])
                if kind == "scale":
                    S.op("dve", lambda e, pf=pf, q=q: e.scalar_tensor_tensor(
                        out=out.t[:, q * 512:(q + 1) * 512], in0=pf.t[:], scalar=1.0, in1=gt.t[:, q * 512:(q + 1) * 512],
                        op0=ALU.add, op1=ALU.mult), reads=[pf.k, gt.k], writes=[out.k])
                else:
                    S.op("dve", lambda e, pf=pf, q=q: e.tensor_copy(out=out.t[:, q * 512:(q + 1) * 512], in_=pf.t[:]),
                         reads=[pf.k], writes=[out.k])
            return out

        def norm_phase(ph, XT, i, which_norm, src):
            vs, vsh = (1, 0) if which_norm == 0 else (4, 3)
            tmps = mod_tmps(ph)
            A = [mod_tile(ph, tmps, i, w, vs, "scale", which_norm) for w in range(2)]
            Sh = [mod_tile(ph, tmps, i, w, vsh, "raw") for w in range(2)]
            xt = [sb(ph, "nx", [128, 2048], F32) for _ in range(2)]
            junk = sb(ph, "njunk", [128, 2048], BF16)
            tmp = sb(ph, "ntmp", [128, 2048], F32)
            hb = [sb(ph, "nhb", [128, 2048], BF16) for _ in range(2)]
            ssq = [sb(ph, "nss", [128, 1], F32) for _ in range(2)]
            for j in range(NT):
                w = 0 if j < NTL else 1
                x = xt[j % 2]
                h = hb[j % 2]
                ss = ssq[j % 2]
                S.dma("sp", x.t[:], src[j * 128:(j + 1) * 128, :], writes=[x.k])
                S.op("act", lambda e, x=x, ss=ss: e.activation(out=junk.t[:], in_=x.t[:], func=AF.Square, accum_out=ss.t[:]),
                     reads=[x.k], writes=[junk.k, ss.k])
                S.op("act", lambda e, ss=ss: e.activation(out=ss.t[:], in_=ss.t[:], func=AF.Sqrt, scale=1.0 / D, bias=EPS),
                     reads=[ss.k], writes=[ss.k])
                S.op("dve", lambda e, ss=ss: e.reciprocal(out=ss.t[:], in_=ss.t[:]),
                     reads=[ss.k], writes=[ss.k])
                S.op("dve", lambda e, x=x, ss=ss, w=w: e.scalar_tensor_tensor(out=tmp.t[:], in0=x.t[:], scalar=ss.t[:, 0:1], in1=A[w].t[:],
                                                                               op0=ALU.mult, op1=ALU.mult),
                     reads=[x.k, ss.k, A[w].k], writes=[tmp.k])
                S.op("dve", lambda e, h=h, w=w: e.tensor_tensor(out=h.t[:], in0=tmp.t[:], in1=Sh[w].t[:], op=ALU.add),
                     reads=[tmp.k, Sh[w].k], writes=[h.k])
                transpose_into(XT, h, j)

        def transpose_into(XT, h, j, nchunks=16, kc0=0):
            for half in range(0, nchunks, 8):
                n = min(8, nchunks - half)
                pb = PB[(half // 8) % 2]
                for c in range(n):
                    S.op("pe", lambda e, pb=pb, c=c, half=half: e.transpose(pb.t[:, c * 128:(c + 1) * 128], h.t[:, (half + c) * 128:(half + c + 1) * 128], ident),
                         reads=[h.k] + CK, writes=[pb.k])
                S.op("act", lambda e, pb=pb, n=n, half=half: e.copy(
                    out=XT.t[:, kc0 + half:kc0 + half + n, j * 128:(j + 1) * 128],
                    in_=pb.t[:, 0:n * 128].rearrange("p (c t) -> p c t", c=n)),
                    reads=[pb.k], writes=[XT.k + "_%d" % j])

        def lin_tm(ph, XT, wtag, col_blocks, epilogue, kchunks=16, tiles=None, bw=512):
            full, wkeys = wfull[wtag]
            slabs = [sb(ph, "slab", [128, kchunks, bw], BF16) for _ in range(2)]
            wv = full.ap().rearrange("(k p) n -> p k n", p=128)
            n = 0
            for cb in col_blocks:
                sl = slabs[n % 2]
                n += 1
                S.dma("sp", sl.t[:], wv[:, :, cb * bw:(cb + 1) * bw], reads=wkeys, writes=[sl.k])
                for j in (tiles if tiles is not None else range(NT)):
                    pf = PF[(j + n) % 6]
                    if DBGF[0] >= 2:
                        continue
                    for kc in range(kchunks):
                        S.op("pe", lambda e, pf=pf, sl=sl, kc=kc, j=j: e.matmul(
                            pf.t[:, 0:bw], lhsT=XT.t[:, kc, j * 128:(j + 1) * 128], rhs=sl.t[:, kc, :],
                            start=(kc == 0), stop=(kc == kchunks - 1)),
                            reads=[XT.k + "_%d" % j, sl.k], writes=[pf.k])
                    if DBGF[0] >= 1:
                        continue
                    epilogue(j, cb, pf)

        def make_resid_epilogue(ph, V, i, gvec, src, dst, last=False):
            tmps = mod_tmps(ph)
            G = [mod_tile(ph, tmps, i, w, gvec, "raw") for w in range(2)]
            xp = [sb(ph, "rx", [128, 512], F32) for _ in range(3)]
            cnt = [0]

            def epi(j, cb, pf):
                w = 0 if j < NTL else 1
                x = xp[cnt[0] % 3]
                cnt[0] += 1
                key = "xres_%d_%d" % (j, cb)
                S.dma("sp", x.t[:], src[j * 128:(j + 1) * 128, cb * 512:(cb + 1) * 512], reads=[key], writes=[x.k])
                S.op("dve", lambda e: e.tensor_tensor(out=pf.t[:], in0=pf.t[:], in1=G[w].t[:, cb * 512:(cb + 1) * 512], op=ALU.mult),
                     reads=[pf.k, G[w].k], writes=[pf.k])
                S.op("dve", lambda e: e.tensor_tensor(out=x.t[:], in0=pf.t[:], in1=x.t[:], op=ALU.add),
                     reads=[pf.k, x.k], writes=[x.k])
                if last:
                    if j < NTL:
                        S.dma("act", V.y[j * 128:(j + 1) * 128, cb * 512:(cb + 1) * 512], x.t[:], reads=[x.k], writes=[key + "y"])
                else:
                    S.dma("act", dst[j * 128:(j + 1) * 128, cb * 512:(cb + 1) * 512], x.t[:], reads=[x.k], writes=[key])
            return epi

        def ffn_gu(ph, XT, i):
            full, wkeys = wfull["wgu%d" % i]
            wv = full.ap().rearrange("(k p) n -> p k n", p=128)
            slg = [sb(ph, "slg", [128, 16, 512], BF16) for _ in range(2)]
            slu = [sb(ph, "slu", [128, 16, 512], BF16) for _ in range(2)]
            sg = [sb(ph, "sg", [128, 512], F32) for _ in range(2)]
            ao = [sb(ph, "ao", [128, 512], BF16) for _ in range(3)]
            tblocks = [(t0, min(512, T - t0)) for t0 in range(0, T, 512)]
            n = 0
            m = 0
            for cb in range(FF // 512):
                g = slg[n % 2]
                u = slu[n % 2]
                n += 1
                S.dma("sp", g.t[:], wv[:, :, cb * 512:(cb + 1) * 512], reads=wkeys, writes=[g.k])
                S.dma("sp", u.t[:], wv[:, :, FF + cb * 512:FF + (cb + 1) * 512], reads=wkeys, writes=[u.k])
                for (t0, tw) in tblocks:
                    tkeys = [XT.k + "_%d" % j for j in range(t0 // 128, (t0 + tw) // 128)]
                    for fc in range(4):
                        pg = PF[(2 * m) % 6]
                        pu = PF[(2 * m + 1) % 6]
                        s_ = sg[m % 2]
                        a = ao[m % 3]
                        m += 1
                        for (pp, sl) in ((pg, g), (pu, u)):
                            for kc in range(16):
                                S.op("pe", lambda e, pp=pp, sl=sl, kc=kc: e.matmul(
                                    pp.t[:, 0:tw], lhsT=sl.t[:, kc, fc * 128:(fc + 1) * 128], rhs=XT.t[:, kc, t0:t0 + tw],
                                    start=(kc == 0), stop=(kc == 15)), reads=[sl.k] + tkeys, writes=[pp.k])
                        S.op("act", lambda e: e.activation(out=s_.t[:, 0:tw], in_=pg.t[:, 0:tw], func=AF.Silu), reads=[pg.k], writes=[s_.k])
                        S.op("dve", lambda e: e.tensor_tensor(out=a.t[:, 0:tw], in0=s_.t[:, 0:tw], in1=pu.t[:, 0:tw], op=ALU.mult),
                             reads=[s_.k, pu.k], writes=[a.k])
                        f0 = cb * 512 + fc * 128
                        S.dma("act", actT[f0:f0 + 128, t0:t0 + tw], a.t[:, 0:tw], reads=[a.k], writes=[])

        def ffn_down(ph, V, i, src, last):
            full, wkeys = wfull["wdn%d" % i]
            wv = full.ap().rearrange("(k p) n -> p k n", p=128)
            KC = FF // 128
            slabs = [sb(ph, "dslab", [128, KC, 512], BF16) for _ in range(2)]
            at = [sb(ph, "dact", [128, KC, 128], BF16) for _ in range(2)]
            epi = make_resid_epilogue(ph, V, i, 5, src, V.xres, last)
            av_ = actT.ap().rearrange("(k p) t -> p k t", p=128)
            n = 0
            m = 0
            for cb in range(4):
                sl = slabs[n % 2]
                n += 1
                S.dma("sp", sl.t[:], wv[:, :, cb * 512:(cb + 1) * 512], reads=wkeys, writes=[sl.k])
                for j in range(NT):
                    if last and j >= NTL:
                        continue
                    a = at[m % 2]
                    pf = PF[m % 4]
                    m += 1
                    S.dma("sp", a.t[:], av_[:, :, j * 128:(j + 1) * 128], reads=[], writes=[a.k])
                    for kc in range(KC):
                        S.op("pe", lambda e, kc=kc: e.matmul(pf.t[:], lhsT=a.t[:, kc, :], rhs=sl.t[:, kc, :], start=(kc == 0), stop=(kc == KC - 1)),
                             reads=[a.k, sl.k], writes=[pf.k])
                    epi(j, cb, pf)

        def mlstm_pre(V, i, src):
            j_ = i // 2
            with ExitStack() as ph:
                XT = sb(ph, "XT", [128, 16, T], BF16)
                with ExitStack() as ph1:
                    norm_phase(ph1, XT, i, 0, src)
                    S.barrier()
                if stop(2):
                    return
                GF = V.GF
                mq, mk, mv, mo = V.mq, V.mk, V.mv, V.mo
                with ExitStack() as ph2:
                    wgf = sb(ph2, "wgf", [128, 16, 32], F32)
                    wgb = sb(ph2, "wgb", [128, 16, 32], BF16)
                    gbt = sb(ph2, "gbt", [128, 32], F32)
                    S.dma("sp", wgf.t[:], V.m_wg[j_].rearrange("(k p) n -> p k n", p=128), writes=[wgf.k])
                    S.dma("sp", gbt.t[:], V.m_gb[j_, :].partition_broadcast(128), writes=[gbt.k])
                    S.op("dve", lambda e: e.tensor_copy(out=wgb.t[:], in_=wgf.t[:]), reads=[wgf.k], writes=[wgb.k])
                    gr = sb(ph2, "gr", [128, 32], F32)
                    ex = sb(ph2, "gex", [128, 16], F32)
                    cum = sb(ph2, "cum", [128, 32], F32)
                    for j in range(NT):
                        pf = PF[j % 2]
                        for kc in range(16):
                            S.op("pe", lambda e, kc=kc: e.matmul(pf.t[:, 0:32], lhsT=XT.t[:, kc, j * 128:(j + 1) * 128], rhs=wgb.t[:, kc, :],
                                                                  start=(kc == 0), stop=(kc == 15)),
                                 reads=[XT.k + "_%d" % j, wgb.k], writes=[pf.k])
                        S.op("dve", lambda e: e.tensor_tensor(out=gr.t[:], in0=pf.t[:, 0:32], in1=gbt.t[:], op=ALU.add), reads=[pf.k, gbt.k], writes=[gr.k])
                        S.op("act", lambda e: e.activation(out=gr.t[:], in_=gr.t[:], func=AF.Tanh, scale=1.0 / GATE_CAP), reads=[gr.k], writes=[gr.k])
                        S.op("dve", lambda e: e.tensor_single_scalar(out=gr.t[:], in_=gr.t[:], scalar=GATE_CAP, op=ALU.mult), reads=[gr.k], writes=[gr.k])
                        fv = gr.t[:, :].rearrange("p (a b) -> p a b", a=2)[:, :, 8:16]
                        exv = ex.t[:, :].rearrange("p (a b) -> p a b", a=2)
                        S.op("act", lambda e: e.activation(out=exv, in_=fv, func=AF.Exp, scale=-1.0), reads=[gr.k], writes=[ex.k])
                        S.op("act", lambda e: e.activation(out=exv, in_=exv, func=AF.Ln, bias=1.0), reads=[ex.k], writes=[ex.k])
                        S.op("dve", lambda e: e.tensor_single_scalar(out=fv, in_=exv, scalar=-1.0, op=ALU.mult), reads=[ex.k], writes=[gr.k])
                        pc = PF[2 + j % 2]
                        S.op("pe", lambda e: e.matmul(pc.t[:, 0:8], lhsT=triU_f, rhs=gr.t[:, 8:16], start=True, stop=True), reads=[gr.k] + CK, writes=[pc.k])
                        S.op("pe", lambda e: e.matmul(pc.t[:, 8:16], lhsT=triL_f, rhs=gr.t[:, 24:32], start=True, stop=True), reads=[gr.k] + CK, writes=[pc.k])
                        S.op("pe", lambda e: e.matmul(pc.t[:, 16:24], lhsT=ones_f, rhs=gr.t[:, 8:16], start=True, stop=True), reads=[gr.k] + CK, writes=[pc.k])
                        S.op("pe", lambda e: e.matmul(pc.t[:, 24:32], lhsT=ones_f, rhs=gr.t[:, 24:32], start=True, stop=True), reads=[gr.k] + CK, writes=[pc.k])
                        S.op("dve", lambda e: e.tensor_copy(out=cum.t[:], in_=pc.t[:, 0:32]), reads=[pc.k], writes=[cum.k])
                        g = GF.t[:, j, :]
                        gk = GF.k + "_%d" % j
                        S.op("act", lambda e: e.activation(out=g[:, 0:8], in_=cum.t[:, 0:8], func=AF.Exp), reads=[cum.k], writes=[gk])
                        S.op("act", lambda e: e.activation(out=g[:, 16:24], in_=cum.t[:, 8:16], func=AF.Exp), reads=[cum.k], writes=[gk])
                        S.op("act", lambda e: e.activation(out=g[:, 32:48], in_=cum.t[:, 16:32], func=AF.Exp), reads=[cum.k], writes=[gk])
                        S.op("dve", lambda e: e.tensor_single_scalar(out=g[:, 0:8], in_=g[:, 0:8], scalar=128.0 ** -0.5, op=ALU.mult), reads=[gk], writes=[gk])
                        S.op("dve", lambda e: e.tensor_single_scalar(out=g[:, 16:24], in_=g[:, 16:24], scalar=128.0 ** -0.5, op=ALU.mult), reads=[gk], writes=[gk])
                        S.op("dve", lambda e: e.tensor_tensor(out=cum.t[:, 0:8], in0=gr.t[:, 0:8], in1=cum.t[:, 0:8], op=ALU.subtract), reads=[cum.k, gr.k], writes=[cum.k])
                        S.op("dve", lambda e: e.tensor_tensor(out=cum.t[:, 8:16], in0=gr.t[:, 16:24], in1=cum.t[:, 8:16], op=ALU.subtract), reads=[cum.k, gr.k], writes=[cum.k])
                        S.op("act", lambda e: e.activation(out=g[:, 8:16], in_=cum.t[:, 0:8], func=AF.Exp), reads=[cum.k], writes=[gk])
                        S.op("act", lambda e: e.activation(out=g[:, 24:32], in_=cum.t[:, 8:16], func=AF.Exp), reads=[cum.k], writes=[gk])
                    S.barrier()
                if stop(3):
                    return
                with ExitStack() as ph3:
                    ob = [sb(ph3, "pob", [128, 2, 512], BF16) for _ in range(3)]
                    cnt = [0]

                    def epi(j, cb, pf):
                        o = ob[cnt[0] % 3]
                        cnt[0] += 1
                        gk = GF.k + "_%d" % j
                        if cb < 4:
                            isk = cb >= 2
                            h0 = (cb % 2) * 4
                            for sty in range(2):
                                c0 = sty * 16 + (8 if isk else 0) + h0
                                S.op("dve", lambda e, sty=sty, c0=c0: e.tensor_tensor(
                                    out=o.t[:, sty, :].rearrange("p (h d) -> p h d", h=4),
                                    in0=pf.t[:, :].rearrange("p (h d) -> p h d", h=4),
                                    in1=GF.t[:, j, c0:c0 + 4].unsqueeze(2).broadcast_to([128, 4, 128]), op=ALU.mult),
                                    reads=[pf.k, gk], writes=[o.k])
                                dst = (mk if isk else mq)[sty]
                                S.dma("act", dst[j * 128:(j + 1) * 128, (cb % 2) * 512:(cb % 2 + 1) * 512], o.t[:, sty, :], reads=[o.k], writes=[])
                        elif cb < 8:
                            S.op("act", lambda e: e.copy(out=o.t[:, 0, :], in_=pf.t[:]), reads=[pf.k], writes=[o.k])
                            S.dma("act", mv[j * 128:(j + 1) * 128, (cb - 4) * 512:(cb - 3) * 512], o.t[:, 0, :], reads=[o.k], writes=[])
                        else:
                            S.op("act", lambda e: e.activation(out=o.t[:, 0, :], in_=pf.t[:], func=AF.Sigmoid), reads=[pf.k], writes=[o.k])
                            S.dma("act", mo[j * 128:(j + 1) * 128, (cb - 8) * 512:(cb - 7) * 512], o.t[:, 0, :], reads=[o.k], writes=[])
                    lin_tm(ph3, XT, "win%d" % i, range(12), epi)
                    S.barrier()

        def mlstm_stage3(V, W, i):
            with ExitStack() as outer:
                XT = sb(outer, "XT", [128, 16, T], BF16)
                mlstm_scan(V, W, i, 3, XT)
                with ExitStack() as ph5:
                    epi = make_resid_epilogue(ph5, V, i, 2, (V.xin if i == 0 else V.xres), V.xres)
                    lin_tm(ph5, XT, "wout%d" % i, range(4), epi)
                    S.barrier()

        def mlstm_scan(V, W, i, stage, XT=None):
            j_ = i // 2
            GF = V.GF
            mq, mk, mv, mo, mhA = V.mq, V.mk, V.mv, V.mo, V.mhA
            with ExitStack() as ps_:
                Cf = sb(ps_, "Cf", [128, 8, 258], F32)
                Cb = sb(ps_, "Cb", [128, 8, 257], BF16)
                qt = [sb(ps_, "sq", [128, 1024], BF16) for _ in range(2)]
                kt = [sb(ps_, "sk", [128, 1024], BF16) for _ in range(2)]
                va = [sb(ps_, "sva", [128, 8, 257], BF16) for _ in range(2)]
                qkT = [sb(ps_, "qkT", [128, 4, 128], BF16) for _ in range(2)]
                stm = [sb(ps_, "stm", [128, 2, 128], BF16) for _ in range(2)]
                den = sb(ps_, "den", [128, 2], F32)
                hacc = sb(ps_, "hacc", [128, 2048], F32)
                hprev = sb(ps_, "hprev", [128, 2048], F32)
                tmpc = sb(ps_, "tmpc", [128, 2, 257], F32)
                stg = sb(ps_, "stg", [128, 8, 258], F32)
                X1 = sb(ps_, "X1", [128, 8, 258], F32)
                if stage == 3:
                    hg = sb(ps_, "hg", [128, 2048], F32)
                    og = sb(ps_, "og", [128, 2048], BF16)
                    hsq = sb(ps_, "hsq", [128, 2048], F32)
                    hss = sb(ps_, "hss", [128, 8], F32)
                    hb16 = sb(ps_, "hb16", [128, 2048], BF16)
                    S.dma("sp", hg.t[:], m_hg[j_, :].partition_broadcast(128), writes=[hg.k])
                for v_ in va:
                    S.op("dve", lambda e, v_=v_: e.memset(v_.t[:, :, 256:257], 1.0), writes=[v_.k])
                maskA = cstb.t[:, 128:256]
                maskB = cstb.t[:, 256:384]
                ntile = [0]

                def refresh_cb():
                    S.op("act", lambda e: e.copy(out=Cb.t[:], in_=Cf.t[:, :, 0:257]), reads=[Cf.k], writes=[Cb.k])

                def load_state(src_dram):
                    S.dma("sp", Cf.t[:, :, :].rearrange("p h w -> p (h w)"), src_dram[:, :], writes=[Cf.k])
                    refresh_cb()

                def save_state(dst_dram, buf):
                    S.dma("act", dst_dram[:, :], buf.t[:, :, :].rearrange("p h w -> p (h w)"), reads=[buf.k], writes=[])

                def chunk(j, sty, out_final):
                    n = ntile[0]
                    ntile[0] += 1
                    q, k, v = qt[n % 2], kt[n % 2], va[n % 2]
                    S.dma("sp", q.t[:], mq[sty][j * 128:(j + 1) * 128, :], reads=[], writes=[q.k])
                    S.dma("sp", k.t[:], mk[sty][j * 128:(j + 1) * 128, :], reads=[], writes=[k.k])
                    S.dma("sp", v.t[:, :, 0:256], mv[j * 128:(j + 1) * 128, :].rearrange("p (h d) -> p h d", h=8), reads=[], writes=[v.k])
                    if out_final:
                        S.dma("sp", hprev.t[:], mhA[j * 128:(j + 1) * 128, :], reads=["mhA_%d" % j], writes=[hprev.k])
                        S.dma("sp", og.t[:], mo[j * 128:(j + 1) * 128, :], reads=[], writes=[og.k])
                    mask = maskA if sty == 0 else maskB
                    dcol = 32 + sty * 8
                    gk = GF.k + "_%d" % j
                    for hp in range(4):
                        qk_ = qkT[hp % 2]
                        sm = stm[hp % 2]
                        pb = PB[hp % 2]
                        for c, (srcb, col) in enumerate(((q, 2 * hp), (q, 2 * hp + 1), (k, 2 * hp), (k, 2 * hp + 1))):
                            S.op("pe", lambda e, c=c, srcb=srcb, col=col: e.transpose(pb.t[:, c * 128:(c + 1) * 128], srcb.t[:, col * 128:(col + 1) * 128], ident),
                                 reads=[srcb.k] + CK, writes=[pb.k])
                        S.op("act", lambda e: e.copy(out=qk_.t[:], in_=pb.t[:, 0:512].rearrange("p (c t) -> p c t", c=4)), reads=[pb.k], writes=[qk_.k])
                        pst = PF[0]
                        for hh in range(2):
                            S.op("pe", lambda e, hh=hh: e.matmul(pst.t[:, hh * 128:(hh + 1) * 128], lhsT=qk_.t[:, 2 + hh, :], rhs=qk_.t[:, hh, :], start=True, stop=True),
                                 reads=[qk_.k], writes=[pst.k])
                        S.op("dve", lambda e: e.tensor_tensor(out=sm.t[:], in0=pst.t[:, 0:256].rearrange("p (h t) -> p h t", h=2),
                                                              in1=mask.unsqueeze(1).broadcast_to([128, 2, 128]), op=ALU.mult),
                             reads=[pst.k] + CK, writes=[sm.k])
                        for hh in range(2):
                            h = 2 * hp + hh
                            pn = PF[1 + hh]
                            S.op("pe", lambda e, hh=hh, h=h, pn=pn: e.matmul(pn.t[:, 0:257], lhsT=sm.t[:, hh, :], rhs=v.t[:, h, :], start=True, stop=False),
                                 reads=[sm.k, v.k], writes=[pn.k])
                            S.op("pe", lambda e, hh=hh, h=h, pn=pn: e.matmul(pn.t[:, 0:257], lhsT=qk_.t[:, hh, :], rhs=Cb.t[:, h, :], start=False, stop=True),
                                 reads=[qk_.k, Cb.k], writes=[pn.k])
                            pd = PF[3 + hh]
                            S.op("pe", lambda e, h=h, pd=pd: e.matmul(pd.t[:, 0:257], lhsT=k.t[:, h * 128:(h + 1) * 128], rhs=v.t[:, h, :], start=True, stop=True),
                                 reads=[k.k, v.k], writes=[pd.k])
                            S.op("act", lambda e, hh=hh, pn=pn: e.activation(out=den.t[:, hh:hh + 1], in_=pn.t[:, 256:257], func=AF.Abs), reads=[pn.k], writes=[den.k])
                            S.op("dve", lambda e, hh=hh: e.tensor_single_scalar(out=den.t[:, hh:hh + 1], in_=den.t[:, hh:hh + 1], scalar=1.0, op=ALU.max), reads=[den.k], writes=[den.k])
                            S.op("dve", lambda e, hh=hh: e.reciprocal(out=den.t[:, hh:hh + 1], in_=den.t[:, hh:hh + 1]), reads=[den.k], writes=[den.k])
                            if out_final:
                                S.op("dve", lambda e, hh=hh, h=h, pn=pn: e.scalar_tensor_tensor(
                                    out=hacc.t[:, h * 256:(h + 1) * 256], in0=pn.t[:, 0:256], scalar=den.t[:, hh:hh + 1], in1=hprev.t[:, h * 256:(h + 1) * 256],
                                    op0=ALU.mult, op1=ALU.add), reads=[pn.k, den.k, hprev.k], writes=[hacc.k])
                            else:
                                S.op("dve", lambda e, hh=hh, h=h, pn=pn: e.tensor_scalar(
                                    out=hacc.t[:, h * 256:(h + 1) * 256], in0=pn.t[:, 0:256], scalar1=den.t[:, hh:hh + 1], scalar2=None, op0=ALU.mult),
                                    reads=[pn.k, den.k], writes=[hacc.k])
                            S.op("dve", lambda e, hh=hh, h=h, pd=pd: e.tensor_tensor(out=tmpc.t[:, hh, :], in0=pd.t[:, 0:257], in1=Cf.t[:, h, 0:257], op=ALU.add),
                                 reads=[pd.k, Cf.k], writes=[tmpc.k])
                            S.op("dve", lambda e, hh=hh, h=h: e.tensor_scalar(out=Cf.t[:, h, 0:257], in0=tmpc.t[:, hh, :], scalar1=GF.t[:, j, dcol + h:dcol + h + 1],
                                                                          scalar2=None, op0=ALU.mult), reads=[tmpc.k, gk, Cb.k], writes=[Cf.k])
                    refresh_cb()
                    if out_final:
                        finalize(j)
                    else:
                        S.dma("act", mhA[j * 128:(j + 1) * 128, :], hacc.t[:], reads=[hacc.k], writes=["mhA_%d" % j])

                def finalize(j):
                    S.op("act", lambda e: e.activation(out=hsq.t[:], in_=hacc.t[:], func=AF.Square), reads=[hacc.k], writes=[hsq.k])
                    S.op("dve", lambda e: e.tensor_reduce(out=hss.t[:], in_=hsq.t[:, :].rearrange("p (h d) -> p h d", h=8), axis=AX.X, op=ALU.add),
                         reads=[hsq.k], writes=[hss.k])
                    S.op("act", lambda e: e.activation(out=hss.t[:], in_=hss.t[:], func=AF.Sqrt, scale=1.0 / 256, bias=EPS), reads=[hss.k], writes=[hss.k])
                    S.op("dve", lambda e: e.reciprocal(out=hss.t[:], in_=hss.t[:]), reads=[hss.k], writes=[hss.k])
                    S.op("dve", lambda e: e.tensor_tensor(out=hsq.t[:, :].rearrange("p (h d) -> p h d", h=8), in0=hacc.t[:, :].rearrange("p (h d) -> p h d", h=8),
                                                          in1=hss.t[:, :].unsqueeze(2).broadcast_to([128, 8, 256]), op=ALU.mult),
                         reads=[hacc.k, hss.k], writes=[hsq.k])
                    S.op("dve", lambda e: e.tensor_tensor(out=hsq.t[:], in0=hsq.t[:], in1=hg.t[:], op=ALU.mult), reads=[hsq.k, hg.k], writes=[hsq.k])
                    S.op("dve", lambda e: e.tensor_tensor(out=hb16.t[:], in0=hsq.t[:], in1=og.t[:], op=ALU.mult), reads=[hsq.k, og.k], writes=[hb16.k])
                    transpose_into(XT, hb16, j)


                jc = NTL
                if stage == 1:
                    S.op("dve", lambda e: e.memset(Cf.t[:], 0.0), writes=[Cf.k])
                    S.op("dve", lambda e: e.memset(Cb.t[:], 0.0), writes=[Cb.k])
                    chunk(jc, 0, False)
                    save_state(V.st[0], Cf)
                    n = ntile[0]
                    ntile[0] += 1
                    kB, vB = kt[n % 2], va[n % 2]
                    S.dma("sp", kB.t[:], mk[1][jc * 128:(jc + 1) * 128, :], writes=[kB.k])
                    S.dma("sp", vB.t[:, :, 0:256], mv[jc * 128:(jc + 1) * 128, :].rearrange("p (h d) -> p h d", h=8), writes=[vB.k])
                    for h in range(8):
                        pd = PF[3 + h % 2]
                        S.op("pe", lambda e, h=h, pd=pd: e.matmul(pd.t[:, 0:257], lhsT=kB.t[:, h * 128:(h + 1) * 128], rhs=vB.t[:, h, :], start=True, stop=True),
                             reads=[kB.k, vB.k], writes=[pd.k])
                        S.op("dve", lambda e, h=h, pd=pd: e.tensor_copy(out=stg.t[:, h, 0:257], in_=pd.t[:, 0:257]), reads=[pd.k], writes=[stg.k])
                    S.op("dve", lambda e: e.tensor_copy(out=stg.t[:, :, 257:258], in_=GF.t[:, jc, 40:48].unsqueeze(2)), reads=[GF.k + "_%d" % jc], writes=[stg.k])
                    save_state(V.st[1], stg)
                elif stage == 2:
                    S.dma("sp", Cf.t[:, :, :].rearrange("p h w -> p (h w)"), V.st[0][:, :], writes=[Cf.k])
                    S.dma("sp", X1.t[:, :, :].rearrange("p h w -> p (h w)"), W.st[1][:, :], writes=[X1.k])
                    S.op("dve", lambda e: e.tensor_tensor(out=Cf.t[:, :, 0:257], in0=Cf.t[:, :, 0:257], in1=X1.t[:, :, 0:257], op=ALU.add), reads=[Cf.k, X1.k], writes=[Cf.k])
                    S.op("dve", lambda e: e.tensor_tensor(out=Cf.t[:, :, 0:257], in0=Cf.t[:, :, 0:257], in1=X1.t[:, :, 257:258].broadcast_to([128, 8, 257]), op=ALU.mult),
                         reads=[Cf.k, X1.k], writes=[Cf.k])
                    refresh_cb()
                    for j in range(NTL):
                        chunk(j, 0, False)
                    save_state(V.st[2], Cf)
                else:
                    load_state(W.st[0])
                    chunk(jc, 1, True)
                    load_state(W.st[2])
                    for j in range(NTL - 1, -1, -1):
                        chunk(j, 1, True)
                S.barrier()

        def attn_pre(V, i, src):
            j_ = i // 2
            aqT, akT, av = V.aqT, V.akT, V.av
            rope = V.rope
            with ExitStack() as ph:
                XT = sb(ph, "XT", [128, 16, T], BF16)
                with ExitStack() as ph1:
                    norm_phase(ph1, XT, i, 0, src)
                    S.barrier()
                with ExitStack() as ph2:
                    gq = sb(ph2, "gq", [128, 2, 128], F32)
                    S.dma("sp", gq.t[:], d_vec[j_, 0:2, :].partition_broadcast(128), writes=[gq.k])
                    S.op("dve", lambda e: e.tensor_single_scalar(out=gq.t[:, 0, :], in_=gq.t[:, 0, :], scalar=128.0 ** -0.5, op=ALU.mult), reads=[gq.k], writes=[gq.k])
                    rp = sb(ph2, "rp", [128, NT, 128], F32)
                    S.dma("sp", rp.t[:], rope.rearrange("(j p) c -> p j c", p=128), writes=[rp.k])
                    sq_l = [sb(ph2, "asq", [128, 512], F32) for _ in range(3)]
                    qss_l = [sb(ph2, "qss", [128, 4], F32) for _ in range(3)]
                    qn_l = [sb(ph2, "qn", [128, 512], F32) for _ in range(3)]
                    t1_l = [sb(ph2, "t1", [128, 512], F32) for _ in range(3)]
                    t2_l = [sb(ph2, "t2", [128, 512], F32) for _ in range(3)]
                    qo = [sb(ph2, "qo", [128, 512], BF16) for _ in range(3)]
                    qTs = [sb(ph2, "qTs", [128, 4, 128], BF16) for _ in range(3)]
                    vo = [sb(ph2, "vo", [128, 512], BF16) for _ in range(2)]
                    cnt = [0]

                    def epi(j, cb, pf):
                        n = cnt[0]
                        cnt[0] += 1
                        sq_, qss, qn, t1, t2 = sq_l[n % 3], qss_l[n % 3], qn_l[n % 3], t1_l[n % 3], t2_l[n % 3]
                        if cb < 8:
                            isk = cb >= 4
                            S.op("act", lambda e: e.activation(out=sq_.t[:], in_=pf.t[:], func=AF.Square), reads=[pf.k], writes=[sq_.k])
                            S.op("dve", lambda e: e.tensor_reduce(out=qss.t[:], in_=sq_.t[:, :].rearrange("p (h d) -> p h d", h=4), axis=AX.X, op=ALU.add),
                                 reads=[sq_.k], writes=[qss.k])
                            S.op("act", lambda e: e.activation(out=qss.t[:], in_=qss.t[:], func=AF.Sqrt, scale=1.0 / 128, bias=EPS), reads=[qss.k], writes=[qss.k])
                            S.op("dve", lambda e: e.reciprocal(out=qss.t[:], in_=qss.t[:]), reads=[qss.k], writes=[qss.k])
                            S.op("dve", lambda e: e.tensor_tensor(out=qn.t[:, :].rearrange("p (h d) -> p h d", h=4), in0=pf.t[:, :].rearrange("p (h d) -> p h d", h=4),
                                                                  in1=qss.t[:, :].unsqueeze(2).broadcast_to([128, 4, 128]), op=ALU.mult), reads=[pf.k, qss.k], writes=[qn.k])
                            S.op("dve", lambda e: e.tensor_tensor(out=qn.t[:, :].rearrange("p (h d) -> p h d", h=4), in0=qn.t[:, :].rearrange("p (h d) -> p h d", h=4),
                                                                   in1=gq.t[:, 1 if isk else 0, :].unsqueeze(1).broadcast_to([128, 4, 128]), op=ALU.mult),
                                 reads=[qn.k, gq.k], writes=[qn.k])
                            cosv = rp.t[:, j, 0:64].rearrange("p (a f) -> p a f", a=2)
                            sinv = rp.t[:, j, 64:128].rearrange("p (a f) -> p a f", a=2)
                            qv5 = qn.t[:, :].rearrange("p (h a j f) -> p h a j f", h=4, a=2, j=2)
                            t15 = t1.t[:, :].rearrange("p (h a j f) -> p h a j f", h=4, a=2, j=2)
                            t25 = t2.t[:, :].rearrange("p (h a j f) -> p h a j f", h=4, a=2, j=2)
                            for jj in range(2):
                                S.op("dve", lambda e, jj=jj: e.tensor_tensor(out=t15[:, :, :, jj, :], in0=qv5[:, :, :, jj, :],
                                                                             in1=cosv.unsqueeze(1).broadcast_to([128, 4, 2, 32]), op=ALU.mult),
                                     reads=[qn.k, rp.k], writes=[t1.k])
                                S.op("dve", lambda e, jj=jj: e.tensor_tensor(out=t25[:, :, :, jj, :], in0=qv5[:, :, :, 1 - jj, :],
                                                                              in1=sinv.unsqueeze(1).broadcast_to([128, 4, 2, 32]), op=ALU.mult),
                                     reads=[qn.k, rp.k], writes=[t2.k])
                            o = qo[n % 3]
                            o5 = o.t[:, :].rearrange("p (h a j f) -> p h a j f", h=4, a=2, j=2)
                            S.op("dve", lambda e: e.tensor_tensor(out=o5[:, :, :, 0, :], in0=t15[:, :, :, 0, :], in1=t25[:, :, :, 0, :], op=ALU.subtract),
                                 reads=[t1.k, t2.k], writes=[o.k])
                            S.op("dve", lambda e: e.tensor_tensor(out=o5[:, :, :, 1, :], in0=t15[:, :, :, 1, :], in1=t25[:, :, :, 1, :], op=ALU.add),
                                 reads=[t1.k, t2.k], writes=[o.k])
                            pb = PB[n % 2]
                            for c in range(4):
                                S.op("pe", lambda e, c=c: e.transpose(pb.t[:, c * 128:(c + 1) * 128], o.t[:, c * 128:(c + 1) * 128], ident), reads=[o.k] + CK, writes=[pb.k])
                            qT_ = qTs[n % 3]
                            S.op("act", lambda e: e.copy(out=qT_.t[:], in_=pb.t[:, 0:512].rearrange("p (c t) -> p c t", c=4)), reads=[pb.k], writes=[qT_.k])
                            dst = akT if isk else aqT
                            r0 = (cb % 4) * 512
                            if isk and PAIR:
                                for c in range(4):
                                    S.dma("act", akc_in[(cb % 4) * 4 + c][:, j * 128:(j + 1) * 128], qT_.t[:, c, :], reads=[qT_.k], writes=[])
                            else:
                                S.dma("act", dst[r0:r0 + 512, j * 128:(j + 1) * 128].rearrange("(c p) t -> p c t", p=128), qT_.t[:], reads=[qT_.k], writes=[])
                        else:
                            o = vo[n % 2]
                            S.op("act", lambda e: e.copy(out=o.t[:], in_=pf.t[:]), reads=[pf.k], writes=[o.k])
                            if PAIR:
                                S.dma("act", avc_in[j // 2][(j % 2) * 128:(j % 2 + 1) * 128, (cb - 8) * 512:(cb - 7) * 512], o.t[:], reads=[o.k], writes=[])
                            else:
                                S.dma("act", av[j * 128:(j + 1) * 128, (cb - 8) * 512:(cb - 7) * 512], o.t[:], reads=[o.k], writes=[])
                    lin_tm(ph2, XT, "win%d" % i, range(12), epi)
                    S.barrier()

        def attn_core(V, i):
            j_ = i // 2
            lam_init = 0.8 - 0.6 * math.exp(-0.3 * i)
            aqT = V.aqT
            with ExitStack() as ph:
                XT = sb(ph, "XT", [128, 16, T], BF16)
                lamt = sb(ph, "lamt", [128, 2], F32)
                sgt = sb(ph, "sgt", [128, 256], F32)
                with ExitStack() as ph2:
                    lv = sb(ph2, "lv", [128, 4, 128], F32)
                    S.dma("sp", lv.t[:], d_vec[j_, 2:6, :].partition_broadcast(128), writes=[lv.k])
                    lj = sb(ph2, "lj", [128, 2, 128], F32)
                    ls = sb(ph2, "ls", [128, 2], F32)
                    lvv = lv.t[:, :, :].rearrange("p (a b) d -> p a b d", a=2)
                    S.op("dve", lambda e: e.tensor_tensor(out=lj.t[:], in0=lvv[:, :, 0, :], in1=lvv[:, :, 1, :], op=ALU.mult), reads=[lv.k], writes=[lj.k])
                    S.op("dve", lambda e: e.tensor_reduce(out=ls.t[:], in_=lj.t[:], axis=AX.X, op=ALU.add), reads=[lj.k], writes=[ls.k])
                    S.op("act", lambda e: e.activation(out=ls.t[:], in_=ls.t[:], func=AF.Exp), reads=[ls.k], writes=[ls.k])
                    S.op("dve", lambda e: e.tensor_tensor(out=lamt.t[:, 0:1], in0=ls.t[:, 1:2], in1=ls.t[:, 0:1], op=ALU.subtract), reads=[ls.k], writes=[lamt.k])
                    S.op("dve", lambda e: e.tensor_single_scalar(out=lamt.t[:, 0:1], in_=lamt.t[:, 0:1], scalar=-lam_init, op=ALU.add), reads=[lamt.k], writes=[lamt.k])
                    S.dma("sp", sgt.t[:], d_sg[j_, :].partition_broadcast(128), writes=[sgt.k])
                    S.op("dve", lambda e: e.tensor_single_scalar(out=sgt.t[:], in_=sgt.t[:], scalar=1.0 - lam_init, op=ALU.mult), reads=[sgt.k], writes=[sgt.k])
                    S.barrier()
                with ExitStack() as ph3:
                    NKT = 2 * NT
                    kTs = [sb(ph3, "kTs", [128, 2, 2 * T], BF16) for _ in range(2)]
                    vs_ = [sb(ph3, "vs", [128, NKT, 257], BF16) for _ in range(2)]
                    qs_ = [sb(ph3, "qs", [128, 2, T], BF16) for _ in range(2)]
                    pt = [sb(ph3, "pt", [128, 2, 256], BF16) for _ in range(3)]
                    rd = sb(ph3, "rd", [128, 2], F32)
                    o1 = sb(ph3, "o1", [128, 256], F32)
                    o2 = sb(ph3, "o2", [128, 256], F32)
                    oss = sb(ph3, "oss", [128, 1], F32)
                    ob16 = sb(ph3, "ob16", [128, 256], BF16)
                    for v_ in vs_:
                        S.op("dve", lambda e, v_=v_: e.memset(v_.t[:, :, 256:257], 1.0), writes=[v_.k])
                    if PAIR:
                        kalls = valls = None
                    else:
                        kalls = [VCS[r_].akT.ap().rearrange("(s d) t -> d s t", d=128) for r_ in range(2)]
                        valls = [VCS[r_].av.ap().rearrange("(k p) c -> p k c", p=128) for r_ in range(2)]
                        kvr = []
                    qall = aqT.ap().rearrange("(s d) t -> d s t", d=128)
                    npt = [0]
                    for h in range(8):
                        kT_ = kTs[h % 2]
                        v_ = vs_[h % 2]
                        q_ = qs_[h % 2]
                        for r_ in range(2):
                            if PAIR:
                                for e_ in range(2):
                                    k = 2 * h + e_
                                    S.dma("sp", kT_.t[:, e_, r_ * T:(r_ + 1) * T], akc[k][r_ * 128:(r_ + 1) * 128, 0:T], reads=["akc%d" % k], writes=[kT_.k])
                                for k in range(NVC):
                                    nt_ = min(2, NT - 2 * k)
                                    S.dma("sp", v_.t[:, r_ * NT + 2 * k:r_ * NT + 2 * k + nt_, 0:256],
                                          avc[k][r_ * 256:r_ * 256 + nt_ * 128, h * 256:(h + 1) * 256].rearrange("(k p) c -> p k c", p=128),
                                          reads=["avc%d" % k], writes=[v_.k])
                            else:
                                S.dma("sp", kT_.t[:, :, r_ * T:(r_ + 1) * T], kalls[r_][:, 2 * h:2 * h + 2, 0:T], reads=kvr, writes=[kT_.k])
                                S.dma("sp", v_.t[:, r_ * NT:(r_ + 1) * NT, 0:256], valls[r_][:, 0:NT, h * 256:(h + 1) * 256], reads=kvr, writes=[v_.k])
                        S.dma("sp", q_.t[:], qall[:, 2 * h:2 * h + 2, :], reads=[], writes=[q_.k])
                        qblocks = []
                        t0 = 0
                        while t0 < TLAT:
                            tw = min(256, TLAT - t0)
                            qblocks.append((t0, tw, [kt_ for kt_ in range(NKT)]))
                            t0 += tw
                        qblocks.append((TLAT, 128, [NTL, NT + NTL]))
                        for (q0, qw, ktiles) in qblocks:
                            nq = qw // 128
                            def qk(ki):
                                kt_ = ktiles[ki]
                                pst = PF[4 + ki % 2]
                                for e_ in range(2):
                                    S.op("pe", lambda e, e_=e_, kt_=kt_, pst=pst: e.matmul(pst.t[:, e_ * 256:e_ * 256 + qw], lhsT=kT_.t[:, e_, kt_ * 128:(kt_ + 1) * 128],
                                                                                        rhs=q_.t[:, e_, q0:q0 + qw], start=True, stop=True),
                                         reads=[kT_.k, q_.k], writes=[pst.k])

                            def exp_pv(ki):
                                kt_ = ktiles[ki]
                                pst = PF[4 + ki % 2]
                                p_ = pt[npt[0] % 3]
                                npt[0] += 1
                                S.op("act", lambda e: e.activation(out=p_.t[:, :, 0:qw], in_=pst.t[:, :].rearrange("p (e q) -> p e q", e=2)[:, :, 0:qw], func=AF.Exp),
                                     reads=[pst.k], writes=[p_.k])
                                for e_ in range(2):
                                    for qt_ in range(nq):
                                        po = PF[e_ * 2 + qt_]
                                        S.op("pe", lambda e, e_=e_, qt_=qt_, po=po, kt_=kt_: e.matmul(
                                            po.t[:, 0:257], lhsT=p_.t[:, e_, qt_ * 128:(qt_ + 1) * 128], rhs=v_.t[:, kt_, :],
                                            start=(ki == 0), stop=(ki == len(ktiles) - 1)), reads=[p_.k, v_.k], writes=[po.k])

                            qk(0)
                            for ki in range(len(ktiles)):
                                if ki + 1 < len(ktiles):
                                    qk(ki + 1)
                                exp_pv(ki)
                            for qt_ in range(nq):
                                j = q0 // 128 + qt_
                                p0 = PF[qt_]
                                p1 = PF[2 + qt_]
                                S.op("dve", lambda e: e.reciprocal(out=rd.t[:, 0:1], in_=p0.t[:, 256:257]), reads=[p0.k], writes=[rd.k])
                                S.op("dve", lambda e: e.reciprocal(out=rd.t[:, 1:2], in_=p1.t[:, 256:257]), reads=[p1.k], writes=[rd.k])
                                S.op("dve", lambda e: e.tensor_tensor(out=rd.t[:, 1:2], in0=rd.t[:, 1:2], in1=lamt.t[:, 0:1], op=ALU.mult), reads=[rd.k, lamt.k], writes=[rd.k])
                                S.op("dve", lambda e: e.tensor_scalar(out=o1.t[:], in0=p1.t[:, 0:256], scalar1=rd.t[:, 1:2], scalar2=None, op0=ALU.mult), reads=[p1.k, rd.k], writes=[o1.k])
                                S.op("dve", lambda e: e.scalar_tensor_tensor(out=o1.t[:], in0=p0.t[:, 0:256], scalar=rd.t[:, 0:1], in1=o1.t[:], op0=ALU.mult, op1=ALU.add),
                                     reads=[p0.k, rd.k, o1.k], writes=[o1.k])
                                S.op("act", lambda e: e.activation(out=o2.t[:], in_=o1.t[:], func=AF.Square, accum_out=oss.t[:]), reads=[o1.k], writes=[o2.k, oss.k])
                                S.op("act", lambda e: e.activation(out=oss.t[:], in_=oss.t[:], func=AF.Sqrt, scale=1.0 / 256, bias=EPS), reads=[oss.k], writes=[oss.k])
                                S.op("dve", lambda e: e.reciprocal(out=oss.t[:], in_=oss.t[:]), reads=[oss.k], writes=[oss.k])
                                S.op("dve", lambda e: e.scalar_tensor_tensor(out=ob16.t[:], in0=o1.t[:], scalar=oss.t[:, 0:1], in1=sgt.t[:], op0=ALU.mult, op1=ALU.mult),
                                     reads=[o1.k, oss.k, sgt.k], writes=[ob16.k])
                                transpose_into(XT, ob16, j, nchunks=2, kc0=2 * h)
                    S.barrier()
                with ExitStack() as ph5:
                    epi = make_resid_epilogue(ph5, V, i, 2, V.xres, V.xres)
                    lin_tm(ph5, XT, "wout%d" % i, range(4), epi)
                    S.barrier()

        def ffn_layer(V, i, last):
            with ExitStack() as ph:
                XT = sb(ph, "XT", [128, 16, T], BF16)
                with ExitStack() as ph1:
                    norm_phase(ph1, XT, i, 1, V.xres)
                    S.barrier()
                with ExitStack() as ph2:
                    ffn_gu(ph2, XT, i)
                    S.barrier()
            with ExitStack() as ph3:
                ffn_down(ph3, V, i, V.xres, last)
                S.barrier()

        def pair_exchange(slot, parts):
            for p_, (src_, dst_) in enumerate(parts):
                S.dma("sp", ex_in[slot][p_][:, 0:EXW], src_[:, :])
            S.barrier(pool=True)
            for p_ in range(len(parts)):
                S.collective("AllGather", ALU.bypass, PAIRS, ex_in[slot][p_].ap().opt(), ex_out[slot][p_].ap().opt(), writes=["exout%d_%d" % (slot, p_)])
            npart = len(parts)
            with ExitStack() as ph:
                exg = sb(ph, "exg", [128, 2, EXW], F32)
                exo = sb(ph, "exo", [128, EXW], F32)
                for p_, (src_, dst_) in enumerate(parts):
                    for r_ in range(2):
                        S.dma("sp", exg.t[:, r_, :], ex_out[slot][p_][r_ * 128:(r_ + 1) * 128, 0:EXW], reads=["exout%d_%d" % (slot, p_)], writes=[exg.k])
                    S.op("dve", lambda e: e.tensor_scalar(out=exo.t[:], in0=exg.t[:, 0, :], scalar1=pmk.t[:, 0:1], scalar2=None, op0=ALU.mult),
                         reads=[exg.k, pmk.k], writes=[exo.k])
                    S.op("dve", lambda e: e.scalar_tensor_tensor(out=exo.t[:], in0=exg.t[:, 1, :], scalar=pmk.t[:, 1:2], in1=exo.t[:], op0=ALU.mult, op1=ALU.add),
                         reads=[exg.k, pmk.k, exo.k], writes=[exo.k])
                    S.dma("act", dst_[:, :], exo.t[:], reads=[exo.k])
                S.barrier(pool=True)

        def program():
            cast_layer(0)
            ada_phase()
            if stop(1):
                return
            for i in range(DEPTH):
                if i % 2 == 0:
                    for V in VCS:
                        mlstm_pre(V, i, V.xin if i == 0 else V.xres)
                        if stopped[0]:
                            return
                    if stop(4):
                        return
                    if PAIR:
                        V = VCS[0]
                        mlstm_scan(V, PV, i, 1)
                        pair_exchange(0, [(V.st[0], PV.st[0]), (V.st[1], PV.st[1])])
                        mlstm_scan(V, PV, i, 2)
                        pair_exchange(1, [(V.st[2], PV.st[2])])
                        mlstm_stage3(V, PV, i)
                    else:
                        for st_ in (1, 2):
                            for V in VCS:
                                mlstm_scan(V, VCS[1 - V.idx], i, st_)
                        for V in VCS:
                            mlstm_stage3(V, VCS[1 - V.idx], i)
                else:
                    for V in VCS:
                        attn_pre(V, i, V.xres)
                    if PAIR:
                        S.barrier(pool=True)
                        for k in range(16):
                            S.collective("AllGather", ALU.bypass, PAIRS, akc_in[k].ap().opt(), akc[k].ap().opt(), writes=["akc%d" % k])
                        for k in range(NVC):
                            S.collective("AllGather", ALU.bypass, PAIRS, avc_in[k].ap().opt(), avc[k].ap().opt(), writes=["avc%d" % k])
                    for V in VCS:
                        attn_core(V, i)
                if stop(6):
                    return
                if i + 1 < DEPTH:
                    S.barrier(pool=True)
                    cast_layer(i + 1)
                for V in VCS:
                    ffn_layer(V, i, i == DEPTH - 1)

        program()
        S.finish()
    build.ninstr = S.ninstr
    return nc


def _prep(inputs, S_len, CTX, DEPTH, ncore, pair=False):
    x = np.asarray(inputs["x"], np.float32)
    ctx = np.asarray(inputs["ctx"], np.float32)
    c = np.asarray(inputs["c"], np.float32)
    c_ctx = np.asarray(inputs["c_ctx"], np.float32)
    NTL = S_len // 2 // 128
    HL = S_len // 2
    HC = CTX // 2
    assert HC == 128
    NA = (DEPTH + 1) // 2
    NBL = DEPTH // 2
    g = lambda k: np.asarray(inputs[k], np.float32)
    n_freq = 32
    freqs = (ROPE_BASE ** (-np.arange(n_freq, dtype=np.float32) / n_freq)).astype(np.float32)
    tok = np.arange(S_len)
    ang = np.stack([tok // GRID_W, tok % GRID_W], -1).astype(np.float32)[:, :, None] * freqs
    cos = np.cos(ang).astype(np.float32).reshape(S_len, 64)
    sin = np.sin(ang).astype(np.float32).reshape(S_len, 64)
    rope_full = np.concatenate([cos, sin], 1)
    rope_ctx = np.concatenate([np.ones((HC, 64), np.float32), np.zeros((HC, 64), np.float32)], 1)
    ident = np.eye(128, dtype=np.float32)
    triU = np.triu(np.ones((128, 128), np.float32))
    triL = np.tril(np.ones((128, 128), np.float32))
    consts = np.concatenate([ident, triU, triL, np.ones((128, 128), np.float32)], 1)
    mwin = g("mlstm_w_in")
    selm = np.zeros((2, 256), np.float32)
    selm[0, 0:128] = 1.0
    selm[1, 128:256] = 1.0
    perm1 = np.concatenate([np.arange(16, 32), np.arange(0, 16)])
    wg0 = mwin[:NA, :, 6144:]
    m_wg = np.ascontiguousarray(np.stack([wg0, wg0[:, :, perm1]], 0))
    gb0 = g("mlstm_gate_b")[:NA]
    m_gb = np.ascontiguousarray(np.stack([gb0, gb0[:, perm1]], 0))
    shared = {
        "sel": selm,
        "ada_w": g("ada_w")[:DEPTH],
        "ada_b": g("ada_b")[:DEPTH],
        "norm_g": g("norm_g")[:DEPTH],
        "m_win": np.ascontiguousarray(mwin[:NA, :, 0:6144]),
        "m_wg": m_wg,
        "m_gb": m_gb,
        "m_hg": g("mlstm_head_g")[:NA],
        "m_wout": g("mlstm_w_out")[:NA],
        "f_wgu": g("ffn_w_gu")[:DEPTH],
        "f_wdn": g("ffn_w_down")[:DEPTH],
        "consts": consts,
    }
    if NBL:
        shared["d_win"] = g("diff_w_in")[:NBL]
        shared["d_wout"] = g("diff_w_out")[:NBL]
        shared["d_vec"] = np.ascontiguousarray(np.stack([g("diff_q_g")[:NBL], g("diff_k_g")[:NBL], g("diff_lq1")[:NBL], g("diff_lk1")[:NBL],
                                                         g("diff_lq2")[:NBL], g("diff_lk2")[:NBL]], 1))
        shared["d_sg"] = g("diff_subln_g")[:NBL]
    in_maps = []
    if pair:
        if DEPTH % 2 == 0:
            ada_split = [np.ascontiguousarray(g("ada_w")[s_:DEPTH:2]) for s_ in range(2)]
        for r in range(ncore):
            b, s_ = r // 2, r % 2
            xl = x[b, s_ * HL:(s_ + 1) * HL]
            cl = ctx[b, s_ * HC:(s_ + 1) * HC]
            rl = rope_full[s_ * HL:(s_ + 1) * HL]
            if s_ == 1:
                xl, cl, rl = xl[::-1], cl[::-1], rl[::-1]
            m = dict(shared)
            if DEPTH % 2 == 0:
                m["ada_w"] = ada_split[s_]
            m["m_wg"] = np.ascontiguousarray(m_wg[s_])
            m["m_gb"] = np.ascontiguousarray(m_gb[s_])
            m["xin"] = np.ascontiguousarray(np.concatenate([xl, cl], 0))
            m["rope"] = np.ascontiguousarray(np.concatenate([rl, rope_ctx], 0))
            m["cT"] = np.ascontiguousarray(np.stack([c[b], c_ctx], 1))
            pm = np.zeros((128, 2), np.float32)
            pm[:, 1 - s_] = 1.0
            m["pmask"] = pm
            in_maps.append(m)
        return in_maps, NTL
    for b in range(ncore):
        xs, rs = [], []
        for s_ in range(2):
            xl = x[b, s_ * HL:(s_ + 1) * HL]
            cl = ctx[b, s_ * HC:(s_ + 1) * HC]
            rl = rope_full[s_ * HL:(s_ + 1) * HL]
            if s_ == 1:
                xl, cl, rl = xl[::-1], cl[::-1], rl[::-1]
            xs.append(np.concatenate([xl, cl], 0))
            rs.append(np.concatenate([rl, rope_ctx], 0))
        m = dict(shared)
        m["xin"] = np.ascontiguousarray(np.stack(xs, 0))
        m["rope"] = np.ascontiguousarray(np.stack(rs, 0))
        m["cT"] = np.ascontiguousarray(np.stack([c[b], c_ctx], 1))
        in_maps.append(m)
    return in_maps, NTL


_cache = {}


def run(inputs, DEPTH=4, ncore=None, pair=True):
    x = np.asarray(inputs["x"])
    B, S_len, _ = x.shape
    if ncore is None:
        ncore = 2 * B if pair else B
    CTX = np.asarray(inputs["ctx"]).shape[1]
    in_maps, NTL = _prep(inputs, S_len, CTX, DEPTH, ncore, pair)
    key = (NTL, DEPTH, pair)
    if key not in _cache:
        _cache[key] = build(NTL, DEPTH, pair)
    nc = _cache[key]
    res = run_bass_kernel_spmd(nc, in_maps, core_ids=list(range(ncore)))
    HL = S_len // 2
    out = np.zeros((B, S_len, D), np.float32)
    if pair:
        for r in range(ncore):
            b, s_ = r // 2, r % 2
            y = res.results[r]["y"]
            out[b, s_ * HL:(s_ + 1) * HL] = y if s_ == 0 else y[::-1]
        return out
    for b in range(ncore):
        y = res.results[b]["y"]
        out[b, 0:HL] = y[0]
        out[b, HL:] = y[1][::-1]
    return out


def kernel(**inputs):
    return run(inputs, DEPTH=4)
```
